# Optimizing a Trainium2 kernel written in Bass

```python
import jax
import jax.numpy as jnp
from jax import lax
import numpy as np


D_MODEL = 1024
BATCH = 2
SEQ = 8192
DEPTH = 2

GRID_W = 64
CTX_LEN = 256
EPS = 1e-6

A_HEADS = 4
A_HEAD_DIM = 128
A_WIDTH = A_HEADS * A_HEAD_DIM
A_IN = 5 * A_WIDTH
A_CHUNK = 64
POOL_WINDOWS = (2, 4, 8, 16)
B_GROUP = 128
B_WIDTH = B_GROUP * len(POOL_WINDOWS)
AB_IN = A_IN + B_WIDTH
AB_MIX = A_WIDTH + B_WIDTH
C_HEADS = 8
C_HEAD_DIM = 64
C_WIDTH = C_HEADS * C_HEAD_DIM
NA_ROWS_MAX = 8
NA_COLS = 16
D_HEADS = 8
MLA_Q_RANK = 256
MLA_KV_RANK = 128
MLA_NOPE = 64
MLA_ROPE = 32
MLA_V = 64
D_WIDTH = D_HEADS * MLA_V
CD_IN = 3 * C_WIDTH + MLA_Q_RANK + MLA_KV_RANK + MLA_ROPE
CD_MIX = C_WIDTH + D_WIDTH
ROPE_THETA = 10000.0
ATTN_BLOCK = 128
MOE_GROUPS = 4
MOE_EXPERTS_PER_GROUP = 8
MOE_TOPK = 2
MOE_HIDDEN = 256

N_AB_LAYERS = (DEPTH + 1) // 2
N_CD_LAYERS = DEPTH // 2

kernel_name = 'hybrid_hgrn2_pool_natten_mla_hmoe_diffusion'


def rmsnorm(x, g):
    xf = x.astype(jnp.float32)
    y = xf * lax.rsqrt(jnp.mean(xf * xf, axis=-1, keepdims=True) + EPS)
    return (y * g.astype(jnp.float32)).astype(x.dtype)


def ada_modulate(x, g, shift, scale):
    return rmsnorm(x, g) * (1.0 + scale) + shift


def _heads(a, n_heads):
    b, n, _ = a.shape
    return a.reshape(b, n, n_heads, -1).transpose(0, 2, 1, 3)


def _merge(a):
    b, h, n, d = a.shape
    return a.transpose(0, 2, 1, 3).reshape(b, n, h * d)


def axial_rope(x, pos_r, pos_c):
    half = x.shape[-1] // 2
    n_freq = half // 2
    inv = ROPE_THETA ** (-jnp.arange(n_freq, dtype=jnp.float32) / n_freq)

    def rot(xa, pos):
        ang = pos[:, None] * inv[None, :]
        cos, sin = jnp.cos(ang), jnp.sin(ang)
        x1, x2 = xa[..., :n_freq], xa[..., n_freq:]
        return jnp.concatenate([x1 * cos - x2 * sin, x1 * sin + x2 * cos], axis=-1)

    xf = x.astype(jnp.float32)
    return jnp.concatenate([rot(xf[..., :half], pos_r), rot(xf[..., half:], pos_c)], axis=-1).astype(x.dtype)


def gla_chunk_scan(q, k, v, logf, s0):
    b_, h, n, _ = q.shape
    nc = n // A_CHUNK

    def chunks(a):
        return jnp.moveaxis(a.reshape(b_, h, nc, A_CHUNK, a.shape[-1]), 2, 0)

    incl = jnp.tril(jnp.ones((A_CHUNK, A_CHUNK), dtype=bool))

    def step(s, inp):
        qc, kc, vc, gc = inp
        bcum = jnp.cumsum(gc, axis=2)
        o_inter = jnp.einsum('bhtd,bhde->bhte', qc * jnp.exp(bcum), s)
        diff = bcum[:, :, :, None, :] - bcum[:, :, None, :, :]
        decay = jnp.exp(jnp.where(incl[:, :, None], diff, -jnp.inf))
        att = jnp.einsum('bhtd,bhsd,bhtsd->bhts', qc, kc, decay)
        o = o_inter + jnp.einsum('bhts,bhse->bhte', att, vc)
        b_last = bcum[:, :, -1:, :]
        s_new = jnp.exp(b_last[:, :, 0, :, None]) * s + jnp.einsum('bhsd,bhse->bhde', kc * jnp.exp(b_last - bcum), vc)
        return s_new, o

    s_fin, o = lax.scan(step, s0, (chunks(q), chunks(k), chunks(v), chunks(logf)))
    o = jnp.moveaxis(o, 0, 2).reshape(b_, h, n, v.shape[-1])
    return o, s_fin


def hgrn2_inputs(u, lb):
    uf = u.astype(jnp.float32)
    q, f_fwd, f_bwd, i, g = jnp.split(uf, 5, axis=-1)
    dirs = []
    for d, fz in enumerate((f_fwd, f_bwd)):
        f = lb[d] + (1.0 - lb[d]) * jax.nn.sigmoid(fz)
        dirs.append((_heads(1.0 - f, A_HEADS), _heads(jnp.log(f), A_HEADS)))
    return _heads(jax.nn.silu(q), A_HEADS), _heads(i, A_HEADS), dirs, g


def hgrn2_bidir(u_ctx, u_lat, lb, onorm_g):
    q_c, i_c, dirs_c, g_c = hgrn2_inputs(u_ctx, lb)
    q_l, i_l, dirs_l, g_l = hgrn2_inputs(u_lat, lb)
    (kf_c, gf_c), (kb_c, gb_c) = dirs_c
    (kf_l, gf_l), (kb_l, gb_l) = dirs_l
    s0 = jnp.zeros(q_c.shape[:2] + (A_HEAD_DIM, A_HEAD_DIM), jnp.float32)

    def flip(a):
        return jnp.flip(a, axis=2)

    o_cf, s_cf = gla_chunk_scan(q_c, kf_c, i_c, gf_c, s0)
    o_lf, _ = gla_chunk_scan(q_l, kf_l, i_l, gf_l, s_cf)
    o_cb, s_cb = gla_chunk_scan(flip(q_c), flip(kb_c), flip(i_c), flip(gb_c), s0)
    o_lb, _ = gla_chunk_scan(flip(q_l), flip(kb_l), flip(i_l), flip(gb_l), s_cb)

    def readout(o, g, dtype):
        return (_merge(rmsnorm(o, onorm_g)) * jax.nn.silu(g)).astype(dtype)

    return readout(o_cf + flip(o_cb), g_c, u_ctx.dtype), readout(o_lf + flip(o_lb), g_l, u_lat.dtype)


def multiscale_pool(u, w, scale):
    b_, n, _ = u.shape
    uf = u.astype(jnp.float32)
    cs = jnp.concatenate([jnp.zeros((b_, 1, B_WIDTH), jnp.float32), jnp.cumsum(uf, axis=1)], axis=1)
    t = jnp.arange(n)
    outs = []
    for gi, win in enumerate(POOL_WINDOWS):
        sl = slice(gi * B_GROUP, (gi + 1) * B_GROUP)
        lo = jnp.clip(t - win // 2, 0, n)
        hi = jnp.clip(t + win - win // 2, 0, n)
        mean = (cs[:, hi, sl] - cs[:, lo, sl]) / (hi - lo).astype(jnp.float32)[None, :, None]
        outs.append(jnp.einsum('bnc,cd->bnd', mean - uf[:, :, sl], w[gi].astype(jnp.float32)))
    return (jnp.concatenate(outs, axis=-1) * scale.astype(jnp.float32)).astype(u.dtype)


def ab_mixer(h_ctx, h_lat, need_ctx, w_in, w_out, lb, onorm_g, pool_w, pool_scale):
    u_ctx = h_ctx @ w_in
    u_lat = h_lat @ w_in
    a_ctx, a_lat = hgrn2_bidir(u_ctx[..., :A_IN], u_lat[..., :A_IN], lb, onorm_g)
    o_lat = jnp.concatenate([a_lat, multiscale_pool(u_lat[..., A_IN:], pool_w, pool_scale)], axis=-1) @ w_out
    o_ctx = None
    if need_ctx:
        o_ctx = jnp.concatenate([a_ctx, multiscale_pool(u_ctx[..., A_IN:], pool_w, pool_scale)], axis=-1) @ w_out
    return o_ctx, o_lat


def dense_attention(q, k, v, scale):
    s = jnp.einsum('bhqd,bhkd->bhqk', q, k).astype(jnp.float32) * scale
    p = jax.nn.softmax(s, axis=-1).astype(v.dtype)
    return jnp.einsum('bhqk,bhkd->bhqd', p, v)


def neighbourhood_attention(q_lat, k_lat, v_lat, k_ctx, v_ctx, rpb):
    b_, h, n, dh = q_lat.shape
    rows = n // GRID_W
    wr = min(NA_ROWS_MAX, rows)
    scale = dh ** -0.5
    kg = k_lat.reshape(b_, h, rows, GRID_W, dh)
    vg = v_lat.reshape(b_, h, rows, GRID_W, dh)
    qg = jnp.moveaxis(q_lat.reshape(b_, h, rows, GRID_W, dh), 2, 0)
    col = jnp.arange(GRID_W)
    col_start = jnp.clip(col - NA_COLS // 2, 0, GRID_W - NA_COLS)
    col_idx = col_start[:, None] + jnp.arange(NA_COLS)[None, :]
    dc = col_idx - col[:, None] + (NA_COLS - 1)
    rpb_f = rpb.astype(jnp.float32)
    n_loc = wr * NA_COLS

    def row_block(args):
        r, q_row = args
        rs = jnp.clip(r - wr // 2, 0, rows - wr)
        k_win = lax.dynamic_slice_in_dim(kg, rs, wr, axis=2)[:, :, :, col_idx]
        v_win = lax.dynamic_slice_in_dim(vg, rs, wr, axis=2)[:, :, :, col_idx]
        dr = rs + jnp.arange(wr) - r + (NA_ROWS_MAX - 1)
        bias = rpb_f[:, dr[None, :, None], dc[:, None, :]]
        s_loc = jnp.einsum('bhqd,bhrqjd->bhqrj', q_row, k_win).astype(jnp.float32) * scale + bias
        s_ctx = jnp.einsum('bhqd,bhkd->bhqk', q_row, k_ctx).astype(jnp.float32) * scale
        s = jnp.concatenate([s_loc.reshape(b_, h, GRID_W, n_loc), s_ctx], axis=-1)
        p = jax.nn.softmax(s, axis=-1).astype(v_lat.dtype)
        p_loc = p[..., :n_loc].reshape(b_, h, GRID_W, wr, NA_COLS)
        return (jnp.einsum('bhqrj,bhrqjd->bhqd', p_loc, v_win)
                + jnp.einsum('bhqk,bhkd->bhqd', p[..., n_loc:], v_ctx))

    o = lax.map(row_block, (jnp.arange(rows), qg))
    return jnp.moveaxis(o, 0, 2).reshape(b_, h, n, dh)


def mla_q(cq, q_norm_g, w_uq, pos):
    q = _heads(rmsnorm(cq, q_norm_g) @ w_uq, D_HEADS)
    q_nope, q_rope = q[..., :MLA_NOPE], q[..., MLA_NOPE:]
    if pos is not None:
        q_rope = axial_rope(q_rope, pos[0], pos[1])
    return jnp.concatenate([q_nope, q_rope], axis=-1)


def mla_kv(ckv, k_rope, kv_norm_g, w_ukv, pos):
    kv = _heads(rmsnorm(ckv, kv_norm_g) @ w_ukv, D_HEADS)
    k_nope, v = kv[..., :MLA_NOPE], kv[..., MLA_NOPE:]
    kr = k_rope[:, None]
    if pos is not None:
        kr = axial_rope(kr, pos[0], pos[1])
    kr = jnp.broadcast_to(kr, k_nope.shape[:3] + (MLA_ROPE,))
    return jnp.concatenate([k_nope, kr], axis=-1), v


def blocked_attention(q, k_all, v_all, scale):
    b_, h, n, dq = q.shape
    nb = n // ATTN_BLOCK
    qb = jnp.moveaxis(q.reshape(b_, h, nb, ATTN_BLOCK, dq), 2, 0)
    o = lax.map(lambda q_blk: dense_attention(q_blk, k_all, v_all, scale), qb)
    return jnp.moveaxis(o, 0, 2).reshape(b_, h, n, v_all.shape[-1])


def split_cd(u):
    b_, n, _ = u.shape
    qkv = u[..., :3 * C_WIDTH].reshape(b_, n, 3, C_HEADS, C_HEAD_DIM).transpose(2, 0, 3, 1, 4)
    o = 3 * C_WIDTH
    cq = u[..., o:o + MLA_Q_RANK]
    ckv = u[..., o + MLA_Q_RANK:o + MLA_Q_RANK + MLA_KV_RANK]
    kr = u[..., o + MLA_Q_RANK + MLA_KV_RANK:]
    return qkv[0], qkv[1], qkv[2], cq, ckv, kr


def cd_mixer(h_ctx, h_lat, need_ctx, w_in, w_out, rpb, q_norm_g, w_uq, kv_norm_g, w_ukv):
    n_lat = h_lat.shape[1]
    t = jnp.arange(n_lat)
    pos = ((t // GRID_W).astype(jnp.float32), (t % GRID_W).astype(jnp.float32))
    nq_c, nk_c, nv_c, cq_c, ckv_c, kr_c = split_cd(h_ctx @ w_in)
    nq_l, nk_l, nv_l, cq_l, ckv_l, kr_l = split_cd(h_lat @ w_in)
    d_scale = (MLA_NOPE + MLA_ROPE) ** -0.5
    c_lat = neighbourhood_attention(nq_l, nk_l, nv_l, nk_c, nv_c, rpb)
    dk_c, dv_c = mla_kv(ckv_c, kr_c, kv_norm_g, w_ukv, None)
    dk_l, dv_l = mla_kv(ckv_l, kr_l, kv_norm_g, w_ukv, pos)
    dq_l = mla_q(cq_l, q_norm_g, w_uq, pos)
    d_lat = blocked_attention(dq_l, jnp.concatenate([dk_c, dk_l], axis=2), jnp.concatenate([dv_c, dv_l], axis=2), d_scale)
    o_lat = jnp.concatenate([_merge(c_lat), _merge(d_lat)], axis=-1) @ w_out
    o_ctx = None
    if need_ctx:
        c_c = dense_attention(nq_c, nk_c, nv_c, C_HEAD_DIM ** -0.5)
        d_c = dense_attention(mla_q(cq_c, q_norm_g, w_uq, None), dk_c, dv_c, d_scale)
        o_ctx = jnp.concatenate([_merge(c_c), _merge(d_c)], axis=-1) @ w_out
    return o_ctx, o_lat


def hier_moe(h, w_rg, b_rg, w_re, b_re, w_gate, w_up, w_down):
    b_, n, d = h.shape
    x = h.reshape(-1, d)
    tok = x.shape[0]
    g_prob = jax.nn.softmax((x @ w_rg + b_rg).astype(jnp.float32), axis=-1)
    g_p, g_idx = lax.top_k(g_prob, 1)
    e_logits = (x @ w_re + b_re).astype(jnp.float32).reshape(tok, MOE_GROUPS, MOE_EXPERTS_PER_GROUP)
    e_logits = jnp.take_along_axis(e_logits, g_idx[:, :, None], axis=1)[:, 0]
    e_p, e_idx = lax.top_k(jax.nn.softmax(e_logits, axis=-1), MOE_TOPK)
    e_p = e_p / jnp.sum(e_p, axis=-1, keepdims=True)
    e_w = jnp.sum(jax.nn.one_hot(e_idx, MOE_EXPERTS_PER_GROUP, dtype=jnp.float32) * e_p[..., None], axis=1)
    comb = (g_p[:, :, None] * jax.nn.one_hot(g_idx[:, 0], MOE_GROUPS, dtype=jnp.float32)[:, :, None]
            * e_w[:, None, :]).astype(h.dtype)
    y = jnp.zeros_like(x)
    for g in range(MOE_GROUPS):
        a = jax.nn.silu(jnp.einsum('td,edf->tef', x, w_gate[g])) * jnp.einsum('td,edf->tef', x, w_up[g])
        y = y + jnp.einsum('tef,efd->td', a * comb[:, g, :, None], w_down[g])
    return y.reshape(b_, n, d)


def setup_inputs(seed: int = 0) -> dict:
    key = jax.random.key(seed)
    ks = jax.random.split(key, 32)
    counter = [0]

    def nrm(shape, scale):
        k = ks[counter[0]]
        counter[0] += 1
        return jax.random.normal(k, shape, jnp.float32) * scale

    def gain(shape):
        return 1.0 + nrm(shape, 0.02)

    G, E, F = MOE_GROUPS, MOE_EXPERTS_PER_GROUP, MOE_HIDDEN
    return {
        'x': nrm((BATCH, SEQ, D_MODEL), 1.0),
        'c': nrm((BATCH, D_MODEL), 1.0),
        'ctx': nrm((BATCH, CTX_LEN, D_MODEL), 1.0),
        'c_ctx': nrm((D_MODEL,), 1.0),
        'ada_w': nrm((DEPTH, D_MODEL, 6 * D_MODEL), 0.5 * D_MODEL ** -0.5),
        'ada_b': nrm((DEPTH, 6 * D_MODEL), 0.02),
        'norm1_g': gain((DEPTH, D_MODEL)),
        'norm2_g': gain((DEPTH, D_MODEL)),
        'ab_w_in': nrm((N_AB_LAYERS, D_MODEL, AB_IN), D_MODEL ** -0.5),
        'ab_w_out': nrm((N_AB_LAYERS, AB_MIX, D_MODEL), AB_MIX ** -0.5),
        'hgrn_lb_logits': nrm((2, N_AB_LAYERS + 1, A_WIDTH), 0.1),
        'hgrn_onorm_g': gain((N_AB_LAYERS, A_HEAD_DIM)),
        'pool_w': nrm((N_AB_LAYERS, len(POOL_WINDOWS), B_GROUP, B_GROUP), B_GROUP ** -0.5),
        'pool_scale': gain((N_AB_LAYERS, B_WIDTH)),
        'cd_w_in': nrm((N_CD_LAYERS, D_MODEL, CD_IN), D_MODEL ** -0.5),
        'cd_w_out': nrm((N_CD_LAYERS, CD_MIX, D_MODEL), CD_MIX ** -0.5),
        'na_rpb': nrm((N_CD_LAYERS, C_HEADS, 2 * NA_ROWS_MAX - 1, 2 * NA_COLS - 1), 0.1),
        'mla_q_norm_g': gain((N_CD_LAYERS, MLA_Q_RANK)),
        'mla_w_uq': nrm((N_CD_LAYERS, MLA_Q_RANK, D_HEADS * (MLA_NOPE + MLA_ROPE)), MLA_Q_RANK ** -0.5),
        'mla_kv_norm_g': gain((N_CD_LAYERS, MLA_KV_RANK)),
        'mla_w_ukv': nrm((N_CD_LAYERS, MLA_KV_RANK, D_HEADS * (MLA_NOPE + MLA_V)), MLA_KV_RANK ** -0.5),
        'moe_w_rg': nrm((DEPTH, D_MODEL, G), D_MODEL ** -0.5),
        'moe_b_rg': nrm((DEPTH, G), 0.01),
        'moe_w_re': nrm((DEPTH, D_MODEL, G * E), D_MODEL ** -0.5),
        'moe_b_re': nrm((DEPTH, G * E), 0.01),
        'moe_w_gate': nrm((DEPTH, G, E, D_MODEL, F), D_MODEL ** -0.5),
        'moe_w_up': nrm((DEPTH, G, E, D_MODEL, F), D_MODEL ** -0.5),
        'moe_w_down': nrm((DEPTH, G, E, F, D_MODEL), F ** -0.5),
        'final_norm_g': gain((D_MODEL,)),
    }


def reference(x, c, ctx, c_ctx, ada_w, ada_b, norm1_g, norm2_g, ab_w_in, ab_w_out, hgrn_lb_logits,
              hgrn_onorm_g, pool_w, pool_scale, cd_w_in, cd_w_out, na_rpb, mla_q_norm_g, mla_w_uq,
              mla_kv_norm_g, mla_w_ukv, moe_w_rg, moe_b_rg, moe_w_re, moe_b_re, moe_w_gate, moe_w_up,
              moe_w_down, final_norm_g):
    lb_all = jnp.cumsum(jax.nn.softmax(hgrn_lb_logits.astype(jnp.float32), axis=1), axis=1)
    xc = ctx
    for layer in range(DEPTH):
        last = layer == DEPTH - 1
        k = layer // 2
        m_lat = (jax.nn.silu(c) @ ada_w[layer] + ada_b[layer])[:, None, :]
        m_ctx = (jax.nn.silu(c_ctx) @ ada_w[layer] + ada_b[layer])[None, None, :]
        sh1, sc1, g1, sh2, sc2, g2 = jnp.split(m_lat, 6, axis=-1)
        sh1c, sc1c, g1c, sh2c, sc2c, g2c = jnp.split(m_ctx, 6, axis=-1)
        h_lat = ada_modulate(x, norm1_g[layer], sh1, sc1)
        h_ctx = ada_modulate(xc, norm1_g[layer], sh1c, sc1c)
        if layer % 2 == 0:
            o_ctx, o_lat = ab_mixer(h_ctx, h_lat, not last, ab_w_in[k], ab_w_out[k], lb_all[:, k],
                                    hgrn_onorm_g[k], pool_w[k], pool_scale[k])
        else:
            o_ctx, o_lat = cd_mixer(h_ctx, h_lat, not last, cd_w_in[k], cd_w_out[k], na_rpb[k],
                                    mla_q_norm_g[k], mla_w_uq[k], mla_kv_norm_g[k], mla_w_ukv[k])
        moe_p = (moe_w_rg[layer], moe_b_rg[layer], moe_w_re[layer], moe_b_re[layer],
                 moe_w_gate[layer], moe_w_up[layer], moe_w_down[layer])
        x = x + g1 * o_lat
        x = x + g2 * hier_moe(ada_modulate(x, norm2_g[layer], sh2, sc2), *moe_p)
        if not last:
            xc = xc + g1c * o_ctx
            xc = xc + g2c * hier_moe(ada_modulate(xc, norm2_g[layer], sh2c, sc2c), *moe_p)
    return rmsnorm(x, final_norm_g)
```

```python
import bisect
import contextlib
import numpy as np
import concourse.bass as bass
import concourse.mybir as mybir
from concourse.bass_utils import run_bass_kernel_spmd

F32 = mybir.dt.float32
BF16 = mybir.dt.bfloat16
AF = mybir.ActivationFunctionType
ALU = mybir.AluOpType
AX = mybir.AxisListType

NCORES = 8
D = 1024
KC = 8
NL = 2048
NCX = 256
NT = NCX + NL
EPS = 1e-6
BIG = 30000.0


class Res:
    __slots__ = ("name", "w", "r", "dsem", "dcnt")

    def __init__(self, name):
        self.name = name
        self.w = None
        self.r = []
        self.dsem = None
        self.dcnt = 0


class Eng:
    def __init__(self, k, name, h):
        self.k = k
        self.name = name
        self.h = h
        self.sem = k.new_sem("e_" + name)
        self.tick = 0
        self.nsig = 0
        self.sig_ticks = []
        self.sig_vals = []
        self.known = {}


class K:
    def __init__(self):
        self.nc = bass.Bass("TRN2", target_bir_lowering=False)
        self.es = contextlib.ExitStack()
        self.nsem = 0
        nc = self.nc
        self.pe = Eng(self, "pe", nc.tensor)
        self.act = Eng(self, "act", nc.scalar)
        self.dve = Eng(self, "dve", nc.vector)
        self.pool = Eng(self, "pool", nc.gpsimd)
        self.sp = Eng(self, "sp", nc.sync)
        self.engs = [self.pe, self.act, self.dve, self.pool, self.sp]
        self.dma_evs = []
        self.rr = 0

    def new_sem(self, name):
        self.nsem += 1
        return self.es.enter_context(self.nc.semaphore(name + "_%d" % self.nsem))

    def R(self, name="r"):
        return Res(name)

    def Rs(self, n, name="r"):
        return [Res(name + str(i)) for i in range(n)]

    def _resolve(self, ev):
        if ev[0] == "d":
            return ev[1], ev[2]
        E, tick = ev[1], ev[2]
        i = bisect.bisect_left(E.sig_ticks, tick)
        assert i < len(E.sig_ticks), "unsignaled dependency on %s tick %d" % (E.name, tick)
        return E.sem, E.sig_vals[i]

    def _wait(self, E, ev, raw):
        if ev[0] == "e" and ev[1] is E and (not raw or E is self.pe):
            return
        sem, val = self._resolve(ev)
        key = id(sem)
        if E.known.get(key, 0) >= val:
            return
        E.h.wait_ge(sem, val)
        E.known[key] = val

    def _deps(self, E, rd, wr):
        for r in rd:
            if r.w is not None:
                self._wait(E, r.w, True)
        for w in wr:
            if w.w is not None:
                self._wait(E, w.w, False)
            for ev in w.r:
                self._wait(E, ev, False)

    def _record(self, ev, rd, wr):
        for w in wr:
            w.w = ev
            w.r = []
        for r in rd:
            if ev[0] == "e":
                r.r = [e for e in r.r if not (e[0] == "e" and e[1] is ev[1])]
            r.r.append(ev)

    def op(self, E, fn, rd=(), wr=(), sig=True):
        self._deps(E, rd, wr)
        inst = fn(E.h)
        E.tick += 1
        if sig:
            inst.then_inc(E.sem, 1)
            E.nsig += 1
            E.sig_ticks.append(E.tick)
            E.sig_vals.append(E.nsig)
        self._record(("e", E, E.tick), rd, wr)
        return inst

    def dma(self, Q, out, in_, rd=(), wr=(), **kw):
        self._deps(Q, rd, wr)
        prim = wr[0] if wr else rd[0]
        if prim.dsem is None:
            prim.dsem = self.new_sem("d_" + prim.name)
        prim.dcnt += 1
        Q.h.dma_start(out=out, in_=in_, **kw).then_inc(prim.dsem, 16)
        ev = ("d", prim.dsem, 16 * prim.dcnt)
        self._record(ev, rd, wr)
        self.dma_evs.append(ev)
        return ev

    def collective(self, fn, rd, wr):
        Q = self.pool
        self._deps(Q, rd, wr)
        prim = wr[0]
        if prim.dsem is None:
            prim.dsem = self.new_sem("c_" + prim.name)
        assert prim.dcnt == 0
        prim.dcnt = 1
        fn(Q.h).then_inc(prim.dsem, 1)
        ev = ("d", prim.dsem, 1)
        self._record(ev, rd, wr)
        self.dma_evs.append(ev)
        prim.dcnt = 0
        prim.dsem = None

    def barrier(self):
        evs = list(self.dma_evs)
        for E in self.engs:
            if E.nsig:
                evs.append(("e", E, E.sig_ticks[-1]))
        for E in self.engs:
            for ev in evs:
                if ev[0] == "e" and ev[1] is E:
                    continue
                self._wait(E, ev, True)
        self.dma_evs = []

    def dq(self):
        self.rr += 1
        return self.sp

    def mm(self, out, lhsT, rhs, start, stop, rd, wr, sig=None):
        if sig is None:
            sig = stop
        return self.op(self.pe, lambda e: e.matmul(out, lhsT, rhs, start=start, stop=stop), rd, wr, sig)

    def tr(self, out, in_, ident, rd, wr, sig=True):
        return self.op(self.pe, lambda e: e.transpose(out, in_, ident), rd, wr, sig)

    def actf(self, out, in_, func, rd, wr, bias=None, scale=None, E=None):
        kw = {}
        if bias is not None:
            kw["bias"] = bias
        if scale is not None:
            kw["scale"] = scale
        return self.op(E or self.act, lambda e: e.activation(out=out, in_=in_, func=func, **kw), rd, wr)

    def tt(self, out, in0, in1, op, rd, wr, E=None):
        return self.op(E or self.dve, lambda e: e.tensor_tensor(out=out, in0=in0, in1=in1, op=op), rd, wr)

    def ts(self, out, in0, s1, s2, op0, op1, rd, wr, E=None):
        if s2 is None:
            return self.op(E or self.dve, lambda e: e.tensor_scalar(out=out, in0=in0, scalar1=s1, scalar2=None, op0=op0), rd, wr)
        return self.op(E or self.dve, lambda e: e.tensor_scalar(out=out, in0=in0, scalar1=s1, scalar2=s2, op0=op0, op1=op1), rd, wr)

    def stt(self, out, in0, scalar, in1, op0, op1, rd, wr):
        return self.op(self.dve, lambda e: e.scalar_tensor_tensor(out=out, in0=in0, scalar=scalar, in1=in1, op0=op0, op1=op1), rd, wr)

    def cp(self, out, in_, rd, wr, E=None):
        E = E or self.dve
        if E is self.act:
            return self.op(E, lambda e: e.activation(out=out, in_=in_, func=AF.Copy), rd, wr)
        return self.op(E, lambda e: e.tensor_copy(out=out, in_=in_), rd, wr)


class Banks:
    def __init__(self, tens, res, idx):
        self.t = [tens[i] for i in idx]
        self.r = [res[i] for i in idx]
        self.i = 0

    def get(self):
        j = self.i % len(self.t)
        self.i += 1
        return self.t[j], self.r[j]


PP = {}
_off = 0
for _n, _s in [("ADA_B", 96), ("N1G", 16), ("N2G", 16), ("FNG", 8), ("LBL", 16), ("ONG", 1), ("PSC", 4),
               ("QNG", 2), ("KVNG", 1), ("CV", 16), ("FLAGL", 1), ("FLAGR", 1), ("SELF", 4), ("SELB", 4), ("SELT", 4), ("SELBT", 4),
               ("BR", 72), ("TABL", 32), ("TABR", 32), ("CTABL", 32), ("CTABR", 32)]:
    PP[_n] = _off
    _off += _s
NPP = _off

CC = {}
_off = 0
for _n, _s in [("IDENT", 128), ("ONES", 512), ("RESET", 512), ("MASKF", 128), ("MASKB", 128)]:
    CC[_n] = _off
    _off += _s
NCC = _off
POOL_W = (2, 4, 8, 16)


def _pool_inv_count(n, win, t):
    lo = np.clip(t - win // 2, 0, n)
    hi = np.clip(t + win - win // 2, 0, n)
    return 1.0 / (hi - lo).astype(np.float32)


def make_cc():
    cc = np.zeros((128, NCC), np.float32)
    cc[:, CC["IDENT"]:CC["IDENT"] + 128] = np.eye(128, dtype=np.float32)
    cc[:, CC["ONES"]:CC["ONES"] + 512] = 1.0
    r = np.ones(512, np.float32)
    r[::64] = 0.0
    cc[:, CC["RESET"]:CC["RESET"] + 512] = r[None, :]
    s = np.arange(128)[:, None]
    t = np.arange(128)[None, :]
    same = (s // 64) == (t // 64)
    mf = (same & (s <= t)).astype(np.float32)
    mb = (same & (s >= t)).astype(np.float32)
    cc[:, CC["MASKF"]:CC["MASKF"] + 128] = mf
    cc[:, CC["MASKB"]:CC["MASKB"] + 128] = mb
    return cc


def make_pp(inp, core):
    b, j = core // 4, core % 4
    pp = np.zeros((128, NPP), np.float32)

    def fm(v, nch):
        return np.ascontiguousarray(v.reshape(nch, 128).T)

    for l in range(2):
        pp[:, PP["ADA_B"] + l * 48:PP["ADA_B"] + (l + 1) * 48] = fm(inp["ada_b"][l], 48)
        pp[:, PP["N1G"] + l * 8:PP["N1G"] + (l + 1) * 8] = fm(inp["norm1_g"][l], 8)
        pp[:, PP["N2G"] + l * 8:PP["N2G"] + (l + 1) * 8] = fm(inp["norm2_g"][l], 8)
        br = np.concatenate([inp["moe_b_rg"][l], inp["moe_b_re"][l]])
        pp[:, PP["BR"] + l * 36:PP["BR"] + (l + 1) * 36] = br[None, :]
    pp[:, PP["FNG"]:PP["FNG"] + 8] = fm(inp["final_norm_g"], 8)
    lbl = inp["hgrn_lb_logits"]
    for d in range(2):
        for s in range(2):
            pp[:, PP["LBL"] + d * 8 + s * 4:PP["LBL"] + d * 8 + s * 4 + 4] = fm(lbl[d, s], 4)
    pp[:, PP["ONG"]] = inp["hgrn_onorm_g"][0]
    pp[:, PP["PSC"]:PP["PSC"] + 4] = fm(inp["pool_scale"][0], 4)
    pp[:, PP["QNG"]:PP["QNG"] + 2] = fm(inp["mla_q_norm_g"][0], 2)
    pp[:, PP["KVNG"]] = inp["mla_kv_norm_g"][0]
    cv = np.stack([fm(inp["c"][b], 8), fm(inp["c_ctx"], 8)], axis=2)
    pp[:, PP["CV"]:PP["CV"] + 16] = cv.reshape(128, 16)
    pp[:, PP["FLAGL"]] = 0.0 if j == 0 else 1.0
    pp[:, PP["FLAGR"]] = 0.0 if j == 3 else 1.0
    pp[:, PP["SELF"] + j] = 1.0
    pp[:, PP["SELB"] + j] = 1.0
    if j > 0:
        pp[:, PP["SELT"] + j - 1] = 1.0
    if j < 3:
        pp[:, PP["SELBT"] + j + 1] = 1.0
    n_lat, n_ctx = 8192, 256
    for gi, win in enumerate(POOL_W):
        t0 = j * NL + np.arange(8)
        t1 = j * NL + NL - 8 + np.arange(8)
        pp[:, PP["TABL"] + gi * 8:PP["TABL"] + gi * 8 + 8] = _pool_inv_count(n_lat, win, t0)[None, :]
        pp[:, PP["TABR"] + gi * 8:PP["TABR"] + gi * 8 + 8] = _pool_inv_count(n_lat, win, t1)[None, :]
        pp[:, PP["CTABL"] + gi * 8:PP["CTABL"] + gi * 8 + 8] = _pool_inv_count(n_ctx, win, np.arange(8))[None, :]
        pp[:, PP["CTABR"] + gi * 8:PP["CTABR"] + gi * 8 + 8] = _pool_inv_count(n_ctx, win, n_ctx - 8 + np.arange(8))[None, :]
    return pp


W_IN_COLS = {"q": 0, "ff": 512, "fb": 1024, "i": 1536, "g": 2048, "pool": 2560}


class Prog:
    def __init__(self, stage=99, debug=False, l1only=False):
        self.l1only = l1only
        self.stage = stage
        self.debug = debug
        self.k = K()
        self.nc = self.k.nc
        self.dbg_outs = []
        self.out_res = []
        self.build()

    def din(self, name, shape, dt=F32):
        return self.nc.dram_tensor(name, list(shape), dt, kind="ExternalInput").ap()

    def sb(self, es, name, shape, dt=F32):
        self._nsb = getattr(self, "_nsb", 0) + 1
        return es.enter_context(self.nc.sbuf_tensor("%s_%d" % (name, self._nsb), list(shape), dt))

    def dbg(self, name, ap, res, shape):
        if not self.debug:
            return
        k = self.k
        o = self.nc.dram_tensor("dbg_" + name, list(shape), F32, kind="ExternalOutput").ap()
        r = k.R("dbg_" + name)
        k.dma(k.pool, o, ap, rd=[res] if isinstance(res, Res) else list(res), wr=[r])
        self.out_res.append(r)
        self.dbg_outs.append("dbg_" + name)

    def ppc(self, name, off=0, n=1):
        c = PP[name] + off
        return self.pp[:, c:c + n]

    def ccc(self, name, n, off=0):
        c = CC[name] + off
        return self.cc[:, c:c + n]

    def build(self):
        k, nc = self.k, self.nc
        es = k.es
        self.x_own = self.din("x_own", [NL, D])
        self.x_halo = self.din("x_halo", [16, D])
        self.ctx_b = self.din("ctx_b", [NCX, D])
        self.pp_d = self.din("pp", [128, NPP])
        self.cc_d = self.din("cc", [128, NCC])
        self.ada_w = self.din("ada_w", [2, D, 6 * D])
        self.ab_w_in = self.din("ab_w_in", [D, 3072])
        self.ab_w_out = self.din("ab_w_out", [D, D])
        self.pool_w = self.din("pool_w", [4, 128, 128])
        self.moe_wr = self.din("moe_wr", [2, D, 36])
        self.moe_w_gate = self.din("moe_w_gate", [2, 32, D, 256])
        self.moe_w_up = self.din("moe_w_up", [2, 32, D, 256])
        self.moe_w_down = self.din("moe_w_down", [2, 32, 256, D])
        self.cd_w_in = self.din("cd_w_in", [D, 1952])
        self.cd_w_out = self.din("cd_w_out", [D, D])
        self.wkr_d = self.din("wkr", [D, 192])
        self.rope_d = self.din("rope", [32, 2, NL])
        self.nab_int = self.din("nab_int", [5, 128, 8, 128])
        self.nab_sp = self.din("nab_sp", [4, 6, 128, 8, 128])
        self.w_uq_d = self.din("w_uq", [256, 768])
        self.w_uqs_d = self.din("w_uqs", [256, 768])
        self.w_ukv_d = self.din("w_ukv", [128, 1024])
        self.y = nc.dram_tensor("y", [NL, D], F32, kind="ExternalOutput").ap()

        self.pp = self.sb(es, "pp_sb", [128, NPP])
        self.cc = self.sb(es, "cc_sb", [128, NCC])
        self.identb = self.sb(es, "identb", [128, 128], BF16)
        self.onesb = self.sb(es, "onesb", [128, 128], BF16)
        self.xT = self.sb(es, "xT", [128, KC, NL])
        self.xcT = self.sb(es, "xcT", [128, KC, NCX])
        self.mod = [self.sb(es, "mod%d" % l, [128, 48, 2]) for l in range(2)]
        self.gs1 = [self.sb(es, "gs1_%d" % l, [128, 8, 2]) for l in range(2)]
        self.gs2 = [self.sb(es, "gs2_%d" % l, [128, 8, 2]) for l in range(2)]
        self.lb = self.sb(es, "lb", [128, 8])
        self.oml = self.sb(es, "oml", [128, 8])
        self.r_pp, self.r_cc, self.r_const = k.R("pp"), k.R("cc"), k.R("const")
        self.r_mod = k.R("mod")
        self.r_xT = k.Rs(4, "xT")
        self.r_xcT = k.R("xcT")
        self.ps = [es.enter_context(nc.psum_tensor("ps%d" % i, [128, 512], F32)) for i in range(8)]
        self.r_ps = k.Rs(8, "ps")

        k.dma(k.sp, self.pp[:], self.pp_d[:, :], wr=[self.r_pp])
        k.dma(k.sp, self.cc[:], self.cc_d[:, :], wr=[self.r_cc])
        k.cp(self.identb[:], self.ccc("IDENT", 128), rd=[self.r_cc], wr=[self.r_const])
        k.cp(self.onesb[:], self.ccc("ONES", 128), rd=[self.r_cc], wr=[self.r_const])

        self.prologue()
        if self.l1only:
            self.blocks = [(0, NCX, True, -1)] + [(NCX + 512 * i, 512, False, i) for i in range(4)]
            with contextlib.ExitStack() as es0:
                srcs = [(self.ctx_b[:, :], NCX, lambda kc: self.xcT[:, kc, :], self.r_xcT)]
                for i in range(4):
                    srcs.append((self.x_own[i * 512:(i + 1) * 512, :], 512,
                                 (lambda kc, i=i: self.xT[:, kc, i * 512:(i + 1) * 512]), self.r_xT[i]))
                self.load_T(es0, srcs)
        if self.stage >= 1 and not self.l1only:
            self.layer0()
        if self.stage >= 5 and not self.l1only:
            self.moe(0)
        if self.stage == 5 or self.stage == 6:
            self.dbg("x1", self.xT[:, 0, :], self.r_xT, [128, NL])
            self.dbg("xc1", self.xcT[:, 0, :], self.r_xcT, [128, NCX])
        if self.stage >= 7:
            self.layer1()
        if self.stage in (8, 9):
            self.dbg("xmix1", self.xT[:, 0, :], self.r_xT, [128, NL])
        if self.stage >= 10:
            self.moe(1)
            self.final_out()
        self.finish()

    def finish(self):
        k = self.k
        for r in self.out_res:
            if r.w is not None:
                k._wait(k.sp, r.w, True)
        k.es.close()

    def prologue(self):
        k = self.k
        with contextlib.ExitStack() as es:
            sc = self.sb(es, "silu_c", [128, 16])
            r_sc = k.R("sc")
            k.actf(sc[:], self.ppc("CV", 0, 16), AF.Silu, rd=[self.r_pp], wr=[r_sc])
            NB = 3
            wb = [self.sb(es, "adaw%d" % i, [128, KC, 512]) for i in range(NB)]
            r_wb = k.Rs(NB, "adaw")
            it = 0
            for l in range(2):
                pm, r_pm = self.ps[l], self.r_ps[l]
                for nb in range(12):
                    b = it % NB
                    it += 1
                    src = self.ada_w[l, :, nb * 512:(nb + 1) * 512].rearrange("(kc p) n -> p kc n", p=128)
                    k.dma(k.sp, wb[b][:], src, wr=[r_wb[b]])
                    for c4 in range(4):
                        col = (nb * 4 + c4) * 2
                        for kc in range(KC):
                            k.mm(pm[:, col:col + 2], wb[b][:, kc, c4 * 128:(c4 + 1) * 128], sc[:, kc * 2:kc * 2 + 2],
                                 start=(kc == 0), stop=(kc == KC - 1), rd=[r_wb[b], r_sc], wr=[r_pm])
                mod = self.mod[l]
                k.tt(mod[:], pm[:, 0:96].rearrange("p (n v) -> p n v", v=2),
                     self.ppc("ADA_B", l * 48, 48).unsqueeze(2).to_broadcast([128, 48, 2]), ALU.add,
                     rd=[r_pm, self.r_pp], wr=[self.r_mod])
                for (gs, i_sc, gname) in ((self.gs1[l], 1, "N1G"), (self.gs2[l], 4, "N2G")):
                    k.ts(gs[:], mod[:, i_sc * 8:(i_sc + 1) * 8, :], 1.0, None, ALU.add, None, rd=[self.r_mod], wr=[self.r_mod])
                    k.tt(gs[:], gs[:], self.ppc(gname, l * 8, 8).unsqueeze(2).to_broadcast([128, 8, 2]), ALU.mult,
                         rd=[self.r_mod, self.r_pp], wr=[self.r_mod])
            lbl = self.ppc("LBL", 0, 16).rearrange("p (d s h) -> p d s h", d=2, s=2)
            lbv = self.lb[:].rearrange("p (d h) -> p d h", d=2)
            k.tt(lbv, lbl[:, :, 0, :], lbl[:, :, 1, :], ALU.subtract, rd=[self.r_pp], wr=[self.r_mod])
            k.actf(self.lb[:], self.lb[:], AF.Sigmoid, rd=[self.r_mod], wr=[self.r_mod])
            k.ts(self.oml[:], self.lb[:], -1.0, 1.0, ALU.mult, ALU.add, rd=[self.r_mod], wr=[self.r_mod])
            self.dbg("mod0", self.mod[0][:].rearrange("p n v -> p (n v)"), self.r_mod, [128, 96])
            self.dbg("mod1", self.mod[1][:].rearrange("p n v -> p (n v)"), self.r_mod, [128, 96])
            self.dbg("lb", self.lb[:], self.r_mod, [128, 8])
            k.barrier()

    def modv(self, l, i, kc, v):
        return self.mod[l][:, i * 8 + kc, v:v + 1]

    def load_T(self, es_outer, srcs):
        k = self.k
        with contextlib.ExitStack() as es:
            xs = [self.sb(es, "xstage%d" % i, [128, 4, D]) for i in range(2)]
            r_xs = k.Rs(2, "xstage")
            banks = Banks(self.ps, self.r_ps, [0, 1, 2, 3])
            for it, (src, ntok, dst_fn, dst_res) in enumerate(srcs):
                b = it % 2
                if ntok >= 128:
                    nt = ntok // 128
                    k.dma(k.sp, xs[b][:, 0:nt, :], src.rearrange("(t p) f -> p t f", p=128), wr=[r_xs[b]])
                    pt = 128
                else:
                    nt = 1
                    pt = ntok
                    k.dma(k.sp, xs[b][0:ntok, 0, :], src, wr=[r_xs[b]])
                for kc in range(KC):
                    pb, r_pb = banks.get()
                    for t in range(nt):
                        k.tr(pb[:, t * pt:(t + 1) * pt], xs[b][0:pt, t, kc * 128:(kc + 1) * 128],
                             self.ccc("IDENT", pt)[0:pt, :], rd=[r_xs[b], self.r_cc], wr=[r_pb], sig=(t == nt - 1))
                    k.cp(dst_fn(kc), pb[:, 0:nt * pt], rd=[r_pb], wr=[dst_res], E=(k.act if kc % 2 else k.dve))
            k.barrier()

    def norm_mod(self, tmp, src_fn, n, gs_fn, sh_fn, out_fn, rd, wr, f32_out_fn=None):
        k = self.k
        pb, r_pb = tmp["banks"].get()
        for kc in range(KC):
            i = tmp["i"] % 3
            tmp["i"] += 1
            sq, r_sq = tmp["sq"][i], tmp["r_sq"][i]
            k.actf(sq[:, 0:n], src_fn(kc), AF.Square, rd=rd, wr=[r_sq])
            k.mm(pb[:, 0:n], self.ccc("ONES", 128), sq[:, 0:n], start=(kc == 0), stop=(kc == KC - 1),
                 rd=[r_sq, self.r_cc], wr=[r_pb], sig=True)
        rstd, r_rstd = tmp["rstd"], tmp["r_rstd"]
        k.ts(rstd[:, 0:n], pb[:, 0:n], 1.0 / D, EPS, ALU.mult, ALU.add, rd=[r_pb], wr=[r_rstd])
        k.actf(rstd[:, 0:n], rstd[:, 0:n], AF.Sqrt, rd=[r_rstd], wr=[r_rstd])
        k.op(k.dve, lambda e: e.reciprocal(out=rstd[:, 0:n], in_=rstd[:, 0:n]), rd=[r_rstd], wr=[r_rstd])
        for kc in range(KC):
            i = tmp["i"] % 3
            tmp["i"] += 1
            sq, r_sq = tmp["sq"][i], tmp["r_sq"][i]
            k.stt(sq[:, 0:n], src_fn(kc), gs_fn(kc), rstd[:, 0:n], ALU.mult, ALU.mult,
                  rd=list(rd) + [r_rstd, self.r_mod], wr=[r_sq])
            k.actf(out_fn(kc), sq[:, 0:n], AF.Identity, rd=[r_sq, self.r_mod], wr=wr, bias=sh_fn(kc))
            if f32_out_fn is not None:
                k.ts(f32_out_fn(kc), sq[:, 0:n], sh_fn(kc), None, ALU.add, None, rd=[r_sq, self.r_mod], wr=wr)

    def norm_tmp(self, es, banks_idx=(4, 5)):
        k = self.k
        return {"banks": Banks(self.ps, self.r_ps, list(banks_idx)), "i": 0,
                "sq": [self.sb(es, "nsq%d" % i, [128, 512]) for i in range(3)], "r_sq": k.Rs(3, "nsq"),
                "rstd": self.sb(es, "nrstd", [128, 512]), "r_rstd": k.R("nrstd")}

    def load_w(self, dst, src, res):
        self.k.dma(self.k.pool, dst, src, wr=[res])

    def layer0(self):
        k = self.k
        with contextlib.ExitStack() as es:
            self.hT = self.sb(es, "hT", [128, KC, NT], BF16)
            self.hh = self.sb(es, "hh", [128, KC, 16], BF16)
            self.r_hT = k.Rs(5, "hT")
            self.r_hh = k.R("hh")
            xh = self.sb(es, "xh", [128, KC, 16])
            r_xh = k.R("xh")
            self.blocks = [(0, NCX, True, -1)] + [(NCX + 512 * i, 512, False, i) for i in range(4)]
            srcs = [(self.ctx_b[:, :], NCX, lambda kc: self.xcT[:, kc, :], self.r_xcT),
                    (self.x_halo[:, :], 16, lambda kc: xh[:, kc, :], r_xh)]
            for i in range(4):
                srcs.append((self.x_own[i * 512:(i + 1) * 512, :], 512,
                             (lambda kc, i=i: self.xT[:, kc, i * 512:(i + 1) * 512]), self.r_xT[i]))
            self.load_T(es, srcs)
            with contextlib.ExitStack() as es2:
                tmp = self.norm_tmp(es2)
                for bi, (c0, n, is_ctx, li) in enumerate(self.blocks):
                    v = 1 if is_ctx else 0
                    src_fn = (lambda kc: self.xcT[:, kc, :]) if is_ctx else (lambda kc, li=li: self.xT[:, kc, li * 512:(li + 1) * 512])
                    self.norm_mod(tmp, src_fn, n, lambda kc, v=v: self.gs1[0][:, kc, v:v + 1],
                                  lambda kc, v=v: self.modv(0, 0, kc, v),
                                  lambda kc, c0=c0, n=n: self.hT[:, kc, c0:c0 + n],
                                  rd=[self.r_xcT if is_ctx else self.r_xT[li]], wr=[self.r_hT[bi]])
                self.norm_mod(tmp, lambda kc: xh[:, kc, :], 16, lambda kc: self.gs1[0][:, kc, 0:1],
                              lambda kc: self.modv(0, 0, kc, 0), lambda kc: self.hh[:, kc, :],
                              rd=[r_xh], wr=[self.r_hh])
                k.barrier()
            self.dbg("hT", self.hT[:, 0, :], self.r_hT, [128, NT])
            if self.stage >= 2:
                self.pool_pass()
            if self.stage >= 3:
                self.hgrn()

    def x_dst(self, is_ctx, li, m, n):
        if is_ctx:
            return self.xcT[:, m, 0:n], self.r_xcT, 1
        return self.xT[:, m, li * 512:li * 512 + n], self.r_xT[li], 0

    def pool_pass(self):
        k = self.k
        with contextlib.ExitStack() as es:
            wp = self.sb(es, "w_pool", [128, KC, 512], BF16)
            wo = self.sb(es, "w_out2", [128, 4, D], BF16)
            pw = self.sb(es, "poolw", [128, 4, 128], BF16)
            r_w = k.R("poolwts")
            self.load_w(wp[:], self.ab_w_in[:, 2560:3072].rearrange("(kc p) n -> p kc n", p=128), r_w)
            self.load_w(wo[:], self.ab_w_out[512:1024, :].rearrange("(kc p) n -> p kc n", p=128), r_w)
            self.load_w(pw[:], self.pool_w.rearrange("g c d -> c g d"), r_w)
            W = 528
            up = self.sb(es, "up", [128, 4, W])
            A = self.sb(es, "pA", [128, 4, W])
            B = self.sb(es, "pB", [128, 4, W])
            S16 = self.sb(es, "pS16", [128, 512])
            t8 = self.sb(es, "pt8", [128, 8])
            diff = self.sb(es, "pdiff", [128, 4, 512], BF16)
            pout = self.sb(es, "ppout", [128, 4, 512], BF16)
            r_up, r_A, r_B, r_S, r_t8, r_diff, r_pout = (k.R(s) for s in ("up", "pA", "pB", "pS", "pt8", "pdiff", "ppout"))
            banks = Banks(self.ps, self.r_ps, [0, 1, 2, 3, 4, 5, 6, 7])
            for bi, (c0, n, is_ctx, li) in enumerate(self.blocks):
                Wb = n + 16
                for gi in range(4):
                    pb, r_pb = banks.get()
                    for kc in range(KC):
                        k.mm(pb[:, 0:n], wp[:, kc, gi * 128:(gi + 1) * 128], self.hT[:, kc, c0:c0 + n],
                             start=(kc == 0), stop=(kc == KC - 1), rd=[r_w, self.r_hT[bi]], wr=[r_pb])
                    k.cp(up[:, gi, 8:8 + n], pb[:, 0:n], rd=[r_pb], wr=[r_up], E=k.act)
                if is_ctx:
                    k.op(k.pool, lambda e: e.memset(up[:, :, 0:8], 0.0), wr=[r_up])
                    k.op(k.pool, lambda e: e.memset(up[:, :, 8 + n:16 + n], 0.0), wr=[r_up])
                else:
                    ph, r_ph = banks.get()
                    for gi in range(4):
                        for side in range(2):
                            if side == 0:
                                src_fn = (lambda kc: self.hh[:, kc, 0:8]) if li == 0 else (lambda kc: self.hT[:, kc, c0 - 8:c0])
                                rr = self.r_hh if li == 0 else self.r_hT[bi - 1]
                            else:
                                src_fn = (lambda kc: self.hh[:, kc, 8:16]) if li == 3 else (lambda kc: self.hT[:, kc, c0 + n:c0 + n + 8])
                                rr = self.r_hh if li == 3 else self.r_hT[bi + 1]
                            col = (gi * 2 + side) * 8
                            for kc in range(KC):
                                k.mm(ph[:, col:col + 8], wp[:, kc, gi * 128:(gi + 1) * 128], src_fn(kc),
                                     start=(kc == 0), stop=(kc == KC - 1), rd=[r_w, rr], wr=[r_ph])
                    phv = ph[:, 0:64].rearrange("p (g s c) -> p g s c", g=4, s=2)
                    if li == 0:
                        k.ts(up[:, :, 0:8], phv[:, :, 0, :], self.ppc("FLAGL"), None, ALU.mult, None, rd=[r_ph, self.r_pp], wr=[r_up])
                    else:
                        k.cp(up[:, :, 0:8], phv[:, :, 0, :], rd=[r_ph], wr=[r_up])
                    if li == 3:
                        k.ts(up[:, :, 8 + n:16 + n], phv[:, :, 1, :], self.ppc("FLAGR"), None, ALU.mult, None, rd=[r_ph, self.r_pp], wr=[r_up])
                    else:
                        k.cp(up[:, :, 8 + n:16 + n], phv[:, :, 1, :], rd=[r_ph], wr=[r_up])
                k.tt(A[:, :, 1:Wb], up[:, :, 0:Wb - 1], up[:, :, 1:Wb], ALU.add, rd=[r_up], wr=[r_A])
                k.tt(B[:, 1:4, 2:Wb - 2], A[:, 1:4, 1:Wb - 3], A[:, 1:4, 3:Wb - 1], ALU.add, rd=[r_A], wr=[r_B])
                k.tt(A[:, 2:4, 4:Wb - 4], B[:, 2:4, 2:Wb - 6], B[:, 2:4, 6:Wb - 2], ALU.add, rd=[r_B], wr=[r_A])
                k.tt(S16[:, 0:n], A[:, 3, 4:4 + n], A[:, 3, 12:12 + n], ALU.add, rd=[r_A], wr=[r_S])
                sums = [A[:, 0, 8:8 + n], B[:, 1, 8:8 + n], A[:, 2, 8:8 + n], S16[:, 0:n]]
                for gi, win in enumerate(POOL_W):
                    k.stt(diff[:, gi, 0:n], sums[gi], 1.0 / win, up[:, gi, 8:8 + n], ALU.mult, ALU.subtract,
                          rd=[r_A, r_B, r_S, r_up], wr=[r_diff])
                edges = []
                if is_ctx:
                    edges = [(0, "CTABL"), (n - 8, "CTABR")]
                elif li == 0:
                    edges = [(0, "TABL")]
                elif li == 3:
                    edges = [(n - 8, "TABR")]
                for (e0, tab) in edges:
                    for gi in range(4):
                        k.tt(t8[:], sums[gi][:, e0:e0 + 8], self.ppc(tab, gi * 8, 8), ALU.mult, rd=[r_A, r_B, r_S, self.r_pp], wr=[r_t8])
                        k.tt(diff[:, gi, e0:e0 + 8], t8[:], up[:, gi, 8 + e0:16 + e0], ALU.subtract, rd=[r_t8, r_up], wr=[r_diff])
                for gi in range(4):
                    pb, r_pb = banks.get()
                    k.mm(pb[:, 0:n], pw[:, gi, :], diff[:, gi, 0:n], start=True, stop=True, rd=[r_w, r_diff], wr=[r_pb])
                    k.actf(pout[:, gi, 0:n], pb[:, 0:n], AF.Copy, rd=[r_pb, self.r_pp], wr=[r_pout], scale=self.ppc("PSC", gi))
                for m in range(KC):
                    pb, r_pb = banks.get()
                    for gi in range(4):
                        k.mm(pb[:, 0:n], wo[:, gi, m * 128:(m + 1) * 128], pout[:, gi, 0:n], start=(gi == 0), stop=(gi == 3),
                             rd=[r_w, r_pout], wr=[r_pb])
                    dst, r_dst, v = self.x_dst(is_ctx, li, m, n)
                    k.stt(dst, pb[:, 0:n], self.modv(0, 2, m, v), dst, ALU.mult, ALU.add, rd=[r_pb, self.r_mod, r_dst], wr=[r_dst])
            if self.stage == 2:
                self.dbg("pout", pout[:, :, :].rearrange("p g n -> p (g n)"), r_pout, [128, 2048])
            k.barrier()

    def hgrn(self):
        k = self.k
        with contextlib.ExitStack() as es:
            self.osum = self.sb(es, "osum", [128, 4, NT], BF16)
            self.r_osum = k.Rs(5, "osum")
            S32 = self.sb(es, "S32", [128, 8, 128])
            Sb = self.sb(es, "Sb", [128, 8, 128], BF16)
            Sd = self.sb(es, "Sd", [128, 4, 128])
            Sfin = self.sb(es, "Sfin", [128, 8, 128])
            xb = self.sb(es, "xb", [128, 8, 129])
            r_S, r_Sb, r_Sd = k.Rs(8, "S32"), k.Rs(8, "Sb"), k.Rs(8, "Sd")
            r_Sfin, r_xb = k.R("Sfin"), k.R("xb")
            wA = self.sb(es, "wA", [128, KC, 512], BF16)
            wB = self.sb(es, "wB", [128, KC, 512], BF16)
            wC = self.sb(es, "wC", [128, KC, 512], BF16)
            r_wA, r_wB, r_wC = k.R("wA"), k.R("wB"), k.R("wC")

            def wsrc(name):
                c = W_IN_COLS[name]
                return self.ab_w_in[:, c:c + 512].rearrange("(kc p) n -> p kc n", p=128)

            with contextlib.ExitStack() as es2:
                T = [self.sb(es2, "hT%d" % i, [128, 512]) for i in range(4)]
                r_T = k.Rs(4, "hTt")
                Qt = [self.sb(es2, "Qt%d" % i, [128, 512], BF16) for i in range(4)]
                Kt = [self.sb(es2, "Kt%d" % i, [128, 512], BF16) for i in range(4)]
                Ktok = [self.sb(es2, "Ktok%d" % i, [128, 4, 128], BF16) for i in range(4)]
                attm = [self.sb(es2, "attm%d" % i, [128, 512], BF16) for i in range(4)]
                dch = [self.sb(es2, "dch%d" % i, [128, 8]) for i in range(4)]
                r_Qt, r_Kt, r_Ktok, r_attm, r_dch = (k.Rs(4, s_) for s_ in ("Qt", "Kt", "Ktok", "attm", "dch"))
                Vtok = self.sb(es2, "Vtok", [128, 4, 512], BF16)
                r_V = k.R("Vtok")
                dend = self.sb(es2, "dend", [128, 1])
                r_dend = k.R("dend")
                gb = Banks(self.ps, self.r_ps, [6, 7])

                def proj_v(bi, c0, n):
                    for t in range(n // 128):
                        pv, r_pv = gb.get()
                        for kc in range(KC):
                            k.mm(pv[:, :], self.hT[:, kc, c0 + t * 128:c0 + (t + 1) * 128], wC[:, kc, :],
                                 start=(kc == 0), stop=(kc == KC - 1), rd=[self.r_hT[bi], r_wC], wr=[r_pv])
                        k.cp(Vtok[:, t, :], pv[:, :], rd=[r_pv], wr=[r_V], E=k.act)

                def gates(bi, c0, n, dirn, h, reset):
                    hd = dirn * 4 + h
                    pf, r_pf = gb.get()
                    for kc in range(KC):
                        k.mm(pf[:, 0:n], wB[:, kc, h * 128:(h + 1) * 128], self.hT[:, kc, c0:c0 + n],
                             start=(kc == 0), stop=(kc == KC - 1), rd=[self.r_hT[bi], r_wB], wr=[r_pf])
                    k.actf(T[1][:, 0:n], pf[:, 0:n], AF.Sigmoid, rd=[r_pf], wr=[r_T[1]])
                    k.ts(T[1][:, 0:n], T[1][:, 0:n], self.oml[:, hd:hd + 1], self.lb[:, hd:hd + 1], ALU.mult, ALU.add,
                         rd=[r_T[1], self.r_mod], wr=[r_T[1]])
                    k.actf(T[2][:, 0:n], T[1][:, 0:n], AF.Ln, rd=[r_T[1]], wr=[r_T[2]])
                    k.ts(T[1][:, 0:n], T[1][:, 0:n], -1.0, 1.0, ALU.mult, ALU.add, rd=[r_T[1], r_T[2]], wr=[r_T[1]])
                    msk = self.ccc("RESET" if reset else "ONES", n)
                    k.op(k.dve, lambda e: e.tensor_tensor_scan(out=T[3][:, 0:n], data0=msk, data1=T[2][:, 0:n], initial=0.0,
                                                               op0=ALU.mult, op1=ALU.add), rd=[r_T[2], self.r_cc], wr=[r_T[3]])

                def kt_transposes(h, n):
                    pt, r_pt = gb.get()
                    ptb = pt[:, :].bitcast(BF16)
                    nt = n // 128
                    for t in range(nt):
                        k.tr(ptb[:, t * 128:(t + 1) * 128], Kt[h][:, t * 128:(t + 1) * 128], self.identb[:],
                             rd=[r_Kt[h], self.r_const], wr=[r_pt], sig=(t == nt - 1))
                    k.cp(Ktok[h][:, 0:nt, :], ptb[:, 0:nt * 128].rearrange("p (t c) -> p t c", c=128), rd=[r_pt], wr=[r_Ktok[h]], E=k.act)

                def main_block(bi, dirn, first):
                    c0, n, is_ctx, li = self.blocks[bi]
                    nch = n // 64
                    proj_v(bi, c0, n)
                    for h in range(4):
                        hd = dirn * 4 + h
                        pq, r_pq = gb.get()
                        for kc in range(KC):
                            k.mm(pq[:, 0:n], wA[:, kc, h * 128:(h + 1) * 128], self.hT[:, kc, c0:c0 + n],
                                 start=(kc == 0), stop=(kc == KC - 1), rd=[self.r_hT[bi], r_wA], wr=[r_pq])
                        k.actf(T[0][:, 0:n], pq[:, 0:n], AF.Silu, rd=[r_pq], wr=[r_T[0]])
                        gates(bi, c0, n, dirn, h, True)
                        tot = T[3][:, 0:n].rearrange("p (c t) -> p c t", t=64)[:, :, 63]
                        k.actf(dch[h][:, 0:nch], tot, AF.Exp, rd=[r_T[3]], wr=[r_dch[h]])
                        if dirn == 1:
                            k.tt(T[3][:, 0:n], T[3][:, 0:n], T[2][:, 0:n], ALU.subtract, rd=[r_T[3], r_T[2], r_dch[h]], wr=[r_T[3]])
                        sg1 = 1.0 if dirn == 0 else -1.0
                        k.actf(T[2][:, 0:n], T[3][:, 0:n], AF.Exp, rd=[r_T[3]], wr=[r_T[2]], scale=sg1)
                        k.tt(Qt[h][:, 0:n], T[0][:, 0:n], T[2][:, 0:n], ALU.mult, rd=[r_T[0], r_T[2]], wr=[r_Qt[h]])
                        k.actf(T[2][:, 0:n], T[3][:, 0:n], AF.Exp, rd=[r_T[3], r_Qt[h]], wr=[r_T[2]], scale=-sg1)
                        k.tt(Kt[h][:, 0:n], T[1][:, 0:n], T[2][:, 0:n], ALU.mult, rd=[r_T[1], r_T[2]], wr=[r_Kt[h]])
                        kt_transposes(h, n)
                        pa, r_pa = gb.get()
                        for t in range(n // 128):
                            sl = slice(t * 128, (t + 1) * 128)
                            k.mm(pa[:, sl], Kt[h][:, sl], Qt[h][:, sl], start=True, stop=True, rd=[r_Kt[h], r_Qt[h]], wr=[r_pa],
                                 sig=(t == n // 128 - 1))
                        mk = self.ccc("MASKF" if dirn == 0 else "MASKB", 128).unsqueeze(1).to_broadcast([128, n // 128, 128])
                        k.tt(attm[h][:, 0:n].rearrange("p (t c) -> p t c", c=128), pa[:, 0:n].rearrange("p (t c) -> p t c", c=128), mk, ALU.mult,
                             rd=[r_pa, self.r_cc], wr=[r_attm[h]])
                    order = list(range(nch)) if dirn == 0 else list(range(nch - 1, -1, -1))
                    pd_banks = Banks(self.ps, self.r_ps, [4, 5])
                    for ci in order:
                        t, p0 = ci // 2, (ci % 2) * 64
                        cs = slice(ci * 64, (ci + 1) * 64)
                        pd, r_pd = pd_banks.get()
                        for h in range(4):
                            hd = dirn * 4 + h
                            po, r_po = self.ps[h], self.r_ps[h]
                            if dirn == 1:
                                k.actf(Sb[:, hd, :], S32[:, hd, :], AF.Copy, rd=[r_S[hd], r_dch[h]], wr=[r_Sb[hd]], scale=dch[h][:, ci:ci + 1])
                                k.actf(S32[:, hd, :], S32[:, hd, :], AF.Copy, rd=[r_S[hd], r_dch[h]], wr=[r_S[hd]], scale=dch[h][:, ci:ci + 1])
                            k.mm(po[:, cs], Sb[:, hd, :], Qt[h][:, cs], start=True, stop=False, rd=[r_Sb[hd], r_Qt[h]], wr=[r_po])
                            k.mm(po[:, cs], Vtok[p0:p0 + 64, t, h * 128:(h + 1) * 128], attm[h][p0:p0 + 64, cs], start=False, stop=True,
                                 rd=[r_V, r_attm[h]], wr=[r_po])
                            k.mm(pd[:, h * 128:(h + 1) * 128], Ktok[h][p0:p0 + 64, t, :], Vtok[p0:p0 + 64, t, h * 128:(h + 1) * 128],
                                 start=True, stop=True, rd=[r_Ktok[h], r_V], wr=[r_pd])
                            if dirn == 0:
                                k.actf(Sd[:, h, :], S32[:, hd, :], AF.Copy, rd=[r_S[hd], r_dch[h]], wr=[r_Sd[hd]], scale=dch[h][:, ci:ci + 1])
                                k.stt(Sb[:, hd, :], pd[:, h * 128:(h + 1) * 128], dch[h][:, ci:ci + 1], Sd[:, h, :], ALU.mult, ALU.add,
                                      rd=[r_pd, r_dch[h], r_Sd[hd]], wr=[r_Sb[hd]])
                                k.stt(S32[:, hd, :], pd[:, h * 128:(h + 1) * 128], dch[h][:, ci:ci + 1], Sd[:, h, :], ALU.mult, ALU.add,
                                      rd=[r_pd, r_dch[h], r_Sd[hd]], wr=[r_S[hd]])
                            else:
                                k.tt(S32[:, hd, :], S32[:, hd, :], pd[:, h * 128:(h + 1) * 128], ALU.add, rd=[r_S[hd], r_pd], wr=[r_S[hd]])
                    for h in range(4):
                        po, r_po = self.ps[h], self.r_ps[h]
                        dst = self.osum[:, h, c0:c0 + n]
                        if first:
                            k.cp(dst, po[:, 0:n], rd=[r_po], wr=[self.r_osum[bi]], E=k.act)
                        else:
                            k.tt(dst, po[:, 0:n], dst, ALU.add, rd=[r_po, self.r_osum[bi]], wr=[self.r_osum[bi]])

                def zero_states():
                    k.op(k.pool, lambda e: e.memset(S32[:], 0.0), wr=r_S)
                    k.op(k.pool, lambda e: e.memset(Sb[:], 0.0), wr=r_Sb)

                self.load_w(wA[:], wsrc("q"), r_wA)
                self.load_w(wB[:], wsrc("ff"), r_wB)
                self.load_w(wC[:], wsrc("i"), r_wC)
                zero_states()
                main_block(0, 0, True)
                k.cp(Sfin[:, 0:4, :], S32[:, 0:4, :], rd=r_S[0:4], wr=[r_Sfin], E=k.pool)
                k.op(k.pool, lambda e: e.memset(xb[:], 0.0), wr=[r_xb])
                k.op(k.pool, lambda e: e.memset(xb[:, :, 128:129], 1.0), wr=[r_xb])

                def prepass(dirn):
                    for bi in range(1, 5):
                        c0, n, is_ctx, li = self.blocks[bi]
                        proj_v(bi, c0, n)
                        for h in range(4):
                            hd = dirn * 4 + h
                            gates(bi, c0, n, dirn, h, False)
                            k.actf(dend[:], T[3][:, n - 1:n], AF.Exp, rd=[r_T[3]], wr=[r_dend])
                            if dirn == 0:
                                k.actf(T[2][:, 0:n], T[3][:, 0:n], AF.Exp, rd=[r_T[3]], wr=[r_T[2]], scale=-1.0, bias=T[3][:, n - 1:n])
                            else:
                                k.tt(T[3][:, 0:n], T[3][:, 0:n], T[2][:, 0:n], ALU.subtract, rd=[r_T[3], r_T[2], r_dend], wr=[r_T[3]])
                                k.actf(T[2][:, 0:n], T[3][:, 0:n], AF.Exp, rd=[r_T[3]], wr=[r_T[2]])
                            k.tt(Kt[h][:, 0:n], T[1][:, 0:n], T[2][:, 0:n], ALU.mult, rd=[r_T[1], r_T[2]], wr=[r_Kt[h]])
                            kt_transposes(h, n)
                            pl, r_pl = gb.get()
                            for t in range(4):
                                k.mm(pl[:, 0:128], Ktok[h][:, t, :], Vtok[:, t, h * 128:(h + 1) * 128], start=(t == 0), stop=(t == 3),
                                     rd=[r_Ktok[h], r_V], wr=[r_pl])
                            Ld, Dd = xb[:, hd, 0:128], xb[:, hd, 128:129]
                            if dirn == 0:
                                k.stt(Ld, Ld, dend[:, 0:1], pl[:, 0:128], ALU.mult, ALU.add, rd=[r_xb, r_dend, r_pl], wr=[r_xb])
                            else:
                                k.stt(Ld, pl[:, 0:128], Dd, Ld, ALU.mult, ALU.add, rd=[r_xb, r_pl], wr=[r_xb])
                            k.tt(Dd, Dd, dend[:, 0:1], ALU.mult, rd=[r_xb, r_dend], wr=[r_xb])

                prepass(0)
                self.load_w(wB[:], wsrc("fb"), r_wB)
                zero_states()
                main_block(0, 1, False)
                k.cp(Sfin[:, 4:8, :], S32[:, 4:8, :], rd=r_S[4:8], wr=[r_Sfin], E=k.pool)
                prepass(1)
                if self.stage == 3:
                    self.dbg("Sfin", Sfin[:].rearrange("p a b -> p (a b)"), r_Sfin, [128, 1024])
                    self.dbg("xb", xb[:].rearrange("p a b -> p (a b)"), r_xb, [128, 8 * 129])
                    self.dbg("osum_ctx", self.osum[:, :, 0:NCX], self.r_osum[0], [128, 4, NCX])
                xin = self.nc.dram_tensor("hg_xin", [128, 8 * 129], F32, kind="Internal").ap()
                xout = self.nc.dram_tensor("hg_xout", [4 * 128, 8 * 129], F32, kind="Internal").ap()
                r_xin, r_xout = k.R("xin"), k.R("xout")
                k.dma(k.pool, xin[:, :], xb[:].rearrange("p a b -> p (a b)"), rd=[r_xb], wr=[r_xin])
                k.collective(lambda g: g.collective_compute("AllGather", ALU.bypass, replica_groups=[[0, 1, 2, 3], [4, 5, 6, 7]],
                                                            ins=[xin[:, :]], outs=[xout[:, :]]), rd=[r_xin], wr=[r_xout])
                xgs = [self.sb(es2, "xg%d" % i, [128, 4, 129]) for i in range(2)]
                r_xgs = k.Rs(2, "xg")
                xov = xout.rearrange("(r p) (a b) -> p r a b", p=128, b=129)
                Ct = T[0][:, 0:128]
                r_C = r_T[0]
                for hd in range(8):
                    fwd = hd < 4
                    sel = "SELF" if fwd else "SELB"
                    ranks = [0, 1, 2, 3] if fwd else [3, 2, 1, 0]
                    xg, r_xg = xgs[hd % 2], r_xgs[hd % 2]
                    k.dma(k.sp, xg[:], xov[:, :, hd, :], rd=[r_xout], wr=[r_xg])
                    k.cp(Ct, Sfin[:, hd, :], rd=[r_Sfin], wr=[r_C])
                    k.ts(S32[:, hd, :], Ct, self.ppc(sel, ranks[0]), None, ALU.mult, None, rd=[r_C, self.r_pp], wr=[r_S[hd]])
                    for i in range(3):
                        r = ranks[i]
                        k.stt(Ct, Ct, xg[:, r, 128:129], xg[:, r, 0:128], ALU.mult, ALU.add, rd=[r_C, r_xg], wr=[r_C])
                        k.stt(S32[:, hd, :], Ct, self.ppc(sel, ranks[i + 1]), S32[:, hd, :], ALU.mult, ALU.add,
                              rd=[r_C, self.r_pp, r_S[hd]], wr=[r_S[hd]])
                    k.cp(Sb[:, hd, :], S32[:, hd, :], rd=[r_S[hd]], wr=[r_Sb[hd]], E=k.pool)
                if self.stage == 3:
                    self.dbg("Sin", S32[:].rearrange("p a b -> p (a b)"), r_S, [128, 1024])
                if self.stage >= 4:
                    for bi in (4, 3, 2, 1):
                        main_block(bi, 1, True)
                    self.load_w(wB[:], wsrc("ff"), r_wB)
                    for bi in (1, 2, 3, 4):
                        main_block(bi, 0, False)
                k.barrier()
            if self.stage >= 4:
                self.hgrn_readout(es, wA, r_wA)

    def hgrn_readout(self, es, wA, r_wA):
        k = self.k
        with contextlib.ExitStack() as es2:
            wo1 = self.sb(es2, "w_out1", [128, 4, D], BF16)
            r_wo1 = k.R("wo1")
            self.load_w(wA[:], self.ab_w_in[:, 2048:2560].rearrange("(kc p) n -> p kc n", p=128), r_wA)
            self.load_w(wo1[:], self.ab_w_out[0:512, :].rearrange("(kc p) n -> p kc n", p=128), r_wo1)
            SG = [self.sb(es2, "roSG%d" % i, [128, 512]) for i in range(2)]
            SQ = [self.sb(es2, "roSQ%d" % i, [128, 512]) for i in range(2)]
            RS = [self.sb(es2, "roRS%d" % i, [128, 512]) for i in range(2)]
            r_SG, r_SQ, r_RS = k.Rs(2, "roSG"), k.Rs(2, "roSQ"), k.Rs(2, "roRS")
            a = self.sb(es2, "ro_a", [128, 4, 512], BF16)
            r_a = k.R("ro_a")
            banks = Banks(self.ps, self.r_ps, [0, 1, 2, 3, 4, 5, 6, 7])
            it = 0
            for bi, (c0, n, is_ctx, li) in enumerate(self.blocks):
                for h in range(4):
                    i2 = it % 2
                    it += 1
                    pg, r_pg = banks.get()
                    for kc in range(KC):
                        k.mm(pg[:, 0:n], wA[:, kc, h * 128:(h + 1) * 128], self.hT[:, kc, c0:c0 + n],
                             start=(kc == 0), stop=(kc == KC - 1), rd=[self.r_hT[bi], r_wA], wr=[r_pg])
                    k.actf(SG[i2][:, 0:n], pg[:, 0:n], AF.Silu, rd=[r_pg], wr=[r_SG[i2]])
                    o = self.osum[:, h, c0:c0 + n]
                    k.actf(SQ[i2][:, 0:n], o, AF.Square, rd=[self.r_osum[bi]], wr=[r_SQ[i2]])
                    pss, r_pss = banks.get()
                    k.mm(pss[:, 0:n], self.ccc("ONES", 128), SQ[i2][:, 0:n], start=True, stop=True, rd=[r_SQ[i2], self.r_cc], wr=[r_pss])
                    k.ts(RS[i2][:, 0:n], pss[:, 0:n], 1.0 / 128, EPS, ALU.mult, ALU.add, rd=[r_pss], wr=[r_RS[i2]])
                    k.actf(RS[i2][:, 0:n], RS[i2][:, 0:n], AF.Sqrt, rd=[r_RS[i2]], wr=[r_RS[i2]])
                    k.op(k.dve, lambda e: e.reciprocal(out=RS[i2][:, 0:n], in_=RS[i2][:, 0:n]), rd=[r_RS[i2]], wr=[r_RS[i2]])
                    k.stt(SQ[i2][:, 0:n], o, self.ppc("ONG"), RS[i2][:, 0:n], ALU.mult, ALU.mult,
                          rd=[self.r_osum[bi], self.r_pp, r_RS[i2], r_pss], wr=[r_SQ[i2]])
                    k.tt(a[:, h, 0:n], SQ[i2][:, 0:n], SG[i2][:, 0:n], ALU.mult, rd=[r_SQ[i2], r_SG[i2]], wr=[r_a])
                for m in range(KC):
                    pb, r_pb = banks.get()
                    for h in range(4):
                        k.mm(pb[:, 0:n], wo1[:, h, m * 128:(m + 1) * 128], a[:, h, 0:n], start=(h == 0), stop=(h == 3),
                             rd=[r_wo1, r_a], wr=[r_pb])
                    dst, r_dst, v = self.x_dst(is_ctx, li, m, n)
                    k.stt(dst, pb[:, 0:n], self.modv(0, 2, m, v), dst, ALU.mult, ALU.add, rd=[r_pb, self.r_mod, r_dst], wr=[r_dst])
            if self.stage == 4:
                self.dbg("a_last", a[:].rearrange("p a b -> p (a b)"), r_a, [128, 2048])
                self.dbg("xmix", self.xT[:, 0, :], self.r_xT, [128, NL])
                self.dbg("xcmix", self.xcT[:, 0, :], self.r_xcT, [128, NCX])
            k.barrier()

    def moe(self, l):
        k = self.k
        blocks = ([(0, NCX, True, -1)] if l == 0 else []) + [(NCX + 512 * i, 512, False, i) for i in range(4)]
        with contextlib.ExitStack() as es:
            h2T = self.sb(es, "h2T", [128, KC, NT], BF16)
            r_h2 = k.Rs(5, "h2T")
            combT = self.sb(es, "combT", [32, NT], BF16)
            r_combT = k.Rs(5, "combT")
            with contextlib.ExitStack() as es2:
                tmp = self.norm_tmp(es2, banks_idx=(4, 5))
                h2f = self.sb(es2, "h2f", [128, KC, 512])
                r_h2f = k.R("h2f")
                wr = self.sb(es2, "moe_wr", [128, KC, 36])
                r_wr = k.R("moe_wr")
                k.dma(k.sp, wr[:], self.moe_wr[l].rearrange("(kc p) n -> p kc n", p=128), wr=[r_wr])
                lg = self.sb(es2, "lg", [128, 4, 36])
                r_lg = k.R("lg")
                names = ["gmax", "gsum", "m1", "m2", "w1", "w2"]
                sm = {nm: self.sb(es2, "rt_" + nm, [128, 4]) for nm in names}
                og = self.sb(es2, "rt_og", [128, 4, 4])
                ge = self.sb(es2, "rt_ge", [128, 4, 4])
                elm = self.sb(es2, "rt_elm", [128, 4, 32])
                oh1 = self.sb(es2, "rt_oh1", [128, 4, 32])
                oh2 = self.sb(es2, "rt_oh2", [128, 4, 32])
                comb = self.sb(es2, "rt_comb", [128, 4, 32])
                r_rt = k.R("rt")
                rb = Banks(self.ps, self.r_ps, [6, 7])
                for bi_, (c0, n, is_ctx, li) in enumerate(blocks):
                    bi = bi_ if l == 0 else bi_ + 1
                    v = 1 if is_ctx else 0
                    nt = n // 128
                    src_fn = (lambda kc: self.xcT[:, kc, :]) if is_ctx else (lambda kc, li=li: self.xT[:, kc, li * 512:(li + 1) * 512])
                    self.norm_mod(tmp, src_fn, n, lambda kc, v=v: self.gs2[l][:, kc, v:v + 1],
                                  lambda kc, v=v: self.modv(l, 3, kc, v),
                                  lambda kc, c0=c0, n=n: h2T[:, kc, c0:c0 + n],
                                  rd=[self.r_xcT if is_ctx else self.r_xT[li]], wr=[r_h2[bi], r_h2f],
                                  f32_out_fn=lambda kc, n=n: h2f[:, kc, 0:n])
                    pr, r_pr = rb.get()
                    for t in range(nt):
                        for kc in range(KC):
                            k.mm(pr[:, t * 36:(t + 1) * 36], h2f[:, kc, t * 128:(t + 1) * 128], wr[:, kc, :],
                                 start=(kc == 0), stop=(kc == KC - 1), rd=[r_h2f, r_wr], wr=[r_pr])
                    k.tt(lg[:, 0:nt, :], pr[:, 0:nt * 36].rearrange("p (t c) -> p t c", c=36),
                         self.ppc("BR", l * 36, 36).unsqueeze(1).to_broadcast([128, nt, 36]), ALU.add, rd=[r_pr, self.r_pp], wr=[r_lg])
                    gl = lg[:, 0:nt, 0:4]
                    el = lg[:, 0:nt, 4:36]
                    R_ = [r_rt]

                    def bc(ap2, last):
                        return ap2.unsqueeze(2).to_broadcast([128, nt, last])

                    k.op(k.dve, lambda e: e.tensor_reduce(out=sm["gmax"][:, 0:nt], in_=gl, axis=AX.X, op=ALU.max), rd=[r_lg], wr=R_)
                    k.tt(og[:, 0:nt, :], gl, bc(sm["gmax"][:, 0:nt], 4), ALU.is_equal, rd=[r_lg] + R_, wr=R_)
                    k.tt(ge[:, 0:nt, :], gl, bc(sm["gmax"][:, 0:nt], 4), ALU.subtract, rd=[r_lg] + R_, wr=R_)
                    k.actf(ge[:, 0:nt, :], ge[:, 0:nt, :], AF.Exp, rd=R_, wr=R_)
                    k.op(k.dve, lambda e: e.tensor_reduce(out=sm["gsum"][:, 0:nt], in_=ge[:, 0:nt, :], axis=AX.X, op=ALU.add), rd=R_, wr=R_)
                    k.op(k.dve, lambda e: e.reciprocal(out=sm["gsum"][:, 0:nt], in_=sm["gsum"][:, 0:nt]), rd=R_, wr=R_)
                    k.ts(ge[:, 0:nt, :], og[:, 0:nt, :], BIG, -BIG, ALU.mult, ALU.add, rd=R_, wr=R_)
                    k.tt(elm[:, 0:nt, :].rearrange("p t (g e) -> p t g e", e=8), el.rearrange("p t (g e) -> p t g e", e=8),
                         ge[:, 0:nt, :].unsqueeze(3).to_broadcast([128, nt, 4, 8]), ALU.add, rd=[r_lg] + R_, wr=R_)
                    k.op(k.dve, lambda e: e.tensor_reduce(out=sm["m1"][:, 0:nt], in_=elm[:, 0:nt, :], axis=AX.X, op=ALU.max), rd=R_, wr=R_)
                    k.tt(oh1[:, 0:nt, :], elm[:, 0:nt, :], bc(sm["m1"][:, 0:nt], 32), ALU.is_equal, rd=R_, wr=R_)
                    k.stt(elm[:, 0:nt, :], oh1[:, 0:nt, :], -BIG, elm[:, 0:nt, :], ALU.mult, ALU.add, rd=R_, wr=R_)
                    k.op(k.dve, lambda e: e.tensor_reduce(out=sm["m2"][:, 0:nt], in_=elm[:, 0:nt, :], axis=AX.X, op=ALU.max), rd=R_, wr=R_)
                    k.tt(oh2[:, 0:nt, :], elm[:, 0:nt, :], bc(sm["m2"][:, 0:nt], 32), ALU.is_equal, rd=R_, wr=R_)
                    k.tt(sm["w2"][:, 0:nt], sm["m2"][:, 0:nt], sm["m1"][:, 0:nt], ALU.subtract, rd=R_, wr=R_)
                    k.actf(sm["w2"][:, 0:nt], sm["w2"][:, 0:nt], AF.Sigmoid, rd=R_, wr=R_)
                    k.ts(sm["w1"][:, 0:nt], sm["w2"][:, 0:nt], -1.0, 1.0, ALU.mult, ALU.add, rd=R_, wr=R_)
                    k.tt(sm["w1"][:, 0:nt], sm["w1"][:, 0:nt], sm["gsum"][:, 0:nt], ALU.mult, rd=R_, wr=R_)
                    k.tt(sm["w2"][:, 0:nt], sm["w2"][:, 0:nt], sm["gsum"][:, 0:nt], ALU.mult, rd=R_, wr=R_)
                    k.tt(oh1[:, 0:nt, :], oh1[:, 0:nt, :], bc(sm["w1"][:, 0:nt], 32), ALU.mult, rd=R_, wr=R_)
                    k.tt(oh2[:, 0:nt, :], oh2[:, 0:nt, :], bc(sm["w2"][:, 0:nt], 32), ALU.mult, rd=R_, wr=R_)
                    k.tt(comb[:, 0:nt, :], oh1[:, 0:nt, :], oh2[:, 0:nt, :], ALU.add, rd=R_, wr=R_)
                    pc, r_pc = rb.get()
                    for t in range(nt):
                        k.tr(pc[0:32, t * 128:(t + 1) * 128], comb[:, t, :], self.ccc("IDENT", 128), rd=R_ + [self.r_cc], wr=[r_pc], sig=(t == nt - 1))
                    k.cp(combT[:, c0:c0 + n], pc[0:32, 0:n], rd=[r_pc], wr=[r_combT[bi]], E=k.act)
                if self.stage == 5:
                    self.dbg("combT", combT[:, :], r_combT, [32, NT])
                k.barrier()
            with contextlib.ExitStack() as es2:
                NWB = 2
                wg = [self.sb(es2, "mwg%d" % i, [128, 2, KC, 256], BF16) for i in range(NWB)]
                wu = [self.sb(es2, "mwu%d" % i, [128, 2, KC, 256], BF16) for i in range(NWB)]
                wd = [self.sb(es2, "mwd%d" % i, [128, 2, 2, D], BF16) for i in range(NWB)]
                r_w = k.Rs(NWB, "moew")
                cme = [self.sb(es2, "cme%d" % i, [32, 512], BF16) for i in range(2)]
                r_cme = k.Rs(2, "cme")
                sgt = [self.sb(es2, "sgt%d" % i, [128, 512]) for i in range(2)]
                r_sgt = k.Rs(2, "sgt")
                tmu = [self.sb(es2, "tmu%d" % i, [128, 512]) for i in range(2)]
                r_tmu = k.Rs(2, "tmu")
                abuf = [self.sb(es2, "abuf%d" % i, [128, 2, 2, 512], BF16) for i in range(2)]
                r_ab = k.Rs(2, "abuf")
                cbanks = Banks(self.ps, self.r_ps, [6, 7])
                dbanks = Banks(self.ps, self.r_ps, [4, 5])
                it2 = 0
                ia = 0
                def issue_w(pi):
                    wbi = pi % NWB
                    for ei in range(2):
                        e = pi * 2 + ei
                        self.load_w(wg[wbi][:, ei, :, :], self.moe_w_gate[l, e].rearrange("(kc p) f -> p kc f", p=128), r_w[wbi])
                        self.load_w(wu[wbi][:, ei, :, :], self.moe_w_up[l, e].rearrange("(kc p) f -> p kc f", p=128), r_w[wbi])
                        self.load_w(wd[wbi][:, ei, :, :], self.moe_w_down[l, e].rearrange("(fc p) d -> p fc d", p=128), r_w[wbi])

                issue_w(0)
                for pi in range(16):
                    wbi = pi % NWB
                    if pi + 1 < 16:
                        issue_w(pi + 1)
                    for bi_, (c0, n, is_ctx, li) in enumerate(blocks):
                        bi = bi_ if l == 0 else bi_ + 1
                        ab, r_a = abuf[ia % 2], r_ab[ia % 2]
                        ia += 1
                        for ei in range(2):
                            e = pi * 2 + ei
                            ci = it2 % 2
                            k.actf(cme[ci][:, 0:n], combT[:, c0:c0 + n], AF.Copy, rd=[r_combT[bi], self.r_cc], wr=[r_cme[ci]],
                                   scale=self.ccc("IDENT", 32)[0:32, e:e + 1])
                            pc, r_pc = cbanks.get()
                            k.mm(pc[:, 0:n], self.onesb[0:32, :], cme[ci][:, 0:n], start=True, stop=True,
                                 rd=[r_cme[ci], self.r_const], wr=[r_pc])
                            for fc in range(2):
                                pgt, r_pgt = self.ps[fc * 2], self.r_ps[fc * 2]
                                put, r_put = self.ps[fc * 2 + 1], self.r_ps[fc * 2 + 1]
                                for kc in range(KC):
                                    k.mm(pgt[:, 0:n], wg[wbi][:, ei, kc, fc * 128:(fc + 1) * 128], h2T[:, kc, c0:c0 + n],
                                         start=(kc == 0), stop=(kc == KC - 1), rd=[r_w[wbi], r_h2[bi]], wr=[r_pgt])
                                for kc in range(KC):
                                    k.mm(put[:, 0:n], wu[wbi][:, ei, kc, fc * 128:(fc + 1) * 128], h2T[:, kc, c0:c0 + n],
                                         start=(kc == 0), stop=(kc == KC - 1), rd=[r_w[wbi], r_h2[bi]], wr=[r_put])
                                si = it2 % 2
                                it2 += 1
                                k.actf(sgt[si][:, 0:n], pgt[:, 0:n], AF.Silu, rd=[r_pgt], wr=[r_sgt[si]])
                                k.tt(tmu[si][:, 0:n], put[:, 0:n], sgt[si][:, 0:n], ALU.mult, rd=[r_put, r_sgt[si]], wr=[r_tmu[si]])
                                k.tt(ab[:, ei, fc, 0:n], tmu[si][:, 0:n], pc[:, 0:n], ALU.mult, rd=[r_tmu[si], r_pc], wr=[r_a])
                        for m in range(KC):
                            pb, r_pb = dbanks.get()
                            for j4 in range(4):
                                ei, fc = j4 // 2, j4 % 2
                                k.mm(pb[:, 0:n], wd[wbi][:, ei, fc, m * 128:(m + 1) * 128], ab[:, ei, fc, 0:n], start=(j4 == 0), stop=(j4 == 3),
                                     rd=[r_w[wbi], r_a], wr=[r_pb])
                            dst, r_dst, v = self.x_dst(is_ctx, li, m, n)
                            k.stt(dst, pb[:, 0:n], self.modv(l, 5, m, v), dst, ALU.mult, ALU.add, rd=[r_pb, self.r_mod, r_dst], wr=[r_dst])
                k.barrier()

    CS = 8224

    def layer1(self):
        k, nc = self.k, self.nc
        L = 1
        with contextlib.ExitStack() as es:
            cqn = self.sb(es, "cqn", [128, 2, NL], BF16)
            kvc = self.sb(es, "kvc", [128, NCX], BF16)
            krc = self.sb(es, "krc", [96, NCX], BF16)
            r_cqn, r_kvc, r_krc = k.R("cqn"), k.R("kvc"), k.R("krc")
            widths = {"A": 4096, "B": 2048, "C": 2304}
            l1_in = {n_: nc.dram_tensor("l1_in" + n_, [128, w_], BF16, kind="Internal").ap() for n_, w_ in widths.items()}
            l1_out = {n_: nc.dram_tensor("l1_out" + n_, [4 * 128, w_], BF16, kind="Internal").ap() for n_, w_ in widths.items()}
            r_in = {n_: k.R("l1_in" + n_) for n_ in widths}
            r_out = {n_: k.R("l1_out" + n_) for n_ in widths}
            with contextlib.ExitStack() as esn:
                self.layer1_na(esn, L, cqn, kvc, krc, r_cqn, r_kvc, r_krc, l1_in, l1_out, r_in, r_out)
            if self.stage >= 9:
                self.mla_attention(cqn, kvc, krc, r_cqn, r_kvc, r_krc, l1_out, r_out)

    def layer1_na(self, es, L, cqn, kvc, krc, r_cqn, r_kvc, r_krc, l1_in, l1_out, r_in, r_out):
        k, nc = self.k, self.nc
        if True:
            QT2 = self.sb(es, "QT2", [128, 4, NL], BF16)
            KT2 = self.sb(es, "KT2", [128, 4, NCX + 2560], BF16)
            VX = self.sb(es, "VX", [128, 22, 8, 72], BF16)
            r_QT2, r_KT2, r_VX = (k.R(n_) for n_ in ("QT2", "KT2", "VX"))
            k.op(k.pool, lambda e: e.memset(VX[:].rearrange("p t h d -> p (t h d)"), 0.0), wr=[r_VX])
            k.op(k.pool, lambda e: e.memset(VX[:, :, :, 64:65], 1.0), wr=[r_VX])
            with contextlib.ExitStack() as es2:
                tmp = self.norm_tmp(es2, banks_idx=(6, 7))
                hb = [self.sb(es2, "hblk%d" % i, [128, KC, 512], BF16) for i in range(1)] * 2
                r_hb = k.Rs(1, "hblk") * 2
                wb = [self.sb(es2, "l1w%d" % i, [128, KC, 512], BF16) for i in range(2)]
                r_wb = k.Rs(2, "l1w")
                wkr = self.sb(es2, "wkr", [128, KC, 192], BF16)
                r_wkr = k.R("wkr")
                self.load_w(wkr[:], self.wkr_d.rearrange("(kc p) n -> p kc n", p=128), r_wkr)
                rope = self.sb(es2, "ropeA", [128, 2, 512])
                r_rope = k.R("ropeA")
                t32 = [self.sb(es2, "l1t%d" % i, [128, 512]) for i in range(4)]
                r_t32 = k.Rs(4, "l1t")
                tb = [self.sb(es2, "l1tb%d" % i, [128, 512], BF16) for i in range(2)]
                r_tb = k.Rs(2, "l1tb")
                banks = Banks(self.ps, self.r_ps, [0, 1, 2, 3, 4, 5])
                iw = 0
                wreq = []
                for (_c0, _n, _ctx, _li) in self.blocks:
                    wreq += ([] if _ctx else [(0, 512)]) + [(512, 512), (1024, 512), (1536, 384)]
                wstate = {"issued": 0, "used": 0}

                def issue_next():
                    i = wstate["issued"]
                    if i >= len(wreq):
                        return
                    col0, ncol = wreq[i]
                    self.load_w(wb[i % 2][:, :, 0:ncol], self.cd_w_in[:, col0:col0 + ncol].rearrange("(kc p) n -> p kc n", p=128), r_wb[i % 2])
                    wstate["issued"] = i + 1

                issue_next()
                for bi, (c0, n, is_ctx, li) in enumerate(self.blocks):
                    v = 1 if is_ctx else 0
                    h, r_h = hb[bi % 2], r_hb[bi % 2]
                    src_fn = (lambda kc: self.xcT[:, kc, :]) if is_ctx else (lambda kc, li=li: self.xT[:, kc, li * 512:(li + 1) * 512])
                    self.norm_mod(tmp, src_fn, n, lambda kc, v=v: self.gs1[L][:, kc, v:v + 1],
                                  lambda kc, v=v: self.modv(L, 0, kc, v), lambda kc, n=n: h[:, kc, 0:n],
                                  rd=[self.r_xcT if is_ctx else self.r_xT[li]], wr=[r_h])
                    lc = 0 if is_ctx else li * 512
                    ntile = n // 128

                    def getw(col0, ncol):
                        i = wstate["used"]
                        assert wreq[i] == (col0, ncol)
                        wstate["used"] = i + 1
                        if wstate["issued"] <= i:
                            issue_next()
                        w, r_w = wb[i % 2], r_wb[i % 2]
                        issue_next()
                        return w, r_w

                    def proj_fm(w, r_w, wc0, M, pb, r_pb):
                        for kc in range(KC):
                            k.mm(pb[0:M, 0:n], w[:, kc, wc0:wc0 + M], h[:, kc, 0:n], start=(kc == 0), stop=(kc == KC - 1),
                                 rd=[r_w, r_h], wr=[r_pb])

                    if not is_ctx:
                        w, r_w = getw(0, 512)
                        for pr in range(4):
                            pb, r_pb = banks.get()
                            proj_fm(w, r_w, pr * 128, 128, pb, r_pb)
                            k.cp(QT2[:, pr, lc:lc + n], pb[:, 0:n], rd=[r_pb], wr=[r_QT2], E=(k.act if pr % 2 else k.dve))
                    w, r_w = getw(512, 512)
                    kc0 = 0 if is_ctx else NCX + 256 + lc
                    for pr in range(4):
                        pb, r_pb = banks.get()
                        proj_fm(w, r_w, pr * 128, 128, pb, r_pb)
                        k.cp(KT2[:, pr, kc0:kc0 + n], pb[:, 0:n], rd=[r_pb], wr=[r_KT2], E=(k.act if pr % 2 else k.dve))
                    w, r_w = getw(1024, 512)
                    vt0 = 0 if is_ctx else 4 + li * 4
                    for t in range(ntile):
                        pb, r_pb = banks.get()
                        for kc in range(KC):
                            k.mm(pb[:, :], h[:, kc, t * 128:(t + 1) * 128], w[:, kc, :], start=(kc == 0), stop=(kc == KC - 1),
                                 rd=[r_w, r_h], wr=[r_pb])
                        k.cp(VX[:, vt0 + t, :, 0:64], pb[:, :].rearrange("p (h d) -> p h d", d=64), rd=[r_pb], wr=[r_VX], E=(k.act if t % 2 else k.dve))
                    w, r_w = getw(1536, 384)
                    if not is_ctx:
                        pcq = []
                        pss, r_pss = banks.get()
                        for c in range(2):
                            pb, r_pb = banks.get()
                            proj_fm(w, r_w, c * 128, 128, pb, r_pb)
                            pcq.append((pb, r_pb))
                            k.actf(t32[c][:, 0:n], pb[:, 0:n], AF.Square, rd=[r_pb], wr=[r_t32[c]])
                            k.mm(pss[:, 0:n], self.ccc("ONES", 128), t32[c][:, 0:n], start=(c == 0), stop=(c == 1), rd=[r_t32[c], self.r_cc], wr=[r_pss], sig=True)
                        k.ts(t32[2][:, 0:n], pss[:, 0:n], 1.0 / 256, EPS, ALU.mult, ALU.add, rd=[r_pss], wr=[r_t32[2]])
                        k.actf(t32[2][:, 0:n], t32[2][:, 0:n], AF.Sqrt, rd=[r_t32[2]], wr=[r_t32[2]])
                        k.op(k.dve, lambda e: e.reciprocal(out=t32[2][:, 0:n], in_=t32[2][:, 0:n]), rd=[r_t32[2]], wr=[r_t32[2]])
                        for c in range(2):
                            pb, r_pb = pcq[c]
                            k.stt(cqn[:, c, lc:lc + n], pb[:, 0:n], self.ppc("QNG", c), t32[2][:, 0:n], ALU.mult, ALU.mult,
                                  rd=[r_pb, self.r_pp, r_t32[2]], wr=[r_cqn])
                    pb, r_pb = banks.get()
                    proj_fm(w, r_w, 256, 128, pb, r_pb)
                    k.actf(t32[0][:, 0:n], pb[:, 0:n], AF.Square, rd=[r_pb], wr=[r_t32[0]])
                    pss, r_pss = banks.get()
                    k.mm(pss[:, 0:n], self.ccc("ONES", 128), t32[0][:, 0:n], start=True, stop=True, rd=[r_t32[0], self.r_cc], wr=[r_pss])
                    k.ts(t32[3][:, 0:n], pss[:, 0:n], 1.0 / 128, EPS, ALU.mult, ALU.add, rd=[r_pss], wr=[r_t32[3]])
                    k.actf(t32[3][:, 0:n], t32[3][:, 0:n], AF.Sqrt, rd=[r_t32[3]], wr=[r_t32[3]])
                    k.op(k.dve, lambda e: e.reciprocal(out=t32[3][:, 0:n], in_=t32[3][:, 0:n]), rd=[r_t32[3]], wr=[r_t32[3]])
                    if is_ctx:
                        k.stt(kvc[:, 0:n], pb[:, 0:n], self.ppc("KVNG"), t32[3][:, 0:n], ALU.mult, ALU.mult,
                              rd=[r_pb, self.r_pp, r_t32[3]], wr=[r_kvc])
                    else:
                        k.stt(tb[0][:, 0:n], pb[:, 0:n], self.ppc("KVNG"), t32[3][:, 0:n], ALU.mult, ALU.mult,
                              rd=[r_pb, self.r_pp, r_t32[3]], wr=[r_tb[0]])
                        k.dma(k.sp, l1_in["A"][:, lc:lc + n], tb[0][:, 0:n], rd=[r_tb[0]], wr=[r_in["A"]])
                    pb, r_pb = banks.get()
                    proj_fm(wkr, r_wkr, 0, 96, pb, r_pb)
                    if is_ctx:
                        k.cp(krc[64:96, 0:n], pb[64:96, 0:n], rd=[r_pb], wr=[r_krc], E=k.act)
                    else:
                        pb2, r_pb2 = banks.get()
                        proj_fm(wkr, r_wkr, 96, 96, pb2, r_pb2)
                        k.dma(k.sp, rope[64:96, :, 0:n], self.rope_d[:, :, lc:lc + n], wr=[r_rope])
                        k.tt(t32[1][64:96, 0:n], pb[64:96, 0:n], rope[64:96, 0, 0:n], ALU.mult, rd=[r_pb, r_rope], wr=[r_t32[1]])
                        k.tt(t32[0][64:96, 0:n], pb2[64:96, 0:n], rope[64:96, 1, 0:n], ALU.mult, rd=[r_pb2, r_rope, r_pss], wr=[r_t32[0]])
                        k.tt(tb[1][64:96, 0:n], t32[1][64:96, 0:n], t32[0][64:96, 0:n], ALU.add, rd=[r_t32[0], r_t32[1]], wr=[r_tb[1]])
                        k.dma(k.sp, l1_in["A"][0:32, 2048 + lc:2048 + lc + n], tb[1][64:96, 0:n], rd=[r_tb[1]], wr=[r_in["A"]])
                o0 = NCX + 256
                kin = l1_in["B"][:, :].rearrange("p (a c) -> p a c", a=4)
                k.dma(k.sp, kin[:, :, 0:256], KT2[:, :, o0:o0 + 256], rd=[r_KT2], wr=[r_in["B"]])
                k.dma(k.sp, kin[:, :, 256:512], KT2[:, :, o0 + NL - 256:o0 + NL], rd=[r_KT2], wr=[r_in["B"]])
                vin = l1_in["C"][:, :].rearrange("p (t c) -> p t c", t=4)
                k.dma(k.sp, vin[:, 0:2, :], VX[:, 4:6, :, :].rearrange("p t h d -> p t (h d)"), rd=[r_VX], wr=[r_in["C"]])
                k.dma(k.sp, vin[:, 2:4, :], VX[:, 18:20, :, :].rearrange("p t h d -> p t (h d)"), rd=[r_VX], wr=[r_in["C"]])
                for n_ in ("A", "B", "C"):
                    k.collective(lambda g, n_=n_: g.collective_compute("AllGather", ALU.bypass, replica_groups=[[0, 1, 2, 3], [4, 5, 6, 7]],
                                                                       ins=[l1_in[n_][:, :]], outs=[l1_out[n_][:, :]]),
                                 rd=[r_in[n_]], wr=[r_out[n_]])
                k.barrier()
            ovB = l1_out["B"].rearrange("(r p) c -> p r c", p=128)
            ovC = l1_out["C"].rearrange("(r p) c -> p r c", p=128)
            with contextlib.ExitStack() as es2:
                kp = self.sb(es2, "kpart", [128, 4, 4, 512], BF16)
                vp = self.sb(es2, "vpart", [128, 4, 4, 576], BF16)
                r_kp, r_vp = k.R("kpart"), k.R("vpart")
                k.dma(k.sp, kp[:].rearrange("p r a c -> p r (a c)"), ovB[:, :, :], rd=[r_out["B"]], wr=[r_kp])
                k.dma(k.sp, vp[:].rearrange("p r t c -> p r (t c)"), ovC[:, :, :], rd=[r_out["C"]], wr=[r_vp])
                top_k = KT2[:, :, NCX:NCX + 256]
                bot_k = KT2[:, :, NCX + 256 + NL:NCX + 512 + NL]
                top_v = VX[:, 2:4, :, :].rearrange("p t h d -> p t (h d)")
                bot_v = VX[:, 20:22, :, :].rearrange("p t h d -> p t (h d)")
                for r in range(4):
                    for (dst, src, sel, rr, r_dst) in ((top_k, kp[:, r, :, 256:512], "SELT", r_kp, r_KT2), (bot_k, kp[:, r, :, 0:256], "SELBT", r_kp, r_KT2),
                                                       (top_v, vp[:, r, 2:4, :], "SELT", r_vp, r_VX), (bot_v, vp[:, r, 0:2, :], "SELBT", r_vp, r_VX)):
                        if r == 0:
                            k.ts(dst, src, self.ppc(sel, r), None, ALU.mult, None, rd=[rr, self.r_pp], wr=[r_dst])
                        else:
                            k.stt(dst, src, self.ppc(sel, r), dst, ALU.mult, ALU.add, rd=[rr, self.r_pp, r_dst], wr=[r_dst])
                k.barrier()
            if self.stage == 7:
                self.dbg("KT2", KT2[:, 0, :], r_KT2, [128, NCX + 2560])
                self.dbg("cqn", cqn[:, 0, :], r_cqn, [128, NL])
                self.dbg("VX", VX[:, :, 0, :], r_VX, [128, 22, 72])
            if self.stage >= 8:
                self.na_attention(L, QT2, KT2, VX, r_QT2, r_KT2, r_VX)

    def apply_wout(self, L, OT2, r_OT2, row0):
        k = self.k
        with contextlib.ExitStack() as es:
            wo = self.sb(es, "l1wo", [128, 4, D], BF16)
            r_wo = k.R("l1wo")
            self.load_w(wo[:], self.cd_w_out[row0:row0 + 512, :].rearrange("(kc p) n -> p kc n", p=128), r_wo)
            banks = Banks(self.ps, self.r_ps, [0, 1, 2, 3])
            for li in range(4):
                for m in range(KC):
                    pb, r_pb = banks.get()
                    for pr in range(4):
                        k.mm(pb[:, :], wo[:, pr, m * 128:(m + 1) * 128], OT2[:, pr, li * 512:(li + 1) * 512], start=(pr == 0), stop=(pr == 3),
                             rd=[r_wo, r_OT2], wr=[r_pb])
                    dst, r_dst, v = self.x_dst(False, li, m, 512)
                    k.stt(dst, pb[:, :], self.modv(L, 2, m, 0), dst, ALU.mult, ALU.add, rd=[r_pb, self.r_mod, r_dst], wr=[r_dst])
            k.barrier()

    def na_attention(self, L, QT2, KT2, VX, r_QT2, r_KT2, r_VX):
        k = self.k
        with contextlib.ExitStack() as es:
            Mi = self.sb(es, "naMi", [128, 5, 8, 128], BF16)
            Ms = self.sb(es, "naMs", [128, 6, 8, 128], BF16)
            r_Mi, r_Ms = k.R("naMi"), k.R("naMs")
            bst = [self.sb(es, "nabst%d" % i, [128, 8, 128]) for i in range(2)]
            r_bst = k.Rs(2, "nabst")
            Et = [self.sb(es, "naE%d" % i, [128, 512], BF16) for i in range(3)]
            Pt = [self.sb(es, "naP%d" % i, [128, 512], BF16) for i in range(3)]
            r_Et, r_Pt = k.Rs(3, "naE"), k.Rs(3, "naP")
            Otok = [self.sb(es, "naOtok%d" % i, [128, 8, 64], BF16) for i in range(2)]
            r_Otok = k.Rs(2, "naOtok")
            rden = [self.sb(es, "narden%d" % i, [128, 4]) for i in range(2)]
            r_rden = k.Rs(2, "narden")
            Qpad = [self.sb(es, "naQpad%d" % i, [128, 8, 128], BF16) for i in range(2)]
            r_Qpad = k.Rs(2, "naQpad")
            for i in range(2):
                k.op(k.pool, lambda e, i=i: e.memset(Qpad[i][:].rearrange("p h q -> p (h q)"), 0.0), wr=[r_Qpad[i]])
            ib = 0
            for kt in range(5):
                b = ib % 2
                ib += 1
                k.dma(k.sp, bst[b][:], self.nab_int[kt], wr=[r_bst[b]])
                k.actf(Mi[:, kt, :, :], bst[b][:], AF.Exp, rd=[r_bst[b]], wr=[r_Mi])
            sbanks = Banks(self.ps, self.r_ps, [0, 1, 2])
            obanks = Banks(self.ps, self.r_ps, [3, 4, 5, 6])
            ie = 0
            for qb in range(16):
                if qb < 2:
                    base, nt, sp = 2 * qb - 4, 6, qb
                elif qb >= 14:
                    base, nt, sp = 2 * qb - 6, 6, 2 + qb - 14
                else:
                    base, nt, sp = 2 * qb - 4, 5, None
                if sp is not None:
                    for kt in range(6):
                        b = ib % 2
                        ib += 1
                        k.dma(k.sp, bst[b][:], self.nab_sp[sp, kt], wr=[r_bst[b]])
                        k.actf(Ms[:, kt, :, :], bst[b][:], AF.Exp, rd=[r_bst[b]], wr=[r_Ms])
                    M, r_M = Ms, r_Ms
                else:
                    M, r_M = Mi, r_Mi
                q0 = qb * 128
                qp, r_qp = Qpad[qb % 2], r_Qpad[qb % 2]
                qpv = qp[:, :, :].rearrange("p (a two) q -> p a two q", two=2)
                k.cp(qpv[0:64, :, 0, :], QT2[0:64, :, q0:q0 + 128], rd=[r_QT2], wr=[r_qp], E=k.pool)
                k.cp(qpv[64:128, :, 1, :], QT2[64:128, :, q0:q0 + 128], rd=[r_QT2], wr=[r_qp], E=k.pool)
                tiles = [("ctx", 0), ("ctx", 1)] + [("loc", kt) for kt in range(nt)]
                ot, r_ot = Otok[qb % 2], r_Otok[qb % 2]
                for hg in range(2):
                    pOs = [(self.ps[3 + hh], self.r_ps[3 + hh]) for hh in range(4)]
                    pend = None
                    for ti in range(len(tiles) + 1):
                        cur = None
                        if ti < len(tiles):
                            kind, kt = tiles[ti]
                            if kind == "ctx":
                                kcol, vt = kt * 128, kt
                            else:
                                row = base + 2 * kt
                                kcol, vt = NCX + (row + 4) * 64, 2 + (row + 4) // 2
                            pS, r_pS = sbanks.get()
                            for hh in range(4):
                                h = 4 * hg + hh
                                pr = h // 2
                                k.mm(pS[:, hh * 128:(hh + 1) * 128], KT2[:, pr, kcol:kcol + 128], qp[:, h, :],
                                     start=True, stop=True, rd=[r_KT2, r_qp], wr=[r_pS], sig=(hh == 3))
                            i3 = ie % 3
                            ie += 1
                            E, r_E = Et[i3], r_Et[i3]
                            k.actf(E[:, :], pS[:, :], AF.Exp, rd=[r_pS], wr=[r_E], scale=0.125)
                            if kind == "loc":
                                P, r_P = Pt[i3], r_Pt[i3]
                                k.tt(P[:, :].rearrange("p (h q) -> p h q", q=128), E[:, :].rearrange("p (h q) -> p h q", q=128),
                                     M[:, kt, 4 * hg:4 * hg + 4, :], ALU.mult, rd=[r_E, r_M], wr=[r_P])
                            else:
                                P, r_P = E, r_E
                            cur = (P, r_P, vt, ti)
                        if pend is not None:
                            P_, r_P_, vt_, ti_ = pend
                            for hh in range(4):
                                h = 4 * hg + hh
                                k.mm(pOs[hh][0][:, 0:65], P_[:, hh * 128:(hh + 1) * 128], VX[:, vt_, h, 0:65], start=(ti_ == 0),
                                     stop=(ti_ == len(tiles) - 1), rd=[r_P_, r_VX], wr=[pOs[hh][1]], sig=True)
                        pend = cur
                    rd_, r_rd = rden[hg], r_rden[hg]
                    for hh in range(4):
                        pO, r_pO = pOs[hh]
                        k.op(k.dve, lambda e: e.reciprocal(out=rd_[:, hh:hh + 1], in_=pO[:, 64:65]), rd=[r_pO], wr=[r_rd])
                        k.ts(ot[:, 4 * hg + hh, :], pO[:, 0:64], rd_[:, hh:hh + 1], None, ALU.mult, None, rd=[r_pO, r_rd], wr=[r_ot])
                pT, r_pT = self.ps[7], self.r_ps[7]
                pTb = pT[:, :].bitcast(BF16)
                for pr in range(4):
                    k.tr(pTb[:, pr * 128:(pr + 1) * 128], ot[:, 2 * pr:2 * pr + 2, :].rearrange("p h d -> p (h d)"), self.identb[:],
                         rd=[r_ot, self.r_const], wr=[r_pT], sig=(pr == 3))
                k.cp(QT2[:, :, q0:q0 + 128], pTb[:, 0:512].rearrange("p (a q) -> p a q", q=128), rd=[r_pT], wr=[r_QT2], E=k.act)
            if self.debug:
                self.dbg("OTna", QT2[:, 0, :], r_QT2, [128, NL])
            k.barrier()
        self.apply_wout(L, QT2, r_QT2, 0)

    def mla_attention(self, cqn, kvc, krc, r_cqn, r_kvc, r_krc, l1_out, r_out):
        k = self.k
        L = 1
        NK = NCX + 4 * NL
        NKT = NK // 128
        scale = 96.0 ** -0.5
        with contextlib.ExitStack() as es:
            OT2 = self.sb(es, "mlaOT2", [128, 4, NL], BF16)
            r_OT2 = k.R("mlaOT2")
            with contextlib.ExitStack() as es1:
                KVall = self.sb(es1, "KVall", [128, NK], BF16)
                Kh = self.sb(es1, "Kh", [128, NK], BF16)
                r_KV, r_Khn, r_Khr = k.R("KVall"), k.R("Khn"), k.R("Khr")
                ov = l1_out["A"].rearrange("(r p) c -> p r c", p=128)
                k.op(k.pool, lambda e: e.memset(Kh[64:128, :], 0.0), wr=[r_Khr])
                k.cp(KVall[:, 0:NCX], kvc[:, :], rd=[r_kvc], wr=[r_KV], E=k.pool)
                k.cp(Kh[64:96, 0:NCX], krc[64:96, :], rd=[r_krc], wr=[r_Khr], E=k.pool)
                k.dma(k.sp, KVall[:, NCX:].rearrange("p (r c) -> p r c", r=4), ov[:, :, 0:NL], rd=[r_out["A"]], wr=[r_KV])
                k.dma(k.sp, Kh[64:96, NCX:].rearrange("p (r c) -> p r c", r=4), ov[0:32, :, 2048:2048 + NL], rd=[r_out["A"]], wr=[r_Khr])
                wuq = self.sb(es1, "wuq", [128, 2, 8, 96], BF16)
                wuqs = self.sb(es1, "wuqs", [128, 2, 8, 96], BF16)
                wukv = self.sb(es1, "wukv", [128, 8, 128], BF16)
                r_wm = k.R("mlaw")
                self.load_w(wuq[:].rearrange("p c h d -> p c (h d)"), self.w_uq_d.rearrange("(c p) n -> p c n", p=128), r_wm)
                self.load_w(wuqs[:].rearrange("p c h d -> p c (h d)"), self.w_uqs_d.rearrange("(c p) n -> p c n", p=128), r_wm)
                self.load_w(wukv[:].rearrange("p h d -> p (h d)"), self.w_ukv_d[:, :], r_wm)
                Vh = self.sb(es1, "Vh", [128, NKT, 72], BF16)
                Qh = self.sb(es1, "Qh", [128, NL], BF16)
                r_Vh, r_Qh = k.R("Vh"), k.R("Qh")
                k.op(k.pool, lambda e: e.memset(Vh[:, :, 64:65], 1.0), wr=[r_Vh])
                k.op(k.pool, lambda e: e.memset(Qh[64:128, :], 0.0), wr=[r_Qh])
                rope = self.sb(es1, "ropeM", [128, 2, 512])
                r_rope = k.R("ropeM")
                rt = [self.sb(es1, "mlart%d" % i, [128, 512]) for i in range(2)]
                r_rt = k.Rs(2, "mlart")
                Otok = self.sb(es1, "mlaOtok", [128, 16, 8, 64], BF16)
                r_Otok = k.R("mlaOtok")
                Et = [self.sb(es1, "mlaE%d" % i, [128, 512], BF16) for i in range(4)]
                r_Et = k.Rs(4, "mlaE")
                rden = self.sb(es1, "mlarden", [128, 4])
                r_rden = k.R("mlarden")
                gbanks = Banks(self.ps, self.r_ps, [0])
                sbanks = Banks(self.ps, self.r_ps, [1, 2, 3])
                ie = 0
                for h in range(8):
                    for kb in range((NK + 511) // 512):
                        c0 = kb * 512
                        n = min(512, NK - c0)
                        pK, r_pK = gbanks.get()
                        k.mm(pK[0:64, 0:n], wukv[:, h, 0:64], KVall[:, c0:c0 + n], start=True, stop=True, rd=[r_wm, r_KV], wr=[r_pK])
                        k.cp(Kh[0:64, c0:c0 + n], pK[0:64, 0:n], rd=[r_pK], wr=[r_Khn], E=(k.act if kb % 2 else k.dve))
                    for g0 in range(0, NKT, 8):
                        nt = min(8, NKT - g0)
                        pV, r_pV = gbanks.get()
                        for t in range(nt):
                            kt = g0 + t
                            k.mm(pV[:, t * 64:(t + 1) * 64], KVall[:, kt * 128:(kt + 1) * 128], wukv[:, h, 64:128], start=True, stop=True,
                                 rd=[r_wm, r_KV], wr=[r_pV], sig=(t == nt - 1))
                        k.cp(Vh[:, g0:g0 + nt, 0:64], pV[:, 0:nt * 64].rearrange("p (t d) -> p t d", d=64), rd=[r_pV], wr=[r_Vh],
                             E=(k.act if (g0 // 8) % 2 else k.dve))
                    for qb in range(4):
                        qc = slice(qb * 512, (qb + 1) * 512)
                        pA, r_pA = gbanks.get()
                        pB, r_pB = sbanks.get()
                        for c in range(2):
                            k.mm(pA[0:96, :], wuq[:, c, h, :], cqn[:, c, qc], start=(c == 0), stop=(c == 1), rd=[r_wm, r_cqn], wr=[r_pA])
                        for c in range(2):
                            k.mm(pB[0:96, :], wuqs[:, c, h, :], cqn[:, c, qc], start=(c == 0), stop=(c == 1), rd=[r_wm, r_cqn], wr=[r_pB])
                        k.cp(Qh[0:64, qc], pA[0:64, :], rd=[r_pA], wr=[r_Qh], E=k.act)
                        k.dma(k.sp, rope[64:96, :, :], self.rope_d[:, :, qc], wr=[r_rope])
                        k.tt(rt[0][64:96, :], pA[64:96, :], rope[64:96, 0, :], ALU.mult, rd=[r_pA, r_rope], wr=[r_rt[0]])
                        k.tt(rt[1][64:96, :], pB[64:96, :], rope[64:96, 1, :], ALU.mult, rd=[r_pB, r_rope], wr=[r_rt[1]])
                        k.tt(Qh[64:96, qc], rt[0][64:96, :], rt[1][64:96, :], ALU.add, rd=r_rt, wr=[r_Qh])
                    for qb in range(4):
                        pOs = [(self.ps[4 + sub], self.r_ps[4 + sub]) for sub in range(4)]
                        pend = None
                        for kt in range(NKT + 1):
                            cur = None
                            if kt < NKT:
                                pS, r_pS = sbanks.get()
                                k.mm(pS[:, :], Kh[:, kt * 128:(kt + 1) * 128], Qh[:, qb * 512:(qb + 1) * 512], start=True, stop=True,
                                     rd=[r_Khn, r_Khr, r_Qh], wr=[r_pS])
                                i3 = ie % 4
                                ie += 1
                                E, r_E = Et[i3], r_Et[i3]
                                k.actf(E[:, :], pS[:, :], AF.Exp, rd=[r_pS], wr=[r_E], scale=scale)
                                cur = (E, r_E, kt)
                            if pend is not None:
                                E_, r_E_, kt_ = pend
                                for sub in range(4):
                                    k.mm(pOs[sub][0][:, 0:65], E_[:, sub * 128:(sub + 1) * 128], Vh[:, kt_, 0:65], start=(kt_ == 0), stop=(kt_ == NKT - 1),
                                         rd=[r_E_, r_Vh], wr=[pOs[sub][1]], sig=(sub == 3 or kt_ == NKT - 1))
                            pend = cur
                        for sub in range(4):
                            pO, r_pO = pOs[sub]
                            k.op(k.dve, lambda e: e.reciprocal(out=rden[:, sub:sub + 1], in_=pO[:, 64:65]), rd=[r_pO], wr=[r_rden])
                            k.ts(Otok[:, qb * 4 + sub, h, :], pO[:, 0:64], rden[:, sub:sub + 1], None, ALU.mult, None, rd=[r_pO, r_rden], wr=[r_Otok])
                pT, r_pT = self.ps[0], self.r_ps[0]
                pTb = pT[:, :].bitcast(BF16)
                for qt in range(16):
                    for pr in range(4):
                        k.tr(pTb[:, pr * 128:(pr + 1) * 128], Otok[:, qt, 2 * pr:2 * pr + 2, :].rearrange("p h d -> p (h d)"), self.identb[:],
                             rd=[r_Otok, self.r_const], wr=[r_pT], sig=(pr == 3))
                    k.cp(OT2[:, :, qt * 128:(qt + 1) * 128], pTb[:, 0:512].rearrange("p (a q) -> p a q", q=128), rd=[r_pT], wr=[r_OT2],
                         E=(k.act if qt % 2 else k.dve))
                if self.debug:
                    self.dbg("OTmla", OT2[:, 0, :], r_OT2, [128, NL])
                k.barrier()
            self.apply_wout(L, OT2, r_OT2, 512)

    def final_out(self):
        k = self.k
        with contextlib.ExitStack() as es:
            tmp = self.norm_tmp(es, banks_idx=(6, 7))
            yf = self.sb(es, "yf", [128, KC, 512])
            r_yf = k.R("yf")
            ytok = [self.sb(es, "ytok%d" % i, [128, D]) for i in range(2)]
            r_ytok = k.Rs(2, "ytok")
            r_y = k.R("y_out")
            self.out_res.append(r_y)
            banks = Banks(self.ps, self.r_ps, [0, 1, 2, 3])
            it = 0
            for li in range(4):
                n = 512
                pb, r_pb = tmp["banks"].get()
                for kc in range(KC):
                    i = tmp["i"] % 3
                    tmp["i"] += 1
                    sq, r_sq = tmp["sq"][i], tmp["r_sq"][i]
                    k.actf(sq[:, :], self.xT[:, kc, li * 512:(li + 1) * 512], AF.Square, rd=[self.r_xT[li]], wr=[r_sq])
                    k.mm(pb[:, :], self.ccc("ONES", 128), sq[:, :], start=(kc == 0), stop=(kc == KC - 1), rd=[r_sq, self.r_cc], wr=[r_pb], sig=True)
                rstd, r_rstd = tmp["rstd"], tmp["r_rstd"]
                k.ts(rstd[:, :], pb[:, :], 1.0 / D, EPS, ALU.mult, ALU.add, rd=[r_pb], wr=[r_rstd])
                k.actf(rstd[:, :], rstd[:, :], AF.Sqrt, rd=[r_rstd], wr=[r_rstd])
                k.op(k.dve, lambda e: e.reciprocal(out=rstd[:, :], in_=rstd[:, :]), rd=[r_rstd], wr=[r_rstd])
                for kc in range(KC):
                    k.stt(yf[:, kc, :], self.xT[:, kc, li * 512:(li + 1) * 512], self.ppc("FNG", kc), rstd[:, :], ALU.mult, ALU.mult,
                          rd=[self.r_xT[li], self.r_pp, r_rstd], wr=[r_yf])
                for t in range(4):
                    yt, r_yt = ytok[it % 2], r_ytok[it % 2]
                    it += 1
                    for half in range(2):
                        pt, r_pt = banks.get()
                        for kk in range(4):
                            kc = half * 4 + kk
                            k.tr(pt[:, kk * 128:(kk + 1) * 128], yf[:, kc, t * 128:(t + 1) * 128], self.ccc("IDENT", 128),
                                 rd=[r_yf, self.r_cc], wr=[r_pt], sig=(kk == 3))
                        k.cp(yt[:, half * 512:(half + 1) * 512], pt[:, :], rd=[r_pt], wr=[r_yt], E=(k.act if half else k.dve))
                    r0 = li * 512 + t * 128
                    k.dma(k.sp, self.y[r0:r0 + 128, :], yt[:, :], rd=[r_yt], wr=[r_y])


def make_in_maps(inp):
    inp = {k_: np.asarray(v) for k_, v in inp.items()}
    cc = make_cc()
    shared = {
        "cc": cc,
        "ada_w": np.ascontiguousarray(inp["ada_w"]),
        "ab_w_in": np.ascontiguousarray(inp["ab_w_in"][0]),
        "ab_w_out": np.ascontiguousarray(inp["ab_w_out"][0]),
        "pool_w": np.ascontiguousarray(inp["pool_w"][0]),
        "moe_wr": np.ascontiguousarray(np.concatenate([inp["moe_w_rg"], inp["moe_w_re"]], axis=2)),
        "moe_w_gate": np.ascontiguousarray(inp["moe_w_gate"].reshape(2, 32, D, 256)),
        "moe_w_up": np.ascontiguousarray(inp["moe_w_up"].reshape(2, 32, D, 256)),
        "moe_w_down": np.ascontiguousarray(inp["moe_w_down"].reshape(2, 32, 256, D)),
    }
    perm = _rope_perm()
    w_in1 = inp["cd_w_in"][0]
    wkr = np.zeros((D, 192), np.float32)
    wkr[:, 64:96] = w_in1[:, 1920:1952]
    wkr[:, 96 + 64:96 + 96] = w_in1[:, 1920:1952][:, perm]
    wuq = inp["mla_w_uq"][0].reshape(256, 8, 96)
    wuqs = wuq.copy()
    wuqs[:, :, 64:96] = wuq[:, :, 64:96][:, :, perm]
    shared.update({
        "cd_w_in": np.ascontiguousarray(w_in1), "cd_w_out": np.ascontiguousarray(inp["cd_w_out"][0]), "wkr": wkr,
        "w_uq": np.ascontiguousarray(wuq.reshape(256, 768)), "w_uqs": np.ascontiguousarray(wuqs.reshape(256, 768)),
        "w_ukv": np.ascontiguousarray(inp["mla_w_ukv"][0]),
    })
    maps = []
    for c in range(NCORES):
        b, j = c // 4, c % 4
        s0 = j * NL
        halo = np.zeros((16, D), np.float32)
        if j > 0:
            halo[0:8] = inp["x"][b, s0 - 8:s0]
        if j < 3:
            halo[8:16] = inp["x"][b, s0 + NL:s0 + NL + 8]
        m = dict(shared)
        m["x_own"] = np.ascontiguousarray(inp["x"][b, s0:s0 + NL])
        m["x_halo"] = halo
        m["ctx_b"] = np.ascontiguousarray(inp["ctx"][b])
        m["pp"] = make_pp(inp, c)
        m["rope"] = make_rope(j)
        m["nab_int"], m["nab_sp"] = make_na_tables(inp["na_rpb"][0], j)
        maps.append(m)
    return maps


def _rope_perm():
    p = np.arange(32)
    for g0 in (0, 16):
        p[g0:g0 + 8] = g0 + 8 + np.arange(8)
        p[g0 + 8:g0 + 16] = g0 + np.arange(8)
    return p


def make_rope(j):
    t = j * NL + np.arange(NL)
    pos = (t // 64).astype(np.float32), (t % 64).astype(np.float32)
    inv = (10000.0 ** (-np.arange(8, dtype=np.float32) / 8)).astype(np.float32)
    tab = np.zeros((32, 2, NL), np.float32)
    for gi, g0 in enumerate((0, 16)):
        ang = pos[gi][None, :] * inv[:, None]
        c, sn = np.cos(ang), np.sin(ang)
        tab[g0:g0 + 8, 0] = c
        tab[g0 + 8:g0 + 16, 0] = c
        tab[g0:g0 + 8, 1] = -sn
        tab[g0 + 8:g0 + 16, 1] = sn
    return tab


def _na_table(rpb, R0, base_row, ntile):
    qr = np.repeat(np.arange(2), 64)
    qc = np.tile(np.arange(64), 2)
    Rq = R0 + qr
    rs = np.clip(Rq - 4, 0, 120)
    cs = np.clip(qc - 8, 0, 48)
    out = np.full((ntile, 128, 8, 128), -BIG, np.float32)
    kc = np.tile(np.arange(64), 2)
    for kt in range(ntile):
        kr = base_row + 2 * kt + np.repeat(np.arange(2), 64)
        valid = ((kr[:, None] >= rs[None, :]) & (kr[:, None] < rs[None, :] + 8) & (kc[:, None] >= cs[None, :])
                 & (kc[:, None] < cs[None, :] + 16) & (kr[:, None] >= 0) & (kr[:, None] < 128))
        dr = np.clip(kr[:, None] - Rq[None, :] + 7, 0, 14)
        dc = np.clip(kc[:, None] - qc[None, :] + 15, 0, 30)
        b = rpb[:, dr, dc]
        out[kt] = np.where(valid[None], b, np.float32(-BIG)).transpose(1, 0, 2)
    return out


def make_na_tables(rpb, j):
    nab_int = _na_table(rpb, 64, 60, 5)
    sp = []
    for qb, off in ((0, 4), (1, 4), (14, 6), (15, 6)):
        R0 = 32 * j + 2 * qb
        sp.append(_na_table(rpb, R0, R0 - off, 6))
    return nab_int, np.stack(sp)


_PROG = {}


def run_prog(inp, stage=99, debug=False, l1only=False):
    key = (stage, debug, l1only)
    if key not in _PROG:
        _PROG[key] = Prog(stage=stage, debug=debug, l1only=l1only)
    prog = _PROG[key]
    maps = make_in_maps(inp)
    res = run_bass_kernel_spmd(prog.nc, maps, core_ids=list(range(NCORES)))
    return prog, res


def kernel(**inputs):
    prog, res = run_prog(inputs)
    out = np.zeros((2, 8192, D), np.float32)
    for c in range(NCORES):
        b, j = c // 4, c % 4
        out[b, j * NL:(j + 1) * NL] = res.results[c]["y"]
    return out
```

```python
import bisect
import contextlib
import numpy as np
import concourse.bass as bass
import concourse.mybir as mybir
from concourse.bass_utils import run_bass_kernel_spmd

F32 = mybir.dt.float32
BF16 = mybir.dt.bfloat16
AF = mybir.ActivationFunctionType
ALU = mybir.AluOpType
AX = mybir.AxisListType

NCORES = 8
D = 1024
KC = 8
NL = 2048
NCX = 256
NT = NCX + NL
EPS = 1e-6
BIG = 30000.0


class Res:
    __slots__ = ("name", "w", "r", "dsem", "dcnt")

    def __init__(self, name):
        self.name = name
        self.w = None
        self.r = []
        self.dsem = None
        self.dcnt = 0


class Eng:
    def __init__(self, k, name, h):
        self.k = k
        self.name = name
        self.h = h
        self.sem = k.new_sem("e_" + name)
        self.tick = 0
        self.nsig = 0
        self.sig_ticks = []
        self.sig_vals = []
        self.known = {}


class K:
    def __init__(self):
        self.nc = bass.Bass("TRN2", target_bir_lowering=False)
        self.es = contextlib.ExitStack()
        self.nsem = 0
        nc = self.nc
        self.pe = Eng(self, "pe", nc.tensor)
        self.act = Eng(self, "act", nc.scalar)
        self.dve = Eng(self, "dve", nc.vector)
        self.pool = Eng(self, "pool", nc.gpsimd)
        self.sp = Eng(self, "sp", nc.sync)
        self.engs = [self.pe, self.act, self.dve, self.pool, self.sp]
        self.dma_evs = []
        self.rr = 0

    def new_sem(self, name):
        self.nsem += 1
        return self.es.enter_context(self.nc.semaphore(name + "_%d" % self.nsem))

    def R(self, name="r"):
        return Res(name)

    def Rs(self, n, name="r"):
        return [Res(name + str(i)) for i in range(n)]

    def _resolve(self, ev):
        if ev[0] == "d":
            return ev[1], ev[2]
        E, tick = ev[1], ev[2]
        i = bisect.bisect_left(E.sig_ticks, tick)
        assert i < len(E.sig_ticks), "unsignaled dependency on %s tick %d" % (E.name, tick)
        return E.sem, E.sig_vals[i]

    def _wait(self, E, ev, raw):
        if ev[0] == "e" and ev[1] is E and (not raw or E is self.pe):
            return
        sem, val = self._resolve(ev)
        key = id(sem)
        if E.known.get(key, 0) >= val:
            return
        E.h.wait_ge(sem, val)
        E.known[key] = val

    def _deps(self, E, rd, wr):
        for r in rd:
            if r.w is not None:
                self._wait(E, r.w, True)
        for w in wr:
            if w.w is not None:
                self._wait(E, w.w, False)
            for ev in w.r:
                self._wait(E, ev, False)

    def _record(self, ev, rd, wr):
        for w in wr:
            w.w = ev
            w.r = []
        for r in rd:
            if ev[0] == "e":
                r.r = [e for e in r.r if not (e[0] == "e" and e[1] is ev[1])]
            r.r.append(ev)

    def op(self, E, fn, rd=(), wr=(), sig=True):
        self._deps(E, rd, wr)
        inst = fn(E.h)
        E.tick += 1
        if sig:
            inst.then_inc(E.sem, 1)
            E.nsig += 1
            E.sig_ticks.append(E.tick)
            E.sig_vals.append(E.nsig)
        self._record(("e", E, E.tick), rd, wr)
        return inst

    def dma(self, Q, out, in_, rd=(), wr=(), **kw):
        self._deps(Q, rd, wr)
        prim = wr[0] if wr else rd[0]
        if prim.dsem is None:
            prim.dsem = self.new_sem("d_" + prim.name)
        prim.dcnt += 1
        Q.h.dma_start(out=out, in_=in_, **kw).then_inc(prim.dsem, 16)
        ev = ("d", prim.dsem, 16 * prim.dcnt)
        self._record(ev, rd, wr)
        self.dma_evs.append(ev)
        return ev

    def collective(self, fn, rd, wr):
        Q = self.pool
        self._deps(Q, rd, wr)
        prim = wr[0]
        if prim.dsem is None:
            prim.dsem = self.new_sem("c_" + prim.name)
        assert prim.dcnt == 0
        prim.dcnt = 1
        fn(Q.h).then_inc(prim.dsem, 1)
        ev = ("d", prim.dsem, 1)
        self._record(ev, rd, wr)
        self.dma_evs.append(ev)
        prim.dcnt = 0
        prim.dsem = None

    def barrier(self):
        evs = list(self.dma_evs)
        for E in self.engs:
            if E.nsig:
                evs.append(("e", E, E.sig_ticks[-1]))
        for E in self.engs:
            for ev in evs:
                if ev[0] == "e" and ev[1] is E:
                    continue
                self._wait(E, ev, True)
        self.dma_evs = []

    def dq(self):
        self.rr += 1
        return self.sp

    def mm(self, out, lhsT, rhs, start, stop, rd, wr, sig=None):
        if sig is None:
            sig = stop
        return self.op(self.pe, lambda e: e.matmul(out, lhsT, rhs, start=start, stop=stop), rd, wr, sig)

    def tr(self, out, in_, ident, rd, wr, sig=True):
        return self.op(self.pe, lambda e: e.transpose(out, in_, ident), rd, wr, sig)

    def actf(self, out, in_, func, rd, wr, bias=None, scale=None, E=None):
        kw = {}
        if bias is not None:
            kw["bias"] = bias
        if scale is not None:
            kw["scale"] = scale
        return self.op(E or self.act, lambda e: e.activation(out=out, in_=in_, func=func, **kw), rd, wr)

    def tt(self, out, in0, in1, op, rd, wr, E=None):
        return self.op(E or self.dve, lambda e: e.tensor_tensor(out=out, in0=in0, in1=in1, op=op), rd, wr)

    def ts(self, out, in0, s1, s2, op0, op1, rd, wr, E=None):
        if s2 is None:
            return self.op(E or self.dve, lambda e: e.tensor_scalar(out=out, in0=in0, scalar1=s1, scalar2=None, op0=op0), rd, wr)
        return self.op(E or self.dve, lambda e: e.tensor_scalar(out=out, in0=in0, scalar1=s1, scalar2=s2, op0=op0, op1=op1), rd, wr)

    def stt(self, out, in0, scalar, in1, op0, op1, rd, wr):
        return self.op(self.dve, lambda e: e.scalar_tensor_tensor(out=out, in0=in0, scalar=scalar, in1=in1, op0=op0, op1=op1), rd, wr)

    def cp(self, out, in_, rd, wr, E=None):
        E = E or self.dve
        if E is self.act:
            return self.op(E, lambda e: e.activation(out=out, in_=in_, func=AF.Copy), rd, wr)
        return self.op(E, lambda e: e.tensor_copy(out=out, in_=in_), rd, wr)


class Banks:
    def __init__(self, tens, res, idx):
        self.t = [tens[i] for i in idx]
        self.r = [res[i] for i in idx]
        self.i = 0

    def get(self):
        j = self.i % len(self.t)
        self.i += 1
        return self.t[j], self.r[j]


PP = {}
_off = 0
for _n, _s in [("ADA_B", 96), ("N1G", 16), ("N2G", 16), ("FNG", 8), ("LBL", 16), ("ONG", 1), ("PSC", 4),
               ("QNG", 2), ("KVNG", 1), ("CV", 16), ("FLAGL", 1), ("FLAGR", 1), ("SELF", 4), ("SELB", 4), ("SELT", 4), ("SELBT", 4),
               ("BR", 72), ("TABL", 32), ("TABR", 32), ("CTABL", 32), ("CTABR", 32)]:
    PP[_n] = _off
    _off += _s
NPP = _off

CC = {}
_off = 0
for _n, _s in [("IDENT", 128), ("ONES", 512), ("RESET", 512), ("MASKF", 128), ("MASKB", 128)]:
    CC[_n] = _off
    _off += _s
NCC = _off
POOL_W = (2, 4, 8, 16)


def _pool_inv_count(n, win, t):
    lo = np.clip(t - win // 2, 0, n)
    hi = np.clip(t + win - win // 2, 0, n)
    return 1.0 / (hi - lo).astype(np.float32)


def make_cc():
    cc = np.zeros((128, NCC), np.float32)
    cc[:, CC["IDENT"]:CC["IDENT"] + 128] = np.eye(128, dtype=np.float32)
    cc[:, CC["ONES"]:CC["ONES"] + 512] = 1.0
    r = np.ones(512, np.float32)
    r[::64] = 0.0
    cc[:, CC["RESET"]:CC["RESET"] + 512] = r[None, :]
    s = np.arange(128)[:, None]
    t = np.arange(128)[None, :]
    same = (s // 64) == (t // 64)
    mf = (same & (s <= t)).astype(np.float32)
    mb = (same & (s >= t)).astype(np.float32)
    cc[:, CC["MASKF"]:CC["MASKF"] + 128] = mf
    cc[:, CC["MASKB"]:CC["MASKB"] + 128] = mb
    return cc


def make_pp(inp, core):
    b, j = core // 4, core % 4
    pp = np.zeros((128, NPP), np.float32)

    def fm(v, nch):
        return np.ascontiguousarray(v.reshape(nch, 128).T)

    for l in range(2):
        pp[:, PP["ADA_B"] + l * 48:PP["ADA_B"] + (l + 1) * 48] = fm(inp["ada_b"][l], 48)
        pp[:, PP["N1G"] + l * 8:PP["N1G"] + (l + 1) * 8] = fm(inp["norm1_g"][l], 8)
        pp[:, PP["N2G"] + l * 8:PP["N2G"] + (l + 1) * 8] = fm(inp["norm2_g"][l], 8)
        br = np.concatenate([inp["moe_b_rg"][l], inp["moe_b_re"][l]])
        pp[:, PP["BR"] + l * 36:PP["BR"] + (l + 1) * 36] = br[None, :]
    pp[:, PP["FNG"]:PP["FNG"] + 8] = fm(inp["final_norm_g"], 8)
    lbl = inp["hgrn_lb_logits"]
    for d in range(2):
        for s in range(2):
            pp[:, PP["LBL"] + d * 8 + s * 4:PP["LBL"] + d * 8 + s * 4 + 4] = fm(lbl[d, s], 4)
    pp[:, PP["ONG"]] = inp["hgrn_onorm_g"][0]
    pp[:, PP["PSC"]:PP["PSC"] + 4] = fm(inp["pool_scale"][0], 4)
    pp[:, PP["QNG"]:PP["QNG"] + 2] = fm(inp["mla_q_norm_g"][0], 2)
    pp[:, PP["KVNG"]] = inp["mla_kv_norm_g"][0]
    cv = np.stack([fm(inp["c"][b], 8), fm(inp["c_ctx"], 8)], axis=2)
    pp[:, PP["CV"]:PP["CV"] + 16] = cv.reshape(128, 16)
    pp[:, PP["FLAGL"]] = 0.0 if j == 0 else 1.0
    pp[:, PP["FLAGR"]] = 0.0 if j == 3 else 1.0
    pp[:, PP["SELF"] + j] = 1.0
    pp[:, PP["SELB"] + j] = 1.0
    if j > 0:
        pp[:, PP["SELT"] + j - 1] = 1.0
    if j < 3:
        pp[:, PP["SELBT"] + j + 1] = 1.0
    n_lat, n_ctx = 8192, 256
    for gi, win in enumerate(POOL_W):
        t0 = j * NL + np.arange(8)
        t1 = j * NL + NL - 8 + np.arange(8)
        pp[:, PP["TABL"] + gi * 8:PP["TABL"] + gi * 8 + 8] = _pool_inv_count(n_lat, win, t0)[None, :]
        pp[:, PP["TABR"] + gi * 8:PP["TABR"] + gi * 8 + 8] = _pool_inv_count(n_lat, win, t1)[None, :]
        pp[:, PP["CTABL"] + gi * 8:PP["CTABL"] + gi * 8 + 8] = _pool_inv_count(n_ctx, win, np.arange(8))[None, :]
        pp[:, PP["CTABR"] + gi * 8:PP["CTABR"] + gi * 8 + 8] = _pool_inv_count(n_ctx, win, n_ctx - 8 + np.arange(8))[None, :]
    return pp


W_IN_COLS = {"q": 0, "ff": 512, "fb": 1024, "i": 1536, "g": 2048, "pool": 2560}


class Prog:
    def __init__(self, stage=99, debug=False, l1only=False):
        self.l1only = l1only
        self.stage = stage
        self.debug = debug
        self.k = K()
        self.nc = self.k.nc
        self.dbg_outs = []
        self.out_res = []
        self.build()

    def din(self, name, shape, dt=F32):
        return self.nc.dram_tensor(name, list(shape), dt, kind="ExternalInput").ap()

    def sb(self, es, name, shape, dt=F32):
        self._nsb = getattr(self, "_nsb", 0) + 1
        return es.enter_context(self.nc.sbuf_tensor("%s_%d" % (name, self._nsb), list(shape), dt))

    def dbg(self, name, ap, res, shape):
        if not self.debug:
            return
        k = self.k
        o = self.nc.dram_tensor("dbg_" + name, list(shape), F32, kind="ExternalOutput").ap()
        r = k.R("dbg_" + name)
        k.dma(k.pool, o, ap, rd=[res] if isinstance(res, Res) else list(res), wr=[r])
        self.out_res.append(r)
        self.dbg_outs.append("dbg_" + name)

    def ppc(self, name, off=0, n=1):
        c = PP[name] + off
        return self.pp[:, c:c + n]

    def ccc(self, name, n, off=0):
        c = CC[name] + off
        return self.cc[:, c:c + n]

    def build(self):
        k, nc = self.k, self.nc
        es = k.es
        self.x_own = self.din("x_own", [NL, D])
        self.x_halo = self.din("x_halo", [16, D])
        self.ctx_b = self.din("ctx_b", [NCX, D])
        self.pp_d = self.din("pp", [128, NPP])
        self.cc_d = self.din("cc", [128, NCC])
        self.ada_w = self.din("ada_w", [2, D, 6 * D])
        self.ab_w_in = self.din("ab_w_in", [D, 3072])
        self.ab_w_out = self.din("ab_w_out", [D, D])
        self.pool_w = self.din("pool_w", [4, 128, 128])
        self.moe_wr = self.din("moe_wr", [2, D, 36])
        self.moe_w_gate = self.din("moe_w_gate", [2, 32, D, 256])
        self.moe_w_up = self.din("moe_w_up", [2, 32, D, 256])
        self.moe_w_down = self.din("moe_w_down", [2, 32, 256, D])
        self.cd_w_in = self.din("cd_w_in", [D, 1952])
        self.cd_w_out = self.din("cd_w_out", [D, D])
        self.wkr_d = self.din("wkr", [D, 192])
        self.rope_d = self.din("rope", [32, 2, NL])
        self.nab_int = self.din("nab_int", [5, 128, 8, 128])
        self.nab_sp = self.din("nab_sp", [4, 6, 128, 8, 128])
        self.w_uq_d = self.din("w_uq", [256, 768])
        self.w_uqs_d = self.din("w_uqs", [256, 768])
        self.w_ukv_d = self.din("w_ukv", [128, 1024])
        self.y = nc.dram_tensor("y", [NL, D], F32, kind="ExternalOutput").ap()

        self.pp = self.sb(es, "pp_sb", [128, NPP])
        self.cc = self.sb(es, "cc_sb", [128, NCC])
        self.identb = self.sb(es, "identb", [128, 128], BF16)
        self.onesb = self.sb(es, "onesb", [128, 128], BF16)
        self.xT = self.sb(es, "xT", [128, KC, NL])
        self.xcT = self.sb(es, "xcT", [128, KC, NCX])
        self.mod = [self.sb(es, "mod%d" % l, [128, 48, 2]) for l in range(2)]
        self.gs1 = [self.sb(es, "gs1_%d" % l, [128, 8, 2]) for l in range(2)]
        self.gs2 = [self.sb(es, "gs2_%d" % l, [128, 8, 2]) for l in range(2)]
        self.lb = self.sb(es, "lb", [128, 8])
        self.oml = self.sb(es, "oml", [128, 8])
        self.r_pp, self.r_cc, self.r_const = k.R("pp"), k.R("cc"), k.R("const")
        self.r_mod = k.R("mod")
        self.r_xT = k.Rs(4, "xT")
        self.r_xcT = k.R("xcT")
        self.ps = [es.enter_context(nc.psum_tensor("ps%d" % i, [128, 512], F32)) for i in range(8)]
        self.r_ps = k.Rs(8, "ps")

        k.dma(k.sp, self.pp[:], self.pp_d[:, :], wr=[self.r_pp])
        k.dma(k.sp, self.cc[:], self.cc_d[:, :], wr=[self.r_cc])
        k.cp(self.identb[:], self.ccc("IDENT", 128), rd=[self.r_cc], wr=[self.r_const])
        k.cp(self.onesb[:], self.ccc("ONES", 128), rd=[self.r_cc], wr=[self.r_const])

        self.prologue()
        if self.l1only:
            self.blocks = [(0, NCX, True, -1)] + [(NCX + 512 * i, 512, False, i) for i in range(4)]
            with contextlib.ExitStack() as es0:
                srcs = [(self.ctx_b[:, :], NCX, lambda kc: self.xcT[:, kc, :], self.r_xcT)]
                for i in range(4):
                    srcs.append((self.x_own[i * 512:(i + 1) * 512, :], 512,
                                 (lambda kc, i=i: self.xT[:, kc, i * 512:(i + 1) * 512]), self.r_xT[i]))
                self.load_T(es0, srcs)
        if self.stage >= 1 and not self.l1only:
            self.layer0()
        if self.stage >= 5 and not self.l1only:
            self.moe(0)
        if self.stage == 5 or self.stage == 6:
            self.dbg("x1", self.xT[:, 0, :], self.r_xT, [128, NL])
            self.dbg("xc1", self.xcT[:, 0, :], self.r_xcT, [128, NCX])
        if self.stage >= 7:
            self.layer1()
        if self.stage in (8, 9):
            self.dbg("xmix1", self.xT[:, 0, :], self.r_xT, [128, NL])
        if self.stage >= 10:
            self.moe(1)
            self.final_out()
        self.finish()

    def finish(self):
        k = self.k
        for r in self.out_res:
            if r.w is not None:
                k._wait(k.sp, r.w, True)
        k.es.close()

    def prologue(self):
        k = self.k
        with contextlib.ExitStack() as es:
            sc = self.sb(es, "silu_c", [128, 16])
            r_sc = k.R("sc")
            k.actf(sc[:], self.ppc("CV", 0, 16), AF.Silu, rd=[self.r_pp], wr=[r_sc])
            NB = 3
            wb = [self.sb(es, "adaw%d" % i, [128, KC, 512]) for i in range(NB)]
            r_wb = k.Rs(NB, "adaw")
            it = 0
            for l in range(2):
                pm, r_pm = self.ps[l], self.r_ps[l]
                for nb in range(12):
                    b = it % NB
                    it += 1
                    src = self.ada_w[l, :, nb * 512:(nb + 1) * 512].rearrange("(kc p) n -> p kc n", p=128)
                    k.dma(k.sp, wb[b][:], src, wr=[r_wb[b]])
                    for c4 in range(4):
                        col = (nb * 4 + c4) * 2
                        for kc in range(KC):
                            k.mm(pm[:, col:col + 2], wb[b][:, kc, c4 * 128:(c4 + 1) * 128], sc[:, kc * 2:kc * 2 + 2],
                                 start=(kc == 0), stop=(kc == KC - 1), rd=[r_wb[b], r_sc], wr=[r_pm])
                mod = self.mod[l]
                k.tt(mod[:], pm[:, 0:96].rearrange("p (n v) -> p n v", v=2),
                     self.ppc("ADA_B", l * 48, 48).unsqueeze(2).to_broadcast([128, 48, 2]), ALU.add,
                     rd=[r_pm, self.r_pp], wr=[self.r_mod])
                for (gs, i_sc, gname) in ((self.gs1[l], 1, "N1G"), (self.gs2[l], 4, "N2G")):
                    k.ts(gs[:], mod[:, i_sc * 8:(i_sc + 1) * 8, :], 1.0, None, ALU.add, None, rd=[self.r_mod], wr=[self.r_mod])
                    k.tt(gs[:], gs[:], self.ppc(gname, l * 8, 8).unsqueeze(2).to_broadcast([128, 8, 2]), ALU.mult,
                         rd=[self.r_mod, self.r_pp], wr=[self.r_mod])
            lbl = self.ppc("LBL", 0, 16).rearrange("p (d s h) -> p d s h", d=2, s=2)
            lbv = self.lb[:].rearrange("p (d h) -> p d h", d=2)
            k.tt(lbv, lbl[:, :, 0, :], lbl[:, :, 1, :], ALU.subtract, rd=[self.r_pp], wr=[self.r_mod])
            k.actf(self.lb[:], self.lb[:], AF.Sigmoid, rd=[self.r_mod], wr=[self.r_mod])
            k.ts(self.oml[:], self.lb[:], -1.0, 1.0, ALU.mult, ALU.add, rd=[self.r_mod], wr=[self.r_mod])
            self.dbg("mod0", self.mod[0][:].rearrange("p n v -> p (n v)"), self.r_mod, [128, 96])
            self.dbg("mod1", self.mod[1][:].rearrange("p n v -> p (n v)"), self.r_mod, [128, 96])
            self.dbg("lb", self.lb[:], self.r_mod, [128, 8])
            k.barrier()

    def modv(self, l, i, kc, v):
        return self.mod[l][:, i * 8 + kc, v:v + 1]

    def load_T(self, es_outer, srcs):
        k = self.k
        with contextlib.ExitStack() as es:
            xs = [self.sb(es, "xstage%d" % i, [128, 4, D]) for i in range(2)]
            r_xs = k.Rs(2, "xstage")
            banks = Banks(self.ps, self.r_ps, [0, 1, 2, 3])
            for it, (src, ntok, dst_fn, dst_res) in enumerate(srcs):
                b = it % 2
                if ntok >= 128:
                    nt = ntok // 128
                    k.dma(k.sp, xs[b][:, 0:nt, :], src.rearrange("(t p) f -> p t f", p=128), wr=[r_xs[b]])
                    pt = 128
                else:
                    nt = 1
                    pt = ntok
                    k.dma(k.sp, xs[b][0:ntok, 0, :], src, wr=[r_xs[b]])
                for kc in range(KC):
                    pb, r_pb = banks.get()
                    for t in range(nt):
                        k.tr(pb[:, t * pt:(t + 1) * pt], xs[b][0:pt, t, kc * 128:(kc + 1) * 128],
                             self.ccc("IDENT", pt)[0:pt, :], rd=[r_xs[b], self.r_cc], wr=[r_pb], sig=(t == nt - 1))
                    k.cp(dst_fn(kc), pb[:, 0:nt * pt], rd=[r_pb], wr=[dst_res], E=(k.act if kc % 2 else k.dve))
            k.barrier()

    def norm_mod(self, tmp, src_fn, n, gs_fn, sh_fn, out_fn, rd, wr, f32_out_fn=None):
        k = self.k
        pb, r_pb = tmp["banks"].get()
        for kc in range(KC):
            i = tmp["i"] % 3
            tmp["i"] += 1
            sq, r_sq = tmp["sq"][i], tmp["r_sq"][i]
            k.actf(sq[:, 0:n], src_fn(kc), AF.Square, rd=rd, wr=[r_sq])
            k.mm(pb[:, 0:n], self.ccc("ONES", 128), sq[:, 0:n], start=(kc == 0), stop=(kc == KC - 1),
                 rd=[r_sq, self.r_cc], wr=[r_pb], sig=True)
        rstd, r_rstd = tmp["rstd"], tmp["r_rstd"]
        k.ts(rstd[:, 0:n], pb[:, 0:n], 1.0 / D, EPS, ALU.mult, ALU.add, rd=[r_pb], wr=[r_rstd])
        k.actf(rstd[:, 0:n], rstd[:, 0:n], AF.Sqrt, rd=[r_rstd], wr=[r_rstd])
        k.op(k.dve, lambda e: e.reciprocal(out=rstd[:, 0:n], in_=rstd[:, 0:n]), rd=[r_rstd], wr=[r_rstd])
        for kc in range(KC):
            i = tmp["i"] % 3
            tmp["i"] += 1
            sq, r_sq = tmp["sq"][i], tmp["r_sq"][i]
            k.stt(sq[:, 0:n], src_fn(kc), gs_fn(kc), rstd[:, 0:n], ALU.mult, ALU.mult,
                  rd=list(rd) + [r_rstd, self.r_mod], wr=[r_sq])
            k.actf(out_fn(kc), sq[:, 0:n], AF.Identity, rd=[r_sq, self.r_mod], wr=wr, bias=sh_fn(kc))
            if f32_out_fn is not None:
                k.ts(f32_out_fn(kc), sq[:, 0:n], sh_fn(kc), None, ALU.add, None, rd=[r_sq, self.r_mod], wr=wr)

    def norm_tmp(self, es, banks_idx=(4, 5)):
        k = self.k
        return {"banks": Banks(self.ps, self.r_ps, list(banks_idx)), "i": 0,
                "sq": [self.sb(es, "nsq%d" % i, [128, 512]) for i in range(3)], "r_sq": k.Rs(3, "nsq"),
                "rstd": self.sb(es, "nrstd", [128, 512]), "r_rstd": k.R("nrstd")}

    def load_w(self, dst, src, res):
        self.k.dma(self.k.pool, dst, src, wr=[res])

    def layer0(self):
        k = self.k
        with contextlib.ExitStack() as es:
            self.hT = self.sb(es, "hT", [128, KC, NT], BF16)
            self.hh = self.sb(es, "hh", [128, KC, 16], BF16)
            self.r_hT = k.Rs(5, "hT")
            self.r_hh = k.R("hh")
            xh = self.sb(es, "xh", [128, KC, 16])
            r_xh = k.R("xh")
            self.blocks = [(0, NCX, True, -1)] + [(NCX + 512 * i, 512, False, i) for i in range(4)]
            srcs = [(self.ctx_b[:, :], NCX, lambda kc: self.xcT[:, kc, :], self.r_xcT),
                    (self.x_halo[:, :], 16, lambda kc: xh[:, kc, :], r_xh)]
            for i in range(4):
                srcs.append((self.x_own[i * 512:(i + 1) * 512, :], 512,
                             (lambda kc, i=i: self.xT[:, kc, i * 512:(i + 1) * 512]), self.r_xT[i]))
            self.load_T(es, srcs)
            with contextlib.ExitStack() as es2:
                tmp = self.norm_tmp(es2)
                for bi, (c0, n, is_ctx, li) in enumerate(self.blocks):
                    v = 1 if is_ctx else 0
                    src_fn = (lambda kc: self.xcT[:, kc, :]) if is_ctx else (lambda kc, li=li: self.xT[:, kc, li * 512:(li + 1) * 512])
                    self.norm_mod(tmp, src_fn, n, lambda kc, v=v: self.gs1[0][:, kc, v:v + 1],
                                  lambda kc, v=v: self.modv(0, 0, kc, v),
                                  lambda kc, c0=c0, n=n: self.hT[:, kc, c0:c0 + n],
                                  rd=[self.r_xcT if is_ctx else self.r_xT[li]], wr=[self.r_hT[bi]])
                self.norm_mod(tmp, lambda kc: xh[:, kc, :], 16, lambda kc: self.gs1[0][:, kc, 0:1],
                              lambda kc: self.modv(0, 0, kc, 0), lambda kc: self.hh[:, kc, :],
                              rd=[r_xh], wr=[self.r_hh])
                k.barrier()
            self.dbg("hT", self.hT[:, 0, :], self.r_hT, [128, NT])
            if self.stage >= 2:
                self.pool_pass()
            if self.stage >= 3:
                self.hgrn()

    def x_dst(self, is_ctx, li, m, n):
        if is_ctx:
            return self.xcT[:, m, 0:n], self.r_xcT, 1
        return self.xT[:, m, li * 512:li * 512 + n], self.r_xT[li], 0

    def pool_pass(self):
        k = self.k
        with contextlib.ExitStack() as es:
            wp = self.sb(es, "w_pool", [128, KC, 512], BF16)
            wo = self.sb(es, "w_out2", [128, 4, D], BF16)
            pw = self.sb(es, "poolw", [128, 4, 128], BF16)
            r_w = k.R("poolwts")
            self.load_w(wp[:], self.ab_w_in[:, 2560:3072].rearrange("(kc p) n -> p kc n", p=128), r_w)
            self.load_w(wo[:], self.ab_w_out[512:1024, :].rearrange("(kc p) n -> p kc n", p=128), r_w)
            self.load_w(pw[:], self.pool_w.rearrange("g c d -> c g d"), r_w)
            W = 528
            up = self.sb(es, "up", [128, 4, W])
            A = self.sb(es, "pA", [128, 4, W])
            B = self.sb(es, "pB", [128, 4, W])
            S16 = self.sb(es, "pS16", [128, 512])
            t8 = self.sb(es, "pt8", [128, 8])
            diff = self.sb(es, "pdiff", [128, 4, 512], BF16)
            pout = self.sb(es, "ppout", [128, 4, 512], BF16)
            r_up, r_A, r_B, r_S, r_t8, r_diff, r_pout = (k.R(s) for s in ("up", "pA", "pB", "pS", "pt8", "pdiff", "ppout"))
            banks = Banks(self.ps, self.r_ps, [0, 1, 2, 3, 4, 5, 6, 7])
            for bi, (c0, n, is_ctx, li) in enumerate(self.blocks):
                Wb = n + 16
                for gi in range(4):
                    pb, r_pb = banks.get()
                    for kc in range(KC):
                        k.mm(pb[:, 0:n], wp[:, kc, gi * 128:(gi + 1) * 128], self.hT[:, kc, c0:c0 + n],
                             start=(kc == 0), stop=(kc == KC - 1), rd=[r_w, self.r_hT[bi]], wr=[r_pb])
                    k.cp(up[:, gi, 8:8 + n], pb[:, 0:n], rd=[r_pb], wr=[r_up], E=k.act)
                if is_ctx:
                    k.op(k.pool, lambda e: e.memset(up[:, :, 0:8], 0.0), wr=[r_up])
                    k.op(k.pool, lambda e: e.memset(up[:, :, 8 + n:16 + n], 0.0), wr=[r_up])
                else:
                    ph, r_ph = banks.get()
                    for gi in range(4):
                        for side in range(2):
                            if side == 0:
                                src_fn = (lambda kc: self.hh[:, kc, 0:8]) if li == 0 else (lambda kc: self.hT[:, kc, c0 - 8:c0])
                                rr = self.r_hh if li == 0 else self.r_hT[bi - 1]
                            else:
                                src_fn = (lambda kc: self.hh[:, kc, 8:16]) if li == 3 else (lambda kc: self.hT[:, kc, c0 + n:c0 + n + 8])
                                rr = self.r_hh if li == 3 else self.r_hT[bi + 1]
                            col = (gi * 2 + side) * 8
                            for kc in range(KC):
                                k.mm(ph[:, col:col + 8], wp[:, kc, gi * 128:(gi + 1) * 128], src_fn(kc),
                                     start=(kc == 0), stop=(kc == KC - 1), rd=[r_w, rr], wr=[r_ph])
                    phv = ph[:, 0:64].rearrange("p (g s c) -> p g s c", g=4, s=2)
                    if li == 0:
                        k.ts(up[:, :, 0:8], phv[:, :, 0, :], self.ppc("FLAGL"), None, ALU.mult, None, rd=[r_ph, self.r_pp], wr=[r_up])
                    else:
                        k.cp(up[:, :, 0:8], phv[:, :, 0, :], rd=[r_ph], wr=[r_up])
                    if li == 3:
                        k.ts(up[:, :, 8 + n:16 + n], phv[:, :, 1, :], self.ppc("FLAGR"), None, ALU.mult, None, rd=[r_ph, self.r_pp], wr=[r_up])
                    else:
                        k.cp(up[:, :, 8 + n:16 + n], phv[:, :, 1, :], rd=[r_ph], wr=[r_up])
                k.tt(A[:, :, 1:Wb], up[:, :, 0:Wb - 1], up[:, :, 1:Wb], ALU.add, rd=[r_up], wr=[r_A])
                k.tt(B[:, 1:4, 2:Wb - 2], A[:, 1:4, 1:Wb - 3], A[:, 1:4, 3:Wb - 1], ALU.add, rd=[r_A], wr=[r_B])
                k.tt(A[:, 2:4, 4:Wb - 4], B[:, 2:4, 2:Wb - 6], B[:, 2:4, 6:Wb - 2], ALU.add, rd=[r_B], wr=[r_A])
                k.tt(S16[:, 0:n], A[:, 3, 4:4 + n], A[:, 3, 12:12 + n], ALU.add, rd=[r_A], wr=[r_S])
                sums = [A[:, 0, 8:8 + n], B[:, 1, 8:8 + n], A[:, 2, 8:8 + n], S16[:, 0:n]]
                for gi, win in enumerate(POOL_W):
                    k.stt(diff[:, gi, 0:n], sums[gi], 1.0 / win, up[:, gi, 8:8 + n], ALU.mult, ALU.subtract,
                          rd=[r_A, r_B, r_S, r_up], wr=[r_diff])
                edges = []
                if is_ctx:
                    edges = [(0, "CTABL"), (n - 8, "CTABR")]
                elif li == 0:
                    edges = [(0, "TABL")]
                elif li == 3:
                    edges = [(n - 8, "TABR")]
                for (e0, tab) in edges:
                    for gi in range(4):
                        k.tt(t8[:], sums[gi][:, e0:e0 + 8], self.ppc(tab, gi * 8, 8), ALU.mult, rd=[r_A, r_B, r_S, self.r_pp], wr=[r_t8])
                        k.tt(diff[:, gi, e0:e0 + 8], t8[:], up[:, gi, 8 + e0:16 + e0], ALU.subtract, rd=[r_t8, r_up], wr=[r_diff])
                for gi in range(4):
                    pb, r_pb = banks.get()
                    k.mm(pb[:, 0:n], pw[:, gi, :], diff[:, gi, 0:n], start=True, stop=True, rd=[r_w, r_diff], wr=[r_pb])
                    k.actf(pout[:, gi, 0:n], pb[:, 0:n], AF.Copy, rd=[r_pb, self.r_pp], wr=[r_pout], scale=self.ppc("PSC", gi))
                for m in range(KC):
                    pb, r_pb = banks.get()
                    for gi in range(4):
                        k.mm(pb[:, 0:n], wo[:, gi, m * 128:(m + 1) * 128], pout[:, gi, 0:n], start=(gi == 0), stop=(gi == 3),
                             rd=[r_w, r_pout], wr=[r_pb])
                    dst, r_dst, v = self.x_dst(is_ctx, li, m, n)
                    k.stt(dst, pb[:, 0:n], self.modv(0, 2, m, v), dst, ALU.mult, ALU.add, rd=[r_pb, self.r_mod, r_dst], wr=[r_dst])
            if self.stage == 2:
                self.dbg("pout", pout[:, :, :].rearrange("p g n -> p (g n)"), r_pout, [128, 2048])
            k.barrier()

    def hgrn(self):
        k = self.k
        with contextlib.ExitStack() as es:
            self.osum = self.sb(es, "osum", [128, 4, NT], BF16)
            self.r_osum = k.Rs(5, "osum")
            S32 = self.sb(es, "S32", [128, 8, 128])
            Sb = self.sb(es, "Sb", [128, 8, 128], BF16)
            Sd = self.sb(es, "Sd", [128, 4, 128])
            Sfin = self.sb(es, "Sfin", [128, 8, 128])
            xb = self.sb(es, "xb", [128, 8, 129])
            r_S, r_Sb, r_Sd = k.Rs(8, "S32"), k.Rs(8, "Sb"), k.Rs(8, "Sd")
            r_Sfin, r_xb = k.R("Sfin"), k.R("xb")
            wA = self.sb(es, "wA", [128, KC, 512], BF16)
            wB = self.sb(es, "wB", [128, KC, 512], BF16)
            wC = self.sb(es, "wC", [128, KC, 512], BF16)
            r_wA, r_wB, r_wC = k.R("wA"), k.R("wB"), k.R("wC")

            def wsrc(name):
                c = W_IN_COLS[name]
                return self.ab_w_in[:, c:c + 512].rearrange("(kc p) n -> p kc n", p=128)

            with contextlib.ExitStack() as es2:
                T = [self.sb(es2, "hT%d" % i, [128, 512]) for i in range(4)]
                r_T = k.Rs(4, "hTt")
                Qt = [self.sb(es2, "Qt%d" % i, [128, 512], BF16) for i in range(4)]
                Kt = [self.sb(es2, "Kt%d" % i, [128, 512], BF16) for i in range(4)]
                Ktok = [self.sb(es2, "Ktok%d" % i, [128, 4, 128], BF16) for i in range(4)]
                attm = [self.sb(es2, "attm%d" % i, [128, 512], BF16) for i in range(4)]
                dch = [self.sb(es2, "dch%d" % i, [128, 8]) for i in range(4)]
                r_Qt, r_Kt, r_Ktok, r_attm, r_dch = (k.Rs(4, s_) for s_ in ("Qt", "Kt", "Ktok", "attm", "dch"))
                Vtok = self.sb(es2, "Vtok", [128, 4, 512], BF16)
                r_V = k.R("Vtok")
                dend = self.sb(es2, "dend", [128, 1])
                r_dend = k.R("dend")
                gb = Banks(self.ps, self.r_ps, [6, 7])

                def proj_v(bi, c0, n):
                    for t in range(n // 128):
                        pv, r_pv = gb.get()
                        for kc in range(KC):
                            k.mm(pv[:, :], self.hT[:, kc, c0 + t * 128:c0 + (t + 1) * 128], wC[:, kc, :],
                                 start=(kc == 0), stop=(kc == KC - 1), rd=[self.r_hT[bi], r_wC], wr=[r_pv])
                        k.cp(Vtok[:, t, :], pv[:, :], rd=[r_pv], wr=[r_V], E=k.act)

                def gates(bi, c0, n, dirn, h, reset):
                    hd = dirn * 4 + h
                    pf, r_pf = gb.get()
                    for kc in range(KC):
                        k.mm(pf[:, 0:n], wB[:, kc, h * 128:(h + 1) * 128], self.hT[:, kc, c0:c0 + n],
                             start=(kc == 0), stop=(kc == KC - 1), rd=[self.r_hT[bi], r_wB], wr=[r_pf])
                    k.actf(T[1][:, 0:n], pf[:, 0:n], AF.Sigmoid, rd=[r_pf], wr=[r_T[1]])
                    k.ts(T[1][:, 0:n], T[1][:, 0:n], self.oml[:, hd:hd + 1], self.lb[:, hd:hd + 1], ALU.mult, ALU.add,
                         rd=[r_T[1], self.r_mod], wr=[r_T[1]])
                    k.actf(T[2][:, 0:n], T[1][:, 0:n], AF.Ln, rd=[r_T[1]], wr=[r_T[2]])
                    k.ts(T[1][:, 0:n], T[1][:, 0:n], -1.0, 1.0, ALU.mult, ALU.add, rd=[r_T[1], r_T[2]], wr=[r_T[1]])
                    msk = self.ccc("RESET" if reset else "ONES", n)
                    k.op(k.dve, lambda e: e.tensor_tensor_scan(out=T[3][:, 0:n], data0=msk, data1=T[2][:, 0:n], initial=0.0,
                                                               op0=ALU.mult, op1=ALU.add), rd=[r_T[2], self.r_cc], wr=[r_T[3]])

                def kt_transposes(h, n):
                    pt, r_pt = gb.get()
                    ptb = pt[:, :].bitcast(BF16)
                    nt = n // 128
                    for t in range(nt):
                        k.tr(ptb[:, t * 128:(t + 1) * 128], Kt[h][:, t * 128:(t + 1) * 128], self.identb[:],
                             rd=[r_Kt[h], self.r_const], wr=[r_pt], sig=(t == nt - 1))
                    k.cp(Ktok[h][:, 0:nt, :], ptb[:, 0:nt * 128].rearrange("p (t c) -> p t c", c=128), rd=[r_pt], wr=[r_Ktok[h]], E=k.act)

                def main_block(bi, dirn, first):
                    c0, n, is_ctx, li = self.blocks[bi]
                    nch = n // 64
                    proj_v(bi, c0, n)
                    for h in range(4):
                        hd = dirn * 4 + h
                        pq, r_pq = gb.get()
                        for kc in range(KC):
                            k.mm(pq[:, 0:n], wA[:, kc, h * 128:(h + 1) * 128], self.hT[:, kc, c0:c0 + n],
                                 start=(kc == 0), stop=(kc == KC - 1), rd=[self.r_hT[bi], r_wA], wr=[r_pq])
                        k.actf(T[0][:, 0:n], pq[:, 0:n], AF.Silu, rd=[r_pq], wr=[r_T[0]])
                        gates(bi, c0, n, dirn, h, True)
                        tot = T[3][:, 0:n].rearrange("p (c t) -> p c t", t=64)[:, :, 63]
                        k.actf(dch[h][:, 0:nch], tot, AF.Exp, rd=[r_T[3]], wr=[r_dch[h]])
                        if dirn == 1:
                            k.tt(T[3][:, 0:n], T[3][:, 0:n], T[2][:, 0:n], ALU.subtract, rd=[r_T[3], r_T[2], r_dch[h]], wr=[r_T[3]])
                        sg1 = 1.0 if dirn == 0 else -1.0
                        k.actf(T[2][:, 0:n], T[3][:, 0:n], AF.Exp, rd=[r_T[3]], wr=[r_T[2]], scale=sg1)
                        k.tt(Qt[h][:, 0:n], T[0][:, 0:n], T[2][:, 0:n], ALU.mult, rd=[r_T[0], r_T[2]], wr=[r_Qt[h]])
                        k.actf(T[2][:, 0:n], T[3][:, 0:n], AF.Exp, rd=[r_T[3], r_Qt[h]], wr=[r_T[2]], scale=-sg1)
                        k.tt(Kt[h][:, 0:n], T[1][:, 0:n], T[2][:, 0:n], ALU.mult, rd=[r_T[1], r_T[2]], wr=[r_Kt[h]])
                        kt_transposes(h, n)
                        pa, r_pa = gb.get()
                        for t in range(n // 128):
                            sl = slice(t * 128, (t + 1) * 128)
                            k.mm(pa[:, sl], Kt[h][:, sl], Qt[h][:, sl], start=True, stop=True, rd=[r_Kt[h], r_Qt[h]], wr=[r_pa],
                                 sig=(t == n // 128 - 1))
                        mk = self.ccc("MASKF" if dirn == 0 else "MASKB", 128).unsqueeze(1).to_broadcast([128, n // 128, 128])
                        k.tt(attm[h][:, 0:n].rearrange("p (t c) -> p t c", c=128), pa[:, 0:n].rearrange("p (t c) -> p t c", c=128), mk, ALU.mult,
                             rd=[r_pa, self.r_cc], wr=[r_attm[h]])
                    order = list(range(nch)) if dirn == 0 else list(range(nch - 1, -1, -1))
                    pd_banks = Banks(self.ps, self.r_ps, [4, 5])
                    for ci in order:
                        t, p0 = ci // 2, (ci % 2) * 64
                        cs = slice(ci * 64, (ci + 1) * 64)
                        pd, r_pd = pd_banks.get()
                        for h in range(4):
                            hd = dirn * 4 + h
                            po, r_po = self.ps[h], self.r_ps[h]
                            if dirn == 1:
                                k.actf(Sb[:, hd, :], S32[:, hd, :], AF.Copy, rd=[r_S[hd], r_dch[h]], wr=[r_Sb[hd]], scale=dch[h][:, ci:ci + 1])
                                k.actf(S32[:, hd, :], S32[:, hd, :], AF.Copy, rd=[r_S[hd], r_dch[h]], wr=[r_S[hd]], scale=dch[h][:, ci:ci + 1])
                            k.mm(po[:, cs], Sb[:, hd, :], Qt[h][:, cs], start=True, stop=False, rd=[r_Sb[hd], r_Qt[h]], wr=[r_po])
                            k.mm(po[:, cs], Vtok[p0:p0 + 64, t, h * 128:(h + 1) * 128], attm[h][p0:p0 + 64, cs], start=False, stop=True,
                                 rd=[r_V, r_attm[h]], wr=[r_po])
                            k.mm(pd[:, h * 128:(h + 1) * 128], Ktok[h][p0:p0 + 64, t, :], Vtok[p0:p0 + 64, t, h * 128:(h + 1) * 128],
                                 start=True, stop=True, rd=[r_Ktok[h], r_V], wr=[r_pd])
                            if dirn == 0:
                                k.actf(Sd[:, h, :], S32[:, hd, :], AF.Copy, rd=[r_S[hd], r_dch[h]], wr=[r_Sd[hd]], scale=dch[h][:, ci:ci + 1])
                                k.stt(Sb[:, hd, :], pd[:, h * 128:(h + 1) * 128], dch[h][:, ci:ci + 1], Sd[:, h, :], ALU.mult, ALU.add,
                                      rd=[r_pd, r_dch[h], r_Sd[hd]], wr=[r_Sb[hd]])
                                k.stt(S32[:, hd, :], pd[:, h * 128:(h + 1) * 128], dch[h][:, ci:ci + 1], Sd[:, h, :], ALU.mult, ALU.add,
                                      rd=[r_pd, r_dch[h], r_Sd[hd]], wr=[r_S[hd]])
                            else:
                                k.tt(S32[:, hd, :], S32[:, hd, :], pd[:, h * 128:(h + 1) * 128], ALU.add, rd=[r_S[hd], r_pd], wr=[r_S[hd]])
                    for h in range(4):
                        po, r_po = self.ps[h], self.r_ps[h]
                        dst = self.osum[:, h, c0:c0 + n]
                        if first:
                            k.cp(dst, po[:, 0:n], rd=[r_po], wr=[self.r_osum[bi]], E=k.act)
                        else:
                            k.tt(dst, po[:, 0:n], dst, ALU.add, rd=[r_po, self.r_osum[bi]], wr=[self.r_osum[bi]])

                def zero_states():
                    k.op(k.pool, lambda e: e.memset(S32[:], 0.0), wr=r_S)
                    k.op(k.pool, lambda e: e.memset(Sb[:], 0.0), wr=r_Sb)

                self.load_w(wA[:], wsrc("q"), r_wA)
                self.load_w(wB[:], wsrc("ff"), r_wB)
                self.load_w(wC[:], wsrc("i"), r_wC)
                zero_states()
                main_block(0, 0, True)
                k.cp(Sfin[:, 0:4, :], S32[:, 0:4, :], rd=r_S[0:4], wr=[r_Sfin], E=k.pool)
                k.op(k.pool, lambda e: e.memset(xb[:], 0.0), wr=[r_xb])
                k.op(k.pool, lambda e: e.memset(xb[:, :, 128:129], 1.0), wr=[r_xb])

                def prepass(dirn):
                    for bi in range(1, 5):
                        c0, n, is_ctx, li = self.blocks[bi]
                        proj_v(bi, c0, n)
                        for h in range(4):
                            hd = dirn * 4 + h
                            gates(bi, c0, n, dirn, h, False)
                            k.actf(dend[:], T[3][:, n - 1:n], AF.Exp, rd=[r_T[3]], wr=[r_dend])
                            if dirn == 0:
                                k.actf(T[2][:, 0:n], T[3][:, 0:n], AF.Exp, rd=[r_T[3]], wr=[r_T[2]], scale=-1.0, bias=T[3][:, n - 1:n])
                            else:
                                k.tt(T[3][:, 0:n], T[3][:, 0:n], T[2][:, 0:n], ALU.subtract, rd=[r_T[3], r_T[2], r_dend], wr=[r_T[3]])
                                k.actf(T[2][:, 0:n], T[3][:, 0:n], AF.Exp, rd=[r_T[3]], wr=[r_T[2]])
                            k.tt(Kt[h][:, 0:n], T[1][:, 0:n], T[2][:, 0:n], ALU.mult, rd=[r_T[1], r_T[2]], wr=[r_Kt[h]])
                            kt_transposes(h, n)
                            pl, r_pl = gb.get()
                            for t in range(4):
                                k.mm(pl[:, 0:128], Ktok[h][:, t, :], Vtok[:, t, h * 128:(h + 1) * 128], start=(t == 0), stop=(t == 3),
                                     rd=[r_Ktok[h], r_V], wr=[r_pl])
                            Ld, Dd = xb[:, hd, 0:128], xb[:, hd, 128:129]
                            if dirn == 0:
                                k.stt(Ld, Ld, dend[:, 0:1], pl[:, 0:128], ALU.mult, ALU.add, rd=[r_xb, r_dend, r_pl], wr=[r_xb])
                            else:
                                k.stt(Ld, pl[:, 0:128], Dd, Ld, ALU.mult, ALU.add, rd=[r_xb, r_pl], wr=[r_xb])
                            k.tt(Dd, Dd, dend[:, 0:1], ALU.mult, rd=[r_xb, r_dend], wr=[r_xb])

                prepass(0)
                self.load_w(wB[:], wsrc("fb"), r_wB)
                zero_states()
                main_block(0, 1, False)
                k.cp(Sfin[:, 4:8, :], S32[:, 4:8, :], rd=r_S[4:8], wr=[r_Sfin], E=k.pool)
                prepass(1)
                if self.stage == 3:
                    self.dbg("Sfin", Sfin[:].rearrange("p a b -> p (a b)"), r_Sfin, [128, 1024])
                    self.dbg("xb", xb[:].rearrange("p a b -> p (a b)"), r_xb, [128, 8 * 129])
                    self.dbg("osum_ctx", self.osum[:, :, 0:NCX], self.r_osum[0], [128, 4, NCX])
                xin = self.nc.dram_tensor("hg_xin", [128, 8 * 129], F32, kind="Internal").ap()
                xout = self.nc.dram_tensor("hg_xout", [4 * 128, 8 * 129], F32, kind="Internal").ap()
                r_xin, r_xout = k.R("xin"), k.R("xout")
                k.dma(k.pool, xin[:, :], xb[:].rearrange("p a b -> p (a b)"), rd=[r_xb], wr=[r_xin])
                k.collective(lambda g: g.collective_compute("AllGather", ALU.bypass, replica_groups=[[0, 1, 2, 3], [4, 5, 6, 7]],
                                                            ins=[xin[:, :]], outs=[xout[:, :]]), rd=[r_xin], wr=[r_xout])
                xgs = [self.sb(es2, "xg%d" % i, [128, 4, 129]) for i in range(2)]
                r_xgs = k.Rs(2, "xg")
                xov = xout.rearrange("(r p) (a b) -> p r a b", p=128, b=129)
                Ct = T[0][:, 0:128]
                r_C = r_T[0]
                for hd in range(8):
                    fwd = hd < 4
                    sel = "SELF" if fwd else "SELB"
                    ranks = [0, 1, 2, 3] if fwd else [3, 2, 1, 0]
                    xg, r_xg = xgs[hd % 2], r_xgs[hd % 2]
                    k.dma(k.sp, xg[:], xov[:, :, hd, :], rd=[r_xout], wr=[r_xg])
                    k.cp(Ct, Sfin[:, hd, :], rd=[r_Sfin], wr=[r_C])
                    k.ts(S32[:, hd, :], Ct, self.ppc(sel, ranks[0]), None, ALU.mult, None, rd=[r_C, self.r_pp], wr=[r_S[hd]])
                    for i in range(3):
                        r = ranks[i]
                        k.stt(Ct, Ct, xg[:, r, 128:129], xg[:, r, 0:128], ALU.mult, ALU.add, rd=[r_C, r_xg], wr=[r_C])
                        k.stt(S32[:, hd, :], Ct, self.ppc(sel, ranks[i + 1]), S32[:, hd, :], ALU.mult, ALU.add,
                              rd=[r_C, self.r_pp, r_S[hd]], wr=[r_S[hd]])
                    k.cp(Sb[:, hd, :], S32[:, hd, :], rd=[r_S[hd]], wr=[r_Sb[hd]], E=k.pool)
                if self.stage == 3:
                    self.dbg("Sin", S32[:].rearrange("p a b -> p (a b)"), r_S, [128, 1024])
                if self.stage >= 4:
                    for bi in (4, 3, 2, 1):
                        main_block(bi, 1, True)
                    self.load_w(wB[:], wsrc("ff"), r_wB)
                    for bi in (1, 2, 3, 4):
                        main_block(bi, 0, False)
                k.barrier()
            if self.stage >= 4:
                self.hgrn_readout(es, wA, r_wA)

    def hgrn_readout(self, es, wA, r_wA):
        k = self.k
        with contextlib.ExitStack() as es2:
            wo1 = self.sb(es2, "w_out1", [128, 4, D], BF16)
            r_wo1 = k.R("wo1")
            self.load_w(wA[:], self.ab_w_in[:, 2048:2560].rearrange("(kc p) n -> p kc n", p=128), r_wA)
            self.load_w(wo1[:], self.ab_w_out[0:512, :].rearrange("(kc p) n -> p kc n", p=128), r_wo1)
            SG = [self.sb(es2, "roSG%d" % i, [128, 512]) for i in range(2)]
            SQ = [self.sb(es2, "roSQ%d" % i, [128, 512]) for i in range(2)]
            RS = [self.sb(es2, "roRS%d" % i, [128, 512]) for i in range(2)]
            r_SG, r_SQ, r_RS = k.Rs(2, "roSG"), k.Rs(2, "roSQ"), k.Rs(2, "roRS")
            a = self.sb(es2, "ro_a", [128, 4, 512], BF16)
            r_a = k.R("ro_a")
            banks = Banks(self.ps, self.r_ps, [0, 1, 2, 3, 4, 5, 6, 7])
            it = 0
            for bi, (c0, n, is_ctx, li) in enumerate(self.blocks):
                for h in range(4):
                    i2 = it % 2
                    it += 1
                    pg, r_pg = banks.get()
                    for kc in range(KC):
                        k.mm(pg[:, 0:n], wA[:, kc, h * 128:(h + 1) * 128], self.hT[:, kc, c0:c0 + n],
                             start=(kc == 0), stop=(kc == KC - 1), rd=[self.r_hT[bi], r_wA], wr=[r_pg])
                    k.actf(SG[i2][:, 0:n], pg[:, 0:n], AF.Silu, rd=[r_pg], wr=[r_SG[i2]])
                    o = self.osum[:, h, c0:c0 + n]
                    k.actf(SQ[i2][:, 0:n], o, AF.Square, rd=[self.r_osum[bi]], wr=[r_SQ[i2]])
                    pss, r_pss = banks.get()
                    k.mm(pss[:, 0:n], self.ccc("ONES", 128), SQ[i2][:, 0:n], start=True, stop=True, rd=[r_SQ[i2], self.r_cc], wr=[r_pss])
                    k.ts(RS[i2][:, 0:n], pss[:, 0:n], 1.0 / 128, EPS, ALU.mult, ALU.add, rd=[r_pss], wr=[r_RS[i2]])
                    k.actf(RS[i2][:, 0:n], RS[i2][:, 0:n], AF.Sqrt, rd=[r_RS[i2]], wr=[r_RS[i2]])
                    k.op(k.dve, lambda e: e.reciprocal(out=RS[i2][:, 0:n], in_=RS[i2][:, 0:n]), rd=[r_RS[i2]], wr=[r_RS[i2]])
                    k.stt(SQ[i2][:, 0:n], o, self.ppc("ONG"), RS[i2][:, 0:n], ALU.mult, ALU.mult,
                          rd=[self.r_osum[bi], self.r_pp, r_RS[i2], r_pss], wr=[r_SQ[i2]])
                    k.tt(a[:, h, 0:n], SQ[i2][:, 0:n], SG[i2][:, 0:n], ALU.mult, rd=[r_SQ[i2], r_SG[i2]], wr=[r_a])
                for m in range(KC):
                    pb, r_pb = banks.get()
                    for h in range(4):
                        k.mm(pb[:, 0:n], wo1[:, h, m * 128:(m + 1) * 128], a[:, h, 0:n], start=(h == 0), stop=(h == 3),
                             rd=[r_wo1, r_a], wr=[r_pb])
                    dst, r_dst, v = self.x_dst(is_ctx, li, m, n)
                    k.stt(dst, pb[:, 0:n], self.modv(0, 2, m, v), dst, ALU.mult, ALU.add, rd=[r_pb, self.r_mod, r_dst], wr=[r_dst])
            if self.stage == 4:
                self.dbg("a_last", a[:].rearrange("p a b -> p (a b)"), r_a, [128, 2048])
                self.dbg("xmix", self.xT[:, 0, :], self.r_xT, [128, NL])
                self.dbg("xcmix", self.xcT[:, 0, :], self.r_xcT, [128, NCX])
            k.barrier()

    def moe(self, l):
        k = self.k
        blocks = ([(0, NCX, True, -1)] if l == 0 else []) + [(NCX + 512 * i, 512, False, i) for i in range(4)]
        with contextlib.ExitStack() as es:
            h2T = self.sb(es, "h2T", [128, KC, NT], BF16)
            r_h2 = k.Rs(5, "h2T")
            combT = self.sb(es, "combT", [32, NT], BF16)
            r_combT = k.Rs(5, "combT")
            with contextlib.ExitStack() as es2:
                tmp = self.norm_tmp(es2, banks_idx=(4, 5))
                h2f = self.sb(es2, "h2f", [128, KC, 512])
                r_h2f = k.R("h2f")
                wr = self.sb(es2, "moe_wr", [128, KC, 36])
                r_wr = k.R("moe_wr")
                k.dma(k.sp, wr[:], self.moe_wr[l].rearrange("(kc p) n -> p kc n", p=128), wr=[r_wr])
                lg = self.sb(es2, "lg", [128, 4, 36])
                r_lg = k.R("lg")
                names = ["gmax", "gsum", "m1", "m2", "w1", "w2"]
                sm = {nm: self.sb(es2, "rt_" + nm, [128, 4]) for nm in names}
                og = self.sb(es2, "rt_og", [128, 4, 4])
                ge = self.sb(es2, "rt_ge", [128, 4, 4])
                elm = self.sb(es2, "rt_elm", [128, 4, 32])
                oh1 = self.sb(es2, "rt_oh1", [128, 4, 32])
                oh2 = self.sb(es2, "rt_oh2", [128, 4, 32])
                comb = self.sb(es2, "rt_comb", [128, 4, 32])
                r_rt = k.R("rt")
                rb = Banks(self.ps, self.r_ps, [6, 7])
                for bi_, (c0, n, is_ctx, li) in enumerate(blocks):
                    bi = bi_ if l == 0 else bi_ + 1
                    v = 1 if is_ctx else 0
                    nt = n // 128
                    src_fn = (lambda kc: self.xcT[:, kc, :]) if is_ctx else (lambda kc, li=li: self.xT[:, kc, li * 512:(li + 1) * 512])
                    self.norm_mod(tmp, src_fn, n, lambda kc, v=v: self.gs2[l][:, kc, v:v + 1],
                                  lambda kc, v=v: self.modv(l, 3, kc, v),
                                  lambda kc, c0=c0, n=n: h2T[:, kc, c0:c0 + n],
                                  rd=[self.r_xcT if is_ctx else self.r_xT[li]], wr=[r_h2[bi], r_h2f],
                                  f32_out_fn=lambda kc, n=n: h2f[:, kc, 0:n])
                    pr, r_pr = rb.get()
                    for t in range(nt):
                        for kc in range(KC):
                            k.mm(pr[:, t * 36:(t + 1) * 36], h2f[:, kc, t * 128:(t + 1) * 128], wr[:, kc, :],
                                 start=(kc == 0), stop=(kc == KC - 1), rd=[r_h2f, r_wr], wr=[r_pr])
                    k.tt(lg[:, 0:nt, :], pr[:, 0:nt * 36].rearrange("p (t c) -> p t c", c=36),
                         self.ppc("BR", l * 36, 36).unsqueeze(1).to_broadcast([128, nt, 36]), ALU.add, rd=[r_pr, self.r_pp], wr=[r_lg])
                    gl = lg[:, 0:nt, 0:4]
                    el = lg[:, 0:nt, 4:36]
                    R_ = [r_rt]

                    def bc(ap2, last):
                        return ap2.unsqueeze(2).to_broadcast([128, nt, last])

                    k.op(k.dve, lambda e: e.tensor_reduce(out=sm["gmax"][:, 0:nt], in_=gl, axis=AX.X, op=ALU.max), rd=[r_lg], wr=R_)
                    k.tt(og[:, 0:nt, :], gl, bc(sm["gmax"][:, 0:nt], 4), ALU.is_equal, rd=[r_lg] + R_, wr=R_)
                    k.tt(ge[:, 0:nt, :], gl, bc(sm["gmax"][:, 0:nt], 4), ALU.subtract, rd=[r_lg] + R_, wr=R_)
                    k.actf(ge[:, 0:nt, :], ge[:, 0:nt, :], AF.Exp, rd=R_, wr=R_)
                    k.op(k.dve, lambda e: e.tensor_reduce(out=sm["gsum"][:, 0:nt], in_=ge[:, 0:nt, :], axis=AX.X, op=ALU.add), rd=R_, wr=R_)
                    k.op(k.dve, lambda e: e.reciprocal(out=sm["gsum"][:, 0:nt], in_=sm["gsum"][:, 0:nt]), rd=R_, wr=R_)
                    k.ts(ge[:, 0:nt, :], og[:, 0:nt, :], BIG, -BIG, ALU.mult, ALU.add, rd=R_, wr=R_)
                    k.tt(elm[:, 0:nt, :].rearrange("p t (g e) -> p t g e", e=8), el.rearrange("p t (g e) -> p t g e", e=8),
                         ge[:, 0:nt, :].unsqueeze(3).to_broadcast([128, nt, 4, 8]), ALU.add, rd=[r_lg] + R_, wr=R_)
                    k.op(k.dve, lambda e: e.tensor_reduce(out=sm["m1"][:, 0:nt], in_=elm[:, 0:nt, :], axis=AX.X, op=ALU.max), rd=R_, wr=R_)
                    k.tt(oh1[:, 0:nt, :], elm[:, 0:nt, :], bc(sm["m1"][:, 0:nt], 32), ALU.is_equal, rd=R_, wr=R_)
                    k.stt(elm[:, 0:nt, :], oh1[:, 0:nt, :], -BIG, elm[:, 0:nt, :], ALU.mult, ALU.add, rd=R_, wr=R_)
                    k.op(k.dve, lambda e: e.tensor_reduce(out=sm["m2"][:, 0:nt], in_=elm[:, 0:nt, :], axis=AX.X, op=ALU.max), rd=R_, wr=R_)
                    k.tt(oh2[:, 0:nt, :], elm[:, 0:nt, :], bc(sm["m2"][:, 0:nt], 32), ALU.is_equal, rd=R_, wr=R_)
                    k.tt(sm["w2"][:, 0:nt], sm["m2"][:, 0:nt], sm["m1"][:, 0:nt], ALU.subtract, rd=R_, wr=R_)
                    k.actf(sm["w2"][:, 0:nt], sm["w2"][:, 0:nt], AF.Sigmoid, rd=R_, wr=R_)
                    k.ts(sm["w1"][:, 0:nt], sm["w2"][:, 0:nt], -1.0, 1.0, ALU.mult, ALU.add, rd=R_, wr=R_)
                    k.tt(sm["w1"][:, 0:nt], sm["w1"][:, 0:nt], sm["gsum"][:, 0:nt], ALU.mult, rd=R_, wr=R_)
                    k.tt(sm["w2"][:, 0:nt], sm["w2"][:, 0:nt], sm["gsum"][:, 0:nt], ALU.mult, rd=R_, wr=R_)
                    k.tt(oh1[:, 0:nt, :], oh1[:, 0:nt, :], bc(sm["w1"][:, 0:nt], 32), ALU.mult, rd=R_, wr=R_)
                    k.tt(oh2[:, 0:nt, :], oh2[:, 0:nt, :], bc(sm["w2"][:, 0:nt], 32), ALU.mult, rd=R_, wr=R_)
                    k.tt(comb[:, 0:nt, :], oh1[:, 0:nt, :], oh2[:, 0:nt, :], ALU.add, rd=R_, wr=R_)
                    pc, r_pc = rb.get()
                    for t in range(nt):
                        k.tr(pc[0:32, t * 128:(t + 1) * 128], comb[:, t, :], self.ccc("IDENT", 128), rd=R_ + [self.r_cc], wr=[r_pc], sig=(t == nt - 1))
                    k.cp(combT[:, c0:c0 + n], pc[0:32, 0:n], rd=[r_pc], wr=[r_combT[bi]], E=k.act)
                if self.stage == 5:
                    self.dbg("combT", combT[:, :], r_combT, [32, NT])
                k.barrier()
            with contextlib.ExitStack() as es2:
                NWB = 2
                wg = [self.sb(es2, "mwg%d" % i, [128, 2, KC, 256], BF16) for i in range(NWB)]
                wu = [self.sb(es2, "mwu%d" % i, [128, 2, KC, 256], BF16) for i in range(NWB)]
                wd = [self.sb(es2, "mwd%d" % i, [128, 2, 2, D], BF16) for i in range(NWB)]
                r_w = k.Rs(NWB, "moew")
                cme = [self.sb(es2, "cme%d" % i, [32, 512], BF16) for i in range(2)]
                r_cme = k.Rs(2, "cme")
                sgt = [self.sb(es2, "sgt%d" % i, [128, 512]) for i in range(2)]
                r_sgt = k.Rs(2, "sgt")
                tmu = [self.sb(es2, "tmu%d" % i, [128, 512]) for i in range(2)]
                r_tmu = k.Rs(2, "tmu")
                abuf = [self.sb(es2, "abuf%d" % i, [128, 2, 2, 512], BF16) for i in range(2)]
                r_ab = k.Rs(2, "abuf")
                cbanks = Banks(self.ps, self.r_ps, [6, 7])
                dbanks = Banks(self.ps, self.r_ps, [4, 5])
                it2 = 0
                ia = 0
                def issue_w(pi):
                    wbi = pi % NWB
                    for ei in range(2):
                        e = pi * 2 + ei
                        self.load_w(wg[wbi][:, ei, :, :], self.moe_w_gate[l, e].rearrange("(kc p) f -> p kc f", p=128), r_w[wbi])
                        self.load_w(wu[wbi][:, ei, :, :], self.moe_w_up[l, e].rearrange("(kc p) f -> p kc f", p=128), r_w[wbi])
                        self.load_w(wd[wbi][:, ei, :, :], self.moe_w_down[l, e].rearrange("(fc p) d -> p fc d", p=128), r_w[wbi])

                issue_w(0)
                for pi in range(16):
                    wbi = pi % NWB
                    if pi + 1 < 16:
                        issue_w(pi + 1)
                    for bi_, (c0, n, is_ctx, li) in enumerate(blocks):
                        bi = bi_ if l == 0 else bi_ + 1
                        ab, r_a = abuf[ia % 2], r_ab[ia % 2]
                        ia += 1
                        for ei in range(2):
                            e = pi * 2 + ei
                            ci = it2 % 2
                            k.actf(cme[ci][:, 0:n], combT[:, c0:c0 + n], AF.Copy, rd=[r_combT[bi], self.r_cc], wr=[r_cme[ci]],
                                   scale=self.ccc("IDENT", 32)[0:32, e:e + 1])
                            pc, r_pc = cbanks.get()
                            k.mm(pc[:, 0:n], self.onesb[0:32, :], cme[ci][:, 0:n], start=True, stop=True,
                                 rd=[r_cme[ci], self.r_const], wr=[r_pc])
                            for fc in range(2):
                                pgt, r_pgt = self.ps[fc * 2], self.r_ps[fc * 2]
                                put, r_put = self.ps[fc * 2 + 1], self.r_ps[fc * 2 + 1]
                                for kc in range(KC):
                                    k.mm(pgt[:, 0:n], wg[wbi][:, ei, kc, fc * 128:(fc + 1) * 128], h2T[:, kc, c0:c0 + n],
                                         start=(kc == 0), stop=(kc == KC - 1), rd=[r_w[wbi], r_h2[bi]], wr=[r_pgt])
                                for kc in range(KC):
                                    k.mm(put[:, 0:n], wu[wbi][:, ei, kc, fc * 128:(fc + 1) * 128], h2T[:, kc, c0:c0 + n],
                                         start=(kc == 0), stop=(kc == KC - 1), rd=[r_w[wbi], r_h2[bi]], wr=[r_put])
                                si = it2 % 2
                                it2 += 1
                                k.actf(sgt[si][:, 0:n], pgt[:, 0:n], AF.Silu, rd=[r_pgt], wr=[r_sgt[si]])
                                k.tt(tmu[si][:, 0:n], put[:, 0:n], sgt[si][:, 0:n], ALU.mult, rd=[r_put, r_sgt[si]], wr=[r_tmu[si]])
                                k.tt(ab[:, ei, fc, 0:n], tmu[si][:, 0:n], pc[:, 0:n], ALU.mult, rd=[r_tmu[si], r_pc], wr=[r_a])
                        for m in range(KC):
                            pb, r_pb = dbanks.get()
                            for j4 in range(4):
                                ei, fc = j4 // 2, j4 % 2
                                k.mm(pb[:, 0:n], wd[wbi][:, ei, fc, m * 128:(m + 1) * 128], ab[:, ei, fc, 0:n], start=(j4 == 0), stop=(j4 == 3),
                                     rd=[r_w[wbi], r_a], wr=[r_pb])
                            dst, r_dst, v = self.x_dst(is_ctx, li, m, n)
                            k.stt(dst, pb[:, 0:n], self.modv(l, 5, m, v), dst, ALU.mult, ALU.add, rd=[r_pb, self.r_mod, r_dst], wr=[r_dst])
                k.barrier()

    CS = 8224

    def layer1(self):
        k, nc = self.k, self.nc
        L = 1
        with contextlib.ExitStack() as es:
            cqn = self.sb(es, "cqn", [128, 2, NL], BF16)
            kvc = self.sb(es, "kvc", [128, NCX], BF16)
            krc = self.sb(es, "krc", [96, NCX], BF16)
            r_cqn, r_kvc, r_krc = k.R("cqn"), k.R("kvc"), k.R("krc")
            widths = {"A": 4096, "B": 2048, "C": 2304}
            l1_in = {n_: nc.dram_tensor("l1_in" + n_, [128, w_], BF16, kind="Internal").ap() for n_, w_ in widths.items()}
            l1_out = {n_: nc.dram_tensor("l1_out" + n_, [4 * 128, w_], BF16, kind="Internal").ap() for n_, w_ in widths.items()}
            r_in = {n_: k.R("l1_in" + n_) for n_ in widths}
            r_out = {n_: k.R("l1_out" + n_) for n_ in widths}
            with contextlib.ExitStack() as esn:
                self.layer1_na(esn, L, cqn, kvc, krc, r_cqn, r_kvc, r_krc, l1_in, l1_out, r_in, r_out)
            if self.stage >= 9:
                self.mla_attention(cqn, kvc, krc, r_cqn, r_kvc, r_krc, l1_out, r_out)

    def layer1_na(self, es, L, cqn, kvc, krc, r_cqn, r_kvc, r_krc, l1_in, l1_out, r_in, r_out):
        k, nc = self.k, self.nc
        if True:
            QT2 = self.sb(es, "QT2", [128, 4, NL], BF16)
            KT2 = self.sb(es, "KT2", [128, 4, NCX + 2560], BF16)
            VX = self.sb(es, "VX", [128, 22, 8, 72], BF16)
            r_QT2, r_KT2, r_VX = (k.R(n_) for n_ in ("QT2", "KT2", "VX"))
            k.op(k.pool, lambda e: e.memset(VX[:].rearrange("p t h d -> p (t h d)"), 0.0), wr=[r_VX])
            k.op(k.pool, lambda e: e.memset(VX[:, :, :, 64:65], 1.0), wr=[r_VX])
            with contextlib.ExitStack() as es2:
                tmp = self.norm_tmp(es2, banks_idx=(6, 7))
                hb = [self.sb(es2, "hblk%d" % i, [128, KC, 512], BF16) for i in range(1)] * 2
                r_hb = k.Rs(1, "hblk") * 2
                wb = [self.sb(es2, "l1w%d" % i, [128, KC, 512], BF16) for i in range(2)]
                r_wb = k.Rs(2, "l1w")
                wkr = self.sb(es2, "wkr", [128, KC, 192], BF16)
                r_wkr = k.R("wkr")
                self.load_w(wkr[:], self.wkr_d.rearrange("(kc p) n -> p kc n", p=128), r_wkr)
                rope = self.sb(es2, "ropeA", [128, 2, 512])
                r_rope = k.R("ropeA")
                t32 = [self.sb(es2, "l1t%d" % i, [128, 512]) for i in range(4)]
                r_t32 = k.Rs(4, "l1t")
                tb = [self.sb(es2, "l1tb%d" % i, [128, 512], BF16) for i in range(2)]
                r_tb = k.Rs(2, "l1tb")
                banks = Banks(self.ps, self.r_ps, [0, 1, 2, 3, 4, 5])
                iw = 0
                wreq = []
                for (_c0, _n, _ctx, _li) in self.blocks:
                    wreq += ([] if _ctx else [(0, 512)]) + [(512, 512), (1024, 512), (1536, 384)]
                wstate = {"issued": 0, "used": 0}

                def issue_next():
                    i = wstate["issued"]
                    if i >= len(wreq):
                        return
                    col0, ncol = wreq[i]
                    self.load_w(wb[i % 2][:, :, 0:ncol], self.cd_w_in[:, col0:col0 + ncol].rearrange("(kc p) n -> p kc n", p=128), r_wb[i % 2])
                    wstate["issued"] = i + 1

                issue_next()
                for bi, (c0, n, is_ctx, li) in enumerate(self.blocks):
                    v = 1 if is_ctx else 0
                    h, r_h = hb[bi % 2], r_hb[bi % 2]
                    src_fn = (lambda kc: self.xcT[:, kc, :]) if is_ctx else (lambda kc, li=li: self.xT[:, kc, li * 512:(li + 1) * 512])
                    self.norm_mod(tmp, src_fn, n, lambda kc, v=v: self.gs1[L][:, kc, v:v + 1],
                                  lambda kc, v=v: self.modv(L, 0, kc, v), lambda kc, n=n: h[:, kc, 0:n],
                                  rd=[self.r_xcT if is_ctx else self.r_xT[li]], wr=[r_h])
                    lc = 0 if is_ctx else li * 512
                    ntile = n // 128

                    def getw(col0, ncol):
                        i = wstate["used"]
                        assert wreq[i] == (col0, ncol)
                        wstate["used"] = i + 1
                        if wstate["issued"] <= i:
                            issue_next()
                        w, r_w = wb[i % 2], r_wb[i % 2]
                        issue_next()
                        return w, r_w

                    def proj_fm(w, r_w, wc0, M, pb, r_pb):
                        for kc in range(KC):
                            k.mm(pb[0:M, 0:n], w[:, kc, wc0:wc0 + M], h[:, kc, 0:n], start=(kc == 0), stop=(kc == KC - 1),
                                 rd=[r_w, r_h], wr=[r_pb])

                    if not is_ctx:
                        w, r_w = getw(0, 512)
                        for pr in range(4):
                            pb, r_pb = banks.get()
                            proj_fm(w, r_w, pr * 128, 128, pb, r_pb)
                            k.cp(QT2[:, pr, lc:lc + n], pb[:, 0:n], rd=[r_pb], wr=[r_QT2], E=(k.act if pr % 2 else k.dve))
                    w, r_w = getw(512, 512)
                    kc0 = 0 if is_ctx else NCX + 256 + lc
                    for pr in range(4):
                        pb, r_pb = banks.get()
                        proj_fm(w, r_w, pr * 128, 128, pb, r_pb)
                        k.cp(KT2[:, pr, kc0:kc0 + n], pb[:, 0:n], rd=[r_pb], wr=[r_KT2], E=(k.act if pr % 2 else k.dve))
                    w, r_w = getw(1024, 512)
                    vt0 = 0 if is_ctx else 4 + li * 4
                    for t in range(ntile):
                        pb, r_pb = banks.get()
                        for kc in range(KC):
                            k.mm(pb[:, :], h[:, kc, t * 128:(t + 1) * 128], w[:, kc, :], start=(kc == 0), stop=(kc == KC - 1),
                                 rd=[r_w, r_h], wr=[r_pb])
                        k.cp(VX[:, vt0 + t, :, 0:64], pb[:, :].rearrange("p (h d) -> p h d", d=64), rd=[r_pb], wr=[r_VX], E=(k.act if t % 2 else k.dve))
                    w, r_w = getw(1536, 384)
                    if not is_ctx:
                        pcq = []
                        pss, r_pss = banks.get()
                        for c in range(2):
                            pb, r_pb = banks.get()
                            proj_fm(w, r_w, c * 128, 128, pb, r_pb)
                            pcq.append((pb, r_pb))
                            k.actf(t32[c][:, 0:n], pb[:, 0:n], AF.Square, rd=[r_pb], wr=[r_t32[c]])
                            k.mm(pss[:, 0:n], self.ccc("ONES", 128), t32[c][:, 0:n], start=(c == 0), stop=(c == 1), rd=[r_t32[c], self.r_cc], wr=[r_pss], sig=True)
                        k.ts(t32[2][:, 0:n], pss[:, 0:n], 1.0 / 256, EPS, ALU.mult, ALU.add, rd=[r_pss], wr=[r_t32[2]])
                        k.actf(t32[2][:, 0:n], t32[2][:, 0:n], AF.Sqrt, rd=[r_t32[2]], wr=[r_t32[2]])
                        k.op(k.dve, lambda e: e.reciprocal(out=t32[2][:, 0:n], in_=t32[2][:, 0:n]), rd=[r_t32[2]], wr=[r_t32[2]])
                        for c in range(2):
                            pb, r_pb = pcq[c]
                            k.stt(cqn[:, c, lc:lc + n], pb[:, 0:n], self.ppc("QNG", c), t32[2][:, 0:n], ALU.mult, ALU.mult,
                                  rd=[r_pb, self.r_pp, r_t32[2]], wr=[r_cqn])
                    pb, r_pb = banks.get()
                    proj_fm(w, r_w, 256, 128, pb, r_pb)
                    k.actf(t32[0][:, 0:n], pb[:, 0:n], AF.Square, rd=[r_pb], wr=[r_t32[0]])
                    pss, r_pss = banks.get()
                    k.mm(pss[:, 0:n], self.ccc("ONES", 128), t32[0][:, 0:n], start=True, stop=True, rd=[r_t32[0], self.r_cc], wr=[r_pss])
                    k.ts(t32[3][:, 0:n], pss[:, 0:n], 1.0 / 128, EPS, ALU.mult, ALU.add, rd=[r_pss], wr=[r_t32[3]])
                    k.actf(t32[3][:, 0:n], t32[3][:, 0:n], AF.Sqrt, rd=[r_t32[3]], wr=[r_t32[3]])
                    k.op(k.dve, lambda e: e.reciprocal(out=t32[3][:, 0:n], in_=t32[3][:, 0:n]), rd=[r_t32[3]], wr=[r_t32[3]])
                    if is_ctx:
                        k.stt(kvc[:, 0:n], pb[:, 0:n], self.ppc("KVNG"), t32[3][:, 0:n], ALU.mult, ALU.mult,
                              rd=[r_pb, self.r_pp, r_t32[3]], wr=[r_kvc])
                    else:
                        k.stt(tb[0][:, 0:n], pb[:, 0:n], self.ppc("KVNG"), t32[3][:, 0:n], ALU.mult, ALU.mult,
                              rd=[r_pb, self.r_pp, r_t32[3]], wr=[r_tb[0]])
                        k.dma(k.sp, l1_in["A"][:, lc:lc + n], tb[0][:, 0:n], rd=[r_tb[0]], wr=[r_in["A"]])
                    pb, r_pb = banks.get()
                    proj_fm(wkr, r_wkr, 0, 96, pb, r_pb)
                    if is_ctx:
                        k.cp(krc[64:96, 0:n], pb[64:96, 0:n], rd=[r_pb], wr=[r_krc], E=k.act)
                    else:
                        pb2, r_pb2 = banks.get()
                        proj_fm(wkr, r_wkr, 96, 96, pb2, r_pb2)
                        k.dma(k.sp, rope[64:96, :, 0:n], self.rope_d[:, :, lc:lc + n], wr=[r_rope])
                        k.tt(t32[1][64:96, 0:n], pb[64:96, 0:n], rope[64:96, 0, 0:n], ALU.mult, rd=[r_pb, r_rope], wr=[r_t32[1]])
                        k.tt(t32[0][64:96, 0:n], pb2[64:96, 0:n], rope[64:96, 1, 0:n], ALU.mult, rd=[r_pb2, r_rope, r_pss], wr=[r_t32[0]])
                        k.tt(tb[1][64:96, 0:n], t32[1][64:96, 0:n], t32[0][64:96, 0:n], ALU.add, rd=[r_t32[0], r_t32[1]], wr=[r_tb[1]])
                        k.dma(k.sp, l1_in["A"][0:32, 2048 + lc:2048 + lc + n], tb[1][64:96, 0:n], rd=[r_tb[1]], wr=[r_in["A"]])
                o0 = NCX + 256
                kin = l1_in["B"][:, :].rearrange("p (a c) -> p a c", a=4)
                k.dma(k.sp, kin[:, :, 0:256], KT2[:, :, o0:o0 + 256], rd=[r_KT2], wr=[r_in["B"]])
                k.dma(k.sp, kin[:, :, 256:512], KT2[:, :, o0 + NL - 256:o0 + NL], rd=[r_KT2], wr=[r_in["B"]])
                vin = l1_in["C"][:, :].rearrange("p (t c) -> p t c", t=4)
                k.dma(k.sp, vin[:, 0:2, :], VX[:, 4:6, :, :].rearrange("p t h d -> p t (h d)"), rd=[r_VX], wr=[r_in["C"]])
                k.dma(k.sp, vin[:, 2:4, :], VX[:, 18:20, :, :].rearrange("p t h d -> p t (h d)"), rd=[r_VX], wr=[r_in["C"]])
                for n_ in ("A", "B", "C"):
                    k.collective(lambda g, n_=n_: g.collective_compute("AllGather", ALU.bypass, replica_groups=[[0, 1, 2, 3], [4, 5, 6, 7]],
                                                                       ins=[l1_in[n_][:, :]], outs=[l1_out[n_][:, :]]),
                                 rd=[r_in[n_]], wr=[r_out[n_]])
                k.barrier()
            ovB = l1_out["B"].rearrange("(r p) c -> p r c", p=128)
            ovC = l1_out["C"].rearrange("(r p) c -> p r c", p=128)
            with contextlib.ExitStack() as es2:
                kp = self.sb(es2, "kpart", [128, 4, 4, 512], BF16)
                vp = self.sb(es2, "vpart", [128, 4, 4, 576], BF16)
                r_kp, r_vp = k.R("kpart"), k.R("vpart")
                k.dma(k.sp, kp[:].rearrange("p r a c -> p r (a c)"), ovB[:, :, :], rd=[r_out["B"]], wr=[r_kp])
                k.dma(k.sp, vp[:].rearrange("p r t c -> p r (t c)"), ovC[:, :, :], rd=[r_out["C"]], wr=[r_vp])
                top_k = KT2[:, :, NCX:NCX + 256]
                bot_k = KT2[:, :, NCX + 256 + NL:NCX + 512 + NL]
                top_v = VX[:, 2:4, :, :].rearrange("p t h d -> p t (h d)")
                bot_v = VX[:, 20:22, :, :].rearrange("p t h d -> p t (h d)")
                for r in range(4):
                    for (dst, src, sel, rr, r_dst) in ((top_k, kp[:, r, :, 256:512], "SELT", r_kp, r_KT2), (bot_k, kp[:, r, :, 0:256], "SELBT", r_kp, r_KT2),
                                                       (top_v, vp[:, r, 2:4, :], "SELT", r_vp, r_VX), (bot_v, vp[:, r, 0:2, :], "SELBT", r_vp, r_VX)):
                        if r == 0:
                            k.ts(dst, src, self.ppc(sel, r), None, ALU.mult, None, rd=[rr, self.r_pp], wr=[r_dst])
                        else:
                            k.stt(dst, src, self.ppc(sel, r), dst, ALU.mult, ALU.add, rd=[rr, self.r_pp, r_dst], wr=[r_dst])
                k.barrier()
            if self.stage == 7:
                self.dbg("KT2", KT2[:, 0, :], r_KT2, [128, NCX + 2560])
                self.dbg("cqn", cqn[:, 0, :], r_cqn, [128, NL])
                self.dbg("VX", VX[:, :, 0, :], r_VX, [128, 22, 72])
            if self.stage >= 8:
                self.na_attention(L, QT2, KT2, VX, r_QT2, r_KT2, r_VX)

    def apply_wout(self, L, OT2, r_OT2, row0):
        k = self.k
        with contextlib.ExitStack() as es:
            wo = self.sb(es, "l1wo", [128, 4, D], BF16)
            r_wo = k.R("l1wo")
            self.load_w(wo[:], self.cd_w_out[row0:row0 + 512, :].rearrange("(kc p) n -> p kc n", p=128), r_wo)
            banks = Banks(self.ps, self.r_ps, [0, 1, 2, 3])
            for li in range(4):
                for m in range(KC):
                    pb, r_pb = banks.get()
                    for pr in range(4):
                        k.mm(pb[:, :], wo[:, pr, m * 128:(m + 1) * 128], OT2[:, pr, li * 512:(li + 1) * 512], start=(pr == 0), stop=(pr == 3),
                             rd=[r_wo, r_OT2], wr=[r_pb])
                    dst, r_dst, v = self.x_dst(False, li, m, 512)
                    k.stt(dst, pb[:, :], self.modv(L, 2, m, 0), dst, ALU.mult, ALU.add, rd=[r_pb, self.r_mod, r_dst], wr=[r_dst])
            k.barrier()

    def na_attention(self, L, QT2, KT2, VX, r_QT2, r_KT2, r_VX):
        k = self.k
        with contextlib.ExitStack() as es:
            Mi = self.sb(es, "naMi", [128, 5, 8, 128], BF16)
            Ms = self.sb(es, "naMs", [128, 6, 8, 128], BF16)
            r_Mi, r_Ms = k.R("naMi"), k.R("naMs")
            bst = [self.sb(es, "nabst%d" % i, [128, 8, 128]) for i in range(2)]
            r_bst = k.Rs(2, "nabst")
            Et = [self.sb(es, "naE%d" % i, [128, 512], BF16) for i in range(3)]
            Pt = [self.sb(es, "naP%d" % i, [128, 512], BF16) for i in range(3)]
            r_Et, r_Pt = k.Rs(3, "naE"), k.Rs(3, "naP")
            Otok = [self.sb(es, "naOtok%d" % i, [128, 8, 64], BF16) for i in range(2)]
            r_Otok = k.Rs(2, "naOtok")
            rden = [self.sb(es, "narden%d" % i, [128, 4]) for i in range(2)]
            r_rden = k.Rs(2, "narden")
            Qpad = [self.sb(es, "naQpad%d" % i, [128, 8, 128], BF16) for i in range(2)]
            r_Qpad = k.Rs(2, "naQpad")
            for i in range(2):
                k.op(k.pool, lambda e, i=i: e.memset(Qpad[i][:].rearrange("p h q -> p (h q)"), 0.0), wr=[r_Qpad[i]])
            ib = 0
            for kt in range(5):
                b = ib % 2
                ib += 1
                k.dma(k.sp, bst[b][:], self.nab_int[kt], wr=[r_bst[b]])
                k.actf(Mi[:, kt, :, :], bst[b][:], AF.Exp, rd=[r_bst[b]], wr=[r_Mi])
            sbanks = Banks(self.ps, self.r_ps, [0, 1, 2])
            obanks = Banks(self.ps, self.r_ps, [3, 4, 5, 6])
            ie = 0
            for qb in range(16):
                if qb < 2:
                    base, nt, sp = 2 * qb - 4, 6, qb
                elif qb >= 14:
                    base, nt, sp = 2 * qb - 6, 6, 2 + qb - 14
                else:
                    base, nt, sp = 2 * qb - 4, 5, None
                if sp is not None:
                    for kt in range(6):
                        b = ib % 2
                        ib += 1
                        k.dma(k.sp, bst[b][:], self.nab_sp[sp, kt], wr=[r_bst[b]])
                        k.actf(Ms[:, kt, :, :], bst[b][:], AF.Exp, rd=[r_bst[b]], wr=[r_Ms])
                    M, r_M = Ms, r_Ms
                else:
                    M, r_M = Mi, r_Mi
                q0 = qb * 128
                qp, r_qp = Qpad[qb % 2], r_Qpad[qb % 2]
                qpv = qp[:, :, :].rearrange("p (a two) q -> p a two q", two=2)
                k.cp(qpv[0:64, :, 0, :], QT2[0:64, :, q0:q0 + 128], rd=[r_QT2], wr=[r_qp], E=k.pool)
                k.cp(qpv[64:128, :, 1, :], QT2[64:128, :, q0:q0 + 128], rd=[r_QT2], wr=[r_qp], E=k.pool)
                tiles = [("ctx", 0), ("ctx", 1)] + [("loc", kt) for kt in range(nt)]
                ot, r_ot = Otok[qb % 2], r_Otok[qb % 2]
                for hg in range(2):
                    pOs = [(self.ps[3 + hh], self.r_ps[3 + hh]) for hh in range(4)]
                    pend = None
                    for ti in range(len(tiles) + 1):
                        cur = None
                        if ti < len(tiles):
                            kind, kt = tiles[ti]
                            if kind == "ctx":
                                kcol, vt = kt * 128, kt
                            else:
                                row = base + 2 * kt
                                kcol, vt = NCX + (row + 4) * 64, 2 + (row + 4) // 2
                            pS, r_pS = sbanks.get()
                            for hh in range(4):
                                h = 4 * hg + hh
                                pr = h // 2
                                k.mm(pS[:, hh * 128:(hh + 1) * 128], KT2[:, pr, kcol:kcol + 128], qp[:, h, :],
                                     start=True, stop=True, rd=[r_KT2, r_qp], wr=[r_pS], sig=(hh == 3))
                            i3 = ie % 3
                            ie += 1
                            E, r_E = Et[i3], r_Et[i3]
                            k.actf(E[:, :], pS[:, :], AF.Exp, rd=[r_pS], wr=[r_E], scale=0.125)
                            if kind == "loc":
                                P, r_P = Pt[i3], r_Pt[i3]
                                k.tt(P[:, :].rearrange("p (h q) -> p h q", q=128), E[:, :].rearrange("p (h q) -> p h q", q=128),
                                     M[:, kt, 4 * hg:4 * hg + 4, :], ALU.mult, rd=[r_E, r_M], wr=[r_P])
                            else:
                                P, r_P = E, r_E
                            cur = (P, r_P, vt, ti)
                        if pend is not None:
                            P_, r_P_, vt_, ti_ = pend
                            for hh in range(4):
                                h = 4 * hg + hh
                                k.mm(pOs[hh][0][:, 0:65], P_[:, hh * 128:(hh + 1) * 128], VX[:, vt_, h, 0:65], start=(ti_ == 0),
                                     stop=(ti_ == len(tiles) - 1), rd=[r_P_, r_VX], wr=[pOs[hh][1]], sig=True)
                        pend = cur
                    rd_, r_rd = rden[hg], r_rden[hg]
                    for hh in range(4):
                        pO, r_pO = pOs[hh]
                        k.op(k.dve, lambda e: e.reciprocal(out=rd_[:, hh:hh + 1], in_=pO[:, 64:65]), rd=[r_pO], wr=[r_rd])
                        k.ts(ot[:, 4 * hg + hh, :], pO[:, 0:64], rd_[:, hh:hh + 1], None, ALU.mult, None, rd=[r_pO, r_rd], wr=[r_ot])
                pT, r_pT = self.ps[7], self.r_ps[7]
                pTb = pT[:, :].bitcast(BF16)
                for pr in range(4):
                    k.tr(pTb[:, pr * 128:(pr + 1) * 128], ot[:, 2 * pr:2 * pr + 2, :].rearrange("p h d -> p (h d)"), self.identb[:],
                         rd=[r_ot, self.r_const], wr=[r_pT], sig=(pr == 3))
                k.cp(QT2[:, :, q0:q0 + 128], pTb[:, 0:512].rearrange("p (a q) -> p a q", q=128), rd=[r_pT], wr=[r_QT2], E=k.act)
            if self.debug:
                self.dbg("OTna", QT2[:, 0, :], r_QT2, [128, NL])
            k.barrier()
        self.apply_wout(L, QT2, r_QT2, 0)

    def mla_attention(self, cqn, kvc, krc, r_cqn, r_kvc, r_krc, l1_out, r_out):
        k = self.k
        L = 1
        NK = NCX + 4 * NL
        NKT = NK // 128
        scale = 96.0 ** -0.5
        with contextlib.ExitStack() as es:
            OT2 = self.sb(es, "mlaOT2", [128, 4, NL], BF16)
            r_OT2 = k.R("mlaOT2")
            with contextlib.ExitStack() as es1:
                KVall = self.sb(es1, "KVall", [128, NK], BF16)
                Kh = self.sb(es1, "Kh", [128, NK], BF16)
                r_KV, r_Khn, r_Khr = k.R("KVall"), k.R("Khn"), k.R("Khr")
                ov = l1_out["A"].rearrange("(r p) c -> p r c", p=128)
                k.op(k.pool, lambda e: e.memset(Kh[64:128, :], 0.0), wr=[r_Khr])
                k.cp(KVall[:, 0:NCX], kvc[:, :], rd=[r_kvc], wr=[r_KV], E=k.pool)
                k.cp(Kh[64:96, 0:NCX], krc[64:96, :], rd=[r_krc], wr=[r_Khr], E=k.pool)
                k.dma(k.sp, KVall[:, NCX:].rearrange("p (r c) -> p r c", r=4), ov[:, :, 0:NL], rd=[r_out["A"]], wr=[r_KV])
                k.dma(k.sp, Kh[64:96, NCX:].rearrange("p (r c) -> p r c", r=4), ov[0:32, :, 2048:2048 + NL], rd=[r_out["A"]], wr=[r_Khr])
                wuq = self.sb(es1, "wuq", [128, 2, 8, 96], BF16)
                wuqs = self.sb(es1, "wuqs", [128, 2, 8, 96], BF16)
                wukv = self.sb(es1, "wukv", [128, 8, 128], BF16)
                r_wm = k.R("mlaw")
                self.load_w(wuq[:].rearrange("p c h d -> p c (h d)"), self.w_uq_d.rearrange("(c p) n -> p c n", p=128), r_wm)
                self.load_w(wuqs[:].rearrange("p c h d -> p c (h d)"), self.w_uqs_d.rearrange("(c p) n -> p c n", p=128), r_wm)
                self.load_w(wukv[:].rearrange("p h d -> p (h d)"), self.w_ukv_d[:, :], r_wm)
                Vh = self.sb(es1, "Vh", [128, NKT, 72], BF16)
                Qh = self.sb(es1, "Qh", [128, NL], BF16)
                r_Vh, r_Qh = k.R("Vh"), k.R("Qh")
                k.op(k.pool, lambda e: e.memset(Vh[:, :, 64:65], 1.0), wr=[r_Vh])
                k.op(k.pool, lambda e: e.memset(Qh[64:128, :], 0.0), wr=[r_Qh])
                rope = self.sb(es1, "ropeM", [128, 2, 512])
                r_rope = k.R("ropeM")
                rt = [self.sb(es1, "mlart%d" % i, [128, 512]) for i in range(2)]
                r_rt = k.Rs(2, "mlart")
                Otok = self.sb(es1, "mlaOtok", [128, 16, 8, 64], BF16)
                r_Otok = k.R("mlaOtok")
                Et = [self.sb(es1, "mlaE%d" % i, [128, 512], BF16) for i in range(4)]
                r_Et = k.Rs(4, "mlaE")
                rden = self.sb(es1, "mlarden", [128, 4])
                r_rden = k.R("mlarden")
                gbanks = Banks(self.ps, self.r_ps, [0])
                sbanks = Banks(self.ps, self.r_ps, [1, 2, 3])
                ie = 0
                for h in range(8):
                    for kb in range((NK + 511) // 512):
                        c0 = kb * 512
                        n = min(512, NK - c0)
                        pK, r_pK = gbanks.get()
                        k.mm(pK[0:64, 0:n], wukv[:, h, 0:64], KVall[:, c0:c0 + n], start=True, stop=True, rd=[r_wm, r_KV], wr=[r_pK])
                        k.cp(Kh[0:64, c0:c0 + n], pK[0:64, 0:n], rd=[r_pK], wr=[r_Khn], E=k.dve)
                    for g0 in range(0, NKT, 8):
                        nt = min(8, NKT - g0)
                        pV, r_pV = gbanks.get()
                        for t in range(nt):
                            kt = g0 + t
                            k.mm(pV[:, t * 64:(t + 1) * 64], KVall[:, kt * 128:(kt + 1) * 128], wukv[:, h, 64:128], start=True, stop=True,
                                 rd=[r_wm, r_KV], wr=[r_pV], sig=(t == nt - 1))
                        k.cp(Vh[:, g0:g0 + nt, 0:64], pV[:, 0:nt * 64].rearrange("p (t d) -> p t d", d=64), rd=[r_pV], wr=[r_Vh],
                             E=k.dve)
                    for qb in range(4):
                        qc = slice(qb * 512, (qb + 1) * 512)
                        pA, r_pA = gbanks.get()
                        pB, r_pB = sbanks.get()
                        for c in range(2):
                            k.mm(pA[0:96, :], wuq[:, c, h, :], cqn[:, c, qc], start=(c == 0), stop=(c == 1), rd=[r_wm, r_cqn], wr=[r_pA])
                        for c in range(2):
                            k.mm(pB[0:96, :], wuqs[:, c, h, :], cqn[:, c, qc], start=(c == 0), stop=(c == 1), rd=[r_wm, r_cqn], wr=[r_pB])
                        k.cp(Qh[0:64, qc], pA[0:64, :], rd=[r_pA], wr=[r_Qh], E=k.dve)
                        k.dma(k.sp, rope[64:96, :, :], self.rope_d[:, :, qc], wr=[r_rope])
                        k.tt(rt[0][64:96, :], pA[64:96, :], rope[64:96, 0, :], ALU.mult, rd=[r_pA, r_rope], wr=[r_rt[0]])
                        k.tt(rt[1][64:96, :], pB[64:96, :], rope[64:96, 1, :], ALU.mult, rd=[r_pB, r_rope], wr=[r_rt[1]])
                        k.tt(Qh[64:96, qc], rt[0][64:96, :], rt[1][64:96, :], ALU.add, rd=r_rt, wr=[r_Qh])
                    for qb in range(4):
                        pOs = [(self.ps[4 + sub], self.r_ps[4 + sub]) for sub in range(4)]
                        pend = []
                        for kt in range(NKT + 2):
                            cur = None
                            if kt < NKT:
                                pS, r_pS = sbanks.get()
                                k.mm(pS[:, :], Kh[:, kt * 128:(kt + 1) * 128], Qh[:, qb * 512:(qb + 1) * 512], start=True, stop=True,
                                     rd=[r_Khn, r_Khr, r_Qh], wr=[r_pS])
                                i3 = ie % 4
                                ie += 1
                                E, r_E = Et[i3], r_Et[i3]
                                k.actf(E[:, :], pS[:, :], AF.Exp, rd=[r_pS], wr=[r_E], scale=scale)
                                cur = (E, r_E, kt)
                                pend.append(cur)
                            if len(pend) > 2 or (kt >= NKT and pend):
                                E_, r_E_, kt_ = pend.pop(0)
                                for sub in range(4):
                                    k.mm(pOs[sub][0][:, 0:65], E_[:, sub * 128:(sub + 1) * 128], Vh[:, kt_, 0:65], start=(kt_ == 0), stop=(kt_ == NKT - 1),
                                         rd=[r_E_, r_Vh], wr=[pOs[sub][1]], sig=(sub == 3 or kt_ == NKT - 1))
                        for sub in range(4):
                            pO, r_pO = pOs[sub]
                            k.op(k.dve, lambda e: e.reciprocal(out=rden[:, sub:sub + 1], in_=pO[:, 64:65]), rd=[r_pO], wr=[r_rden])
                            k.ts(Otok[:, qb * 4 + sub, h, :], pO[:, 0:64], rden[:, sub:sub + 1], None, ALU.mult, None, rd=[r_pO, r_rden], wr=[r_Otok])
                pT, r_pT = self.ps[0], self.r_ps[0]
                pTb = pT[:, :].bitcast(BF16)
                for qt in range(16):
                    for pr in range(4):
                        k.tr(pTb[:, pr * 128:(pr + 1) * 128], Otok[:, qt, 2 * pr:2 * pr + 2, :].rearrange("p h d -> p (h d)"), self.identb[:],
                             rd=[r_Otok, self.r_const], wr=[r_pT], sig=(pr == 3))
                    k.cp(OT2[:, :, qt * 128:(qt + 1) * 128], pTb[:, 0:512].rearrange("p (a q) -> p a q", q=128), rd=[r_pT], wr=[r_OT2],
                         E=(k.act if qt % 2 else k.dve))
                if self.debug:
                    self.dbg("OTmla", OT2[:, 0, :], r_OT2, [128, NL])
                k.barrier()
            self.apply_wout(L, OT2, r_OT2, 512)

    def final_out(self):
        k = self.k
        with contextlib.ExitStack() as es:
            tmp = self.norm_tmp(es, banks_idx=(6, 7))
            yf = self.sb(es, "yf", [128, KC, 512])
            r_yf = k.R("yf")
            ytok = [self.sb(es, "ytok%d" % i, [128, D]) for i in range(2)]
            r_ytok = k.Rs(2, "ytok")
            r_y = k.R("y_out")
            self.out_res.append(r_y)
            banks = Banks(self.ps, self.r_ps, [0, 1, 2, 3])
            it = 0
            for li in range(4):
                n = 512
                pb, r_pb = tmp["banks"].get()
                for kc in range(KC):
                    i = tmp["i"] % 3
                    tmp["i"] += 1
                    sq, r_sq = tmp["sq"][i], tmp["r_sq"][i]
                    k.actf(sq[:, :], self.xT[:, kc, li * 512:(li + 1) * 512], AF.Square, rd=[self.r_xT[li]], wr=[r_sq])
                    k.mm(pb[:, :], self.ccc("ONES", 128), sq[:, :], start=(kc == 0), stop=(kc == KC - 1), rd=[r_sq, self.r_cc], wr=[r_pb], sig=True)
                rstd, r_rstd = tmp["rstd"], tmp["r_rstd"]
                k.ts(rstd[:, :], pb[:, :], 1.0 / D, EPS, ALU.mult, ALU.add, rd=[r_pb], wr=[r_rstd])
                k.actf(rstd[:, :], rstd[:, :], AF.Sqrt, rd=[r_rstd], wr=[r_rstd])
                k.op(k.dve, lambda e: e.reciprocal(out=rstd[:, :], in_=rstd[:, :]), rd=[r_rstd], wr=[r_rstd])
                for kc in range(KC):
                    k.stt(yf[:, kc, :], self.xT[:, kc, li * 512:(li + 1) * 512], self.ppc("FNG", kc), rstd[:, :], ALU.mult, ALU.mult,
                          rd=[self.r_xT[li], self.r_pp, r_rstd], wr=[r_yf])
                for t in range(4):
                    yt, r_yt = ytok[it % 2], r_ytok[it % 2]
                    it += 1
                    for half in range(2):
                        pt, r_pt = banks.get()
                        for kk in range(4):
                            kc = half * 4 + kk
                            k.tr(pt[:, kk * 128:(kk + 1) * 128], yf[:, kc, t * 128:(t + 1) * 128], self.ccc("IDENT", 128),
                                 rd=[r_yf, self.r_cc], wr=[r_pt], sig=(kk == 3))
                        k.cp(yt[:, half * 512:(half + 1) * 512], pt[:, :], rd=[r_pt], wr=[r_yt], E=(k.act if half else k.dve))
                    r0 = li * 512 + t * 128
                    k.dma(k.sp, self.y[r0:r0 + 128, :], yt[:, :], rd=[r_yt], wr=[r_y])


def make_in_maps(inp):
    inp = {k_: np.asarray(v) for k_, v in inp.items()}
    cc = make_cc()
    shared = {
        "cc": cc,
        "ada_w": np.ascontiguousarray(inp["ada_w"]),
        "ab_w_in": np.ascontiguousarray(inp["ab_w_in"][0]),
        "ab_w_out": np.ascontiguousarray(inp["ab_w_out"][0]),
        "pool_w": np.ascontiguousarray(inp["pool_w"][0]),
        "moe_wr": np.ascontiguousarray(np.concatenate([inp["moe_w_rg"], inp["moe_w_re"]], axis=2)),
        "moe_w_gate": np.ascontiguousarray(inp["moe_w_gate"].reshape(2, 32, D, 256)),
        "moe_w_up": np.ascontiguousarray(inp["moe_w_up"].reshape(2, 32, D, 256)),
        "moe_w_down": np.ascontiguousarray(inp["moe_w_down"].reshape(2, 32, 256, D)),
    }
    perm = _rope_perm()
    w_in1 = inp["cd_w_in"][0]
    wkr = np.zeros((D, 192), np.float32)
    wkr[:, 64:96] = w_in1[:, 1920:1952]
    wkr[:, 96 + 64:96 + 96] = w_in1[:, 1920:1952][:, perm]
    wuq = inp["mla_w_uq"][0].reshape(256, 8, 96)
    wuqs = wuq.copy()
    wuqs[:, :, 64:96] = wuq[:, :, 64:96][:, :, perm]
    shared.update({
        "cd_w_in": np.ascontiguousarray(w_in1), "cd_w_out": np.ascontiguousarray(inp["cd_w_out"][0]), "wkr": wkr,
        "w_uq": np.ascontiguousarray(wuq.reshape(256, 768)), "w_uqs": np.ascontiguousarray(wuqs.reshape(256, 768)),
        "w_ukv": np.ascontiguousarray(inp["mla_w_ukv"][0]),
    })
    maps = []
    for c in range(NCORES):
        b, j = c // 4, c % 4
        s0 = j * NL
        halo = np.zeros((16, D), np.float32)
        if j > 0:
            halo[0:8] = inp["x"][b, s0 - 8:s0]
        if j < 3:
            halo[8:16] = inp["x"][b, s0 + NL:s0 + NL + 8]
        m = dict(shared)
        m["x_own"] = np.ascontiguousarray(inp["x"][b, s0:s0 + NL])
        m["x_halo"] = halo
        m["ctx_b"] = np.ascontiguousarray(inp["ctx"][b])
        m["pp"] = make_pp(inp, c)
        m["rope"] = make_rope(j)
        m["nab_int"], m["nab_sp"] = make_na_tables(inp["na_rpb"][0], j)
        maps.append(m)
    return maps


def _rope_perm():
    p = np.arange(32)
    for g0 in (0, 16):
        p[g0:g0 + 8] = g0 + 8 + np.arange(8)
        p[g0 + 8:g0 + 16] = g0 + np.arange(8)
    return p


def make_rope(j):
    t = j * NL + np.arange(NL)
    pos = (t // 64).astype(np.float32), (t % 64).astype(np.float32)
    inv = (10000.0 ** (-np.arange(8, dtype=np.float32) / 8)).astype(np.float32)
    tab = np.zeros((32, 2, NL), np.float32)
    for gi, g0 in enumerate((0, 16)):
        ang = pos[gi][None, :] * inv[:, None]
        c, sn = np.cos(ang), np.sin(ang)
        tab[g0:g0 + 8, 0] = c
        tab[g0 + 8:g0 + 16, 0] = c
        tab[g0:g0 + 8, 1] = -sn
        tab[g0 + 8:g0 + 16, 1] = sn
    return tab


def _na_table(rpb, R0, base_row, ntile):
    qr = np.repeat(np.arange(2), 64)
    qc = np.tile(np.arange(64), 2)
    Rq = R0 + qr
    rs = np.clip(Rq - 4, 0, 120)
    cs = np.clip(qc - 8, 0, 48)
    out = np.full((ntile, 128, 8, 128), -BIG, np.float32)
    kc = np.tile(np.arange(64), 2)
    for kt in range(ntile):
        kr = base_row + 2 * kt + np.repeat(np.arange(2), 64)
        valid = ((kr[:, None] >= rs[None, :]) & (kr[:, None] < rs[None, :] + 8) & (kc[:, None] >= cs[None, :])
                 & (kc[:, None] < cs[None, :] + 16) & (kr[:, None] >= 0) & (kr[:, None] < 128))
        dr = np.clip(kr[:, None] - Rq[None, :] + 7, 0, 14)
        dc = np.clip(kc[:, None] - qc[None, :] + 15, 0, 30)
        b = rpb[:, dr, dc]
        out[kt] = np.where(valid[None], b, np.float32(-BIG)).transpose(1, 0, 2)
    return out


def make_na_tables(rpb, j):
    nab_int = _na_table(rpb, 64, 60, 5)
    sp = []
    for qb, off in ((0, 4), (1, 4), (14, 6), (15, 6)):
        R0 = 32 * j + 2 * qb
        sp.append(_na_table(rpb, R0, R0 - off, 6))
    return nab_int, np.stack(sp)


_PROG = {}


def run_prog(inp, stage=99, debug=False, l1only=False):
    key = (stage, debug, l1only)
    if key not in _PROG:
        _PROG[key] = Prog(stage=stage, debug=debug, l1only=l1only)
    prog = _PROG[key]
    maps = make_in_maps(inp)
    res = run_bass_kernel_spmd(prog.nc, maps, core_ids=list(range(NCORES)))
    return prog, res


def kernel(**inputs):
    prog, res = run_prog(inputs)
    out = np.zeros((2, 8192, D), np.float32)
    for c in range(NCORES):
        b, j = c // 4, c % 4
        out[b, j * NL:(j + 1) * NL] = res.results[c]["y"]
    return out
```
